# Optimizing a Trainium2 kernel written in Bass

```python
import jax, jax.numpy as jnp
from jax import lax
import numpy as np

D_MODEL = 1024
BATCH = 1
SEQ = 16384
DEPTH = 2

GRID_W = 64
CTX_LEN = 256
N_MIXERS = 2
N_HEADS = 8
N_KV_HEADS = 2
HEAD_DIM = 128
KV_GROUP = N_HEADS // N_KV_HEADS
ROPE_THETA = 10000.0
Q_BLOCK = 128
POOL_WINDOWS = (2, 4, 8, 16)
N_POOL_GROUPS = len(POOL_WINDOWS)
POOL_GROUP = D_MODEL // N_POOL_GROUPS
N_EXPERTS = 32
TOP_K = 4
D_FF_EXPERT = D_MODEL
SWIGLU_LIMIT = 7.0
SWIGLU_ALPHA = 1.702
NORM_EPS = 1e-6
N_MOD = 6

kernel_name = "hybrid_attn_pool_moe_dit_block"


def _rmsnorm(x, g):
    x32 = x.astype(jnp.float32)
    y = x32 * lax.rsqrt(jnp.mean(x32 * x32, axis=-1, keepdims=True) + NORM_EPS)
    return (y * g.astype(jnp.float32)).astype(x.dtype)


def _modulate(h, shift, scale):
    return h * (1 + scale) + shift


def _rope_1d(x, pos):
    half = x.shape[-1] // 2
    inv_freq = ROPE_THETA ** (-jnp.arange(half, dtype=jnp.float32) / half)
    ang = pos.astype(jnp.float32)[:, None] * inv_freq[None, :]
    cos = jnp.cos(ang)[:, None, :]
    sin = jnp.sin(ang)[:, None, :]
    x32 = x.astype(jnp.float32)
    x1, x2 = x32[..., :half], x32[..., half:]
    return jnp.concatenate([x1 * cos - x2 * sin, x2 * cos + x1 * sin], axis=-1).astype(x.dtype)


def _rope_2d(x, row, col):
    rd = HEAD_DIM // 2
    return jnp.concatenate([_rope_1d(x[..., :rd], row), _rope_1d(x[..., rd:], col)], axis=-1)


def _attend(q, k, v):
    s = jnp.einsum('bqkgd,btkd->bkgqt', q, k, preferred_element_type=jnp.float32) * (HEAD_DIM ** -0.5)
    p = jax.nn.softmax(s, axis=-1)
    o = jnp.einsum('bkgqt,btkd->bqkgd', p, v.astype(jnp.float32))
    return o.astype(v.dtype)


def _attention_mixer(h_lat, h_ctx, w_qkv, q_g, k_g, w_o, row, col, need_ctx_out):
    B, S, _ = h_lat.shape
    C = h_ctx.shape[1]

    def project(h):
        qkv = h @ w_qkv
        q, k, v = jnp.split(qkv, [N_HEADS * HEAD_DIM, (N_HEADS + N_KV_HEADS) * HEAD_DIM], axis=-1)
        q = _rmsnorm(q.reshape(B, -1, N_HEADS, HEAD_DIM), q_g)
        k = _rmsnorm(k.reshape(B, -1, N_KV_HEADS, HEAD_DIM), k_g)
        v = v.reshape(B, -1, N_KV_HEADS, HEAD_DIM)
        return q, k, v

    q_l, k_l, v_l = project(h_lat)
    q_c, k_c, v_c = project(h_ctx)
    q_l = _rope_2d(q_l, row, col)
    k_l = _rope_2d(k_l, row, col)
    k_all = jnp.concatenate([k_l, k_c], axis=1)
    v_all = jnp.concatenate([v_l, v_c], axis=1)

    n_blk = S // Q_BLOCK
    q_blocks = q_l.reshape(B, n_blk, Q_BLOCK, N_KV_HEADS, KV_GROUP, HEAD_DIM).transpose(1, 0, 2, 3, 4, 5)
    o_l = lax.map(lambda qb: _attend(qb, k_all, v_all), q_blocks)
    o_l = o_l.transpose(1, 0, 2, 3, 4, 5).reshape(B, S, N_HEADS * HEAD_DIM) @ w_o
    if need_ctx_out:
        o_c = _attend(q_c.reshape(B, C, N_KV_HEADS, KV_GROUP, HEAD_DIM), k_c, v_c)
        o_c = o_c.reshape(B, C, N_HEADS * HEAD_DIM) @ w_o
    else:
        o_c = None
    return o_l, o_c


def _pool_mixer(h, w_pool, scale):
    B, S, D = h.shape
    h32 = h.astype(jnp.float32)
    cs = jnp.concatenate([jnp.zeros((B, 1, D), jnp.float32), lax.cumsum(h32, axis=1)], axis=1)
    t = jnp.arange(S)
    outs = []
    for gi, w in enumerate(POOL_WINDOWS):
        lo = jnp.clip(t - w // 2, 0, S)
        hi = jnp.clip(t - w // 2 + w, 0, S)
        sl = slice(gi * POOL_GROUP, (gi + 1) * POOL_GROUP)
        csg = cs[:, :, sl]
        mean = (csg[:, hi, :] - csg[:, lo, :]) / (hi - lo).astype(jnp.float32)[None, :, None]
        outs.append(mean - h32[:, :, sl])
    pooled = jnp.concatenate(outs, axis=-1).astype(h.dtype).reshape(B, S, N_POOL_GROUPS, POOL_GROUP)
    y = jnp.einsum('bsgc,gcd->bsgd', pooled, w_pool).reshape(B, S, D)
    return y * scale


def _moe(h, router_w, router_b, w_gu, b_gu, w_down, b_down):
    logits = (h @ router_w + router_b).astype(jnp.float32)
    top_val, top_idx = lax.top_k(logits, TOP_K)
    top_w = jax.nn.softmax(top_val, axis=-1)
    gates = jnp.sum(jax.nn.one_hot(top_idx, N_EXPERTS, dtype=jnp.float32) * top_w[..., None], axis=1)

    def expert(acc, p):
        wgu, bgu, wd, bd, g = p
        gu = h @ wgu + bgu
        gate = jnp.minimum(gu[:, :D_FF_EXPERT], SWIGLU_LIMIT)
        up = jnp.clip(gu[:, D_FF_EXPERT:], -SWIGLU_LIMIT, SWIGLU_LIMIT)
        glu = gate * jax.nn.sigmoid(SWIGLU_ALPHA * gate)
        y = ((up + 1) * glu) @ wd + bd
        return acc + g[:, None].astype(acc.dtype) * y, None

    out, _ = lax.scan(expert, jnp.zeros_like(h), (w_gu, b_gu, w_down, b_down, gates.T))
    return out


def setup_inputs(seed: int = 0) -> dict:
    key = jax.random.key(seed)
    ks = jax.random.split(key, 24)
    f32 = jnp.float32

    def nrm(k, shape, s):
        return jax.random.normal(k, shape, f32) * s

    n_attn = len([i for i in range(DEPTH) if i % N_MIXERS == 0])
    n_pool = DEPTH - n_attn
    D, E, F = D_MODEL, N_EXPERTS, D_FF_EXPERT
    qkv_w = (N_HEADS + 2 * N_KV_HEADS) * HEAD_DIM
    return {
        "x": nrm(ks[0], (BATCH, SEQ, D), 1.0),
        "c": nrm(ks[1], (BATCH, D), 1.0),
        "ctx": nrm(ks[2], (BATCH, CTX_LEN, D), 1.0),
        "c_ctx": nrm(ks[3], (D,), 1.0),
        "ada_w": nrm(ks[4], (DEPTH, D, N_MOD * D), 0.5 * D ** -0.5),
        "ada_b": nrm(ks[5], (DEPTH, N_MOD * D), 0.01),
        "norm_mix": 1.0 + nrm(ks[6], (DEPTH, D), 0.05),
        "norm_ffn": 1.0 + nrm(ks[7], (DEPTH, D), 0.05),
        "attn_w_qkv": nrm(ks[8], (n_attn, D, qkv_w), D ** -0.5),
        "attn_q_norm": 1.0 + nrm(ks[9], (n_attn, HEAD_DIM), 0.05),
        "attn_k_norm": 1.0 + nrm(ks[10], (n_attn, HEAD_DIM), 0.05),
        "attn_w_o": nrm(ks[11], (n_attn, N_HEADS * HEAD_DIM, D), (N_HEADS * HEAD_DIM) ** -0.5),
        "pool_w": nrm(ks[12], (n_pool, N_POOL_GROUPS, POOL_GROUP, POOL_GROUP), POOL_GROUP ** -0.5),
        "pool_scale": 1.0 + nrm(ks[13], (n_pool, D), 0.1),
        "moe_router_w": nrm(ks[14], (DEPTH, D, E), D ** -0.5),
        "moe_router_b": nrm(ks[15], (DEPTH, E), 0.01),
        "moe_w_gu": nrm(ks[16], (DEPTH, E, D, 2 * F), D ** -0.5),
        "moe_b_gu": nrm(ks[17], (DEPTH, E, 2 * F), 0.01),
        "moe_w_down": nrm(ks[18], (DEPTH, E, F, D), F ** -0.5),
        "moe_b_down": nrm(ks[19], (DEPTH, E, D), 0.01),
        "final_norm": 1.0 + nrm(ks[20], (D,), 0.05),
    }


def reference(x, c, ctx, c_ctx, ada_w, ada_b, norm_mix, norm_ffn, attn_w_qkv, attn_q_norm, attn_k_norm,
              attn_w_o, pool_w, pool_scale, moe_router_w, moe_router_b, moe_w_gu, moe_b_gu, moe_w_down,
              moe_b_down, final_norm):
    B, S, D = x.shape
    C = ctx.shape[1]
    ROWS = S // GRID_W
    row = jnp.repeat(jnp.arange(ROWS), GRID_W)
    col = jnp.tile(jnp.arange(GRID_W), ROWS)

    x_lat, x_ctx = x, ctx
    for i in range(DEPTH):
        kind = i % N_MIXERS
        mix_idx = i // N_MIXERS
        ctx_needed = any((j % N_MIXERS) == 0 for j in range(i + 1, DEPTH))

        m_l = jax.nn.silu(c) @ ada_w[i] + ada_b[i]
        m_c = jax.nn.silu(c_ctx) @ ada_w[i] + ada_b[i]
        sh_l, sc_l, g_l, shf_l, scf_l, gf_l = jnp.split(m_l[:, None, :], N_MOD, axis=-1)
        sh_c, sc_c, g_c, shf_c, scf_c, gf_c = jnp.split(m_c[None, None, :], N_MOD, axis=-1)

        h_l = _modulate(_rmsnorm(x_lat, norm_mix[i]), sh_l, sc_l)
        h_c = _modulate(_rmsnorm(x_ctx, norm_mix[i]), sh_c, sc_c)
        if kind == 0:
            y_l, y_c = _attention_mixer(h_l, h_c, attn_w_qkv[mix_idx], attn_q_norm[mix_idx],
                                        attn_k_norm[mix_idx], attn_w_o[mix_idx], row, col, ctx_needed)
        else:
            y_l = _pool_mixer(h_l, pool_w[mix_idx], pool_scale[mix_idx])
            y_c = _pool_mixer(h_c, pool_w[mix_idx], pool_scale[mix_idx]) if ctx_needed else None
        x_lat = x_lat + g_l * y_l
        if ctx_needed:
            x_ctx = x_ctx + g_c * y_c

        hf_l = _modulate(_rmsnorm(x_lat, norm_ffn[i]), shf_l, scf_l).reshape(B * S, D)
        moe_args = (moe_router_w[i], moe_router_b[i], moe_w_gu[i], moe_b_gu[i], moe_w_down[i], moe_b_down[i])
        if ctx_needed:
            hf_c = _modulate(_rmsnorm(x_ctx, norm_ffn[i]), shf_c, scf_c).reshape(B * C, D)
            f = _moe(jnp.concatenate([hf_l, hf_c], axis=0), *moe_args)
            x_lat = x_lat + gf_l * f[:B * S].reshape(B, S, D)
            x_ctx = x_ctx + gf_c * f[B * S:].reshape(B, C, D)
        else:
            f = _moe(hf_l, *moe_args)
            x_lat = x_lat + gf_l * f.reshape(B, S, D)

    return _rmsnorm(x_lat, final_norm)
```

```python
import contextlib
import math
import numpy as np
import ml_dtypes
import concourse.bass as bass
import concourse.mybir as mybir
from concourse.bass_utils import run_bass_kernel_spmd

F32 = mybir.dt.float32
BF16 = mybir.dt.bfloat16
ALU = mybir.AluOpType
AF = mybir.ActivationFunctionType

NCORES = 8
D = 1024
S = 16384
CTX = 256
OWN = S // NCORES
NT = 17
NEXT = NT * 128
NKT = (S + CTX) // 128
E = 32
EPS = 1e-6
ALPHA = 1.702
LIM = 7.0
ENGS = ("pe", "act", "dve", "pool", "sp")
SPARSE = True
CAP = 512
NTILE = 48
NSLOT = NTILE * CAP
BLKS = [(0, 512), (512, 512), (1024, 512), (1536, 512), (2048, 16)]


class Reg:
    __slots__ = ("w", "r")

    def __init__(self):
        self.w = None
        self.r = {}


class Prog:
    def __init__(self, nc):
        self.nc = nc
        self.q = {e: [] for e in ENGS}
        self.cnt = {e: 0 for e in ENGS}
        self.dma_sems = {}
        self.sem_handles = {}

    def _deps(self, eng, reads, writes):
        deps = {}

        def add(t):
            if t is None:
                return
            k, v = t
            if eng == "pe" and k == "Epe":
                return
            if deps.get(k, 0) < v:
                deps[k] = v
        for x in reads:
            add(x.w)
        for x in writes:
            add(x.w)
            for k, v in x.r.items():
                add((k, v))
        return list(deps.items())

    def _mark(self, t, reads, writes):
        k, v = t
        for x in reads:
            if x.r.get(k, 0) < v:
                x.r[k] = v
        for x in writes:
            x.w = t
            x.r = {}

    def op(self, eng, fns, reads=(), writes=(), extra=()):
        if callable(fns):
            fns = [fns]
        deps = self._deps(eng, reads, writes) + [d for d in extra if d is not None]
        self.cnt[eng] += 1
        t = ("E" + eng, self.cnt[eng])
        self.q[eng].append(("op", fns, deps, t))
        self._mark(t, reads, writes)
        return t

    def dma(self, eng, out, in_, sem, reads=(), writes=(), extra=(), **kw):
        if sem in ("c0", "c1", "dbg"):
            self.auto_i = getattr(self, "auto_i", 0) + 1
            sem = f"a{self.auto_i % 24}"
        deps = self._deps(eng, reads, writes) + [d for d in extra if d is not None]
        if sem in self.dma_sems:
            deps.append(("D" + sem, self.dma_sems[sem]))
        self.dma_sems[sem] = self.dma_sems.get(sem, 0) + 16
        t = ("D" + sem, self.dma_sems[sem])
        self.q[eng].append(("dma", (out, in_, kw), deps, t))
        self._mark(t, reads, writes)
        return t

    def dmaf(self, eng, fn, sem, reads=(), writes=(), extra=()):
        deps = self._deps(eng, reads, writes) + [d for d in extra if d is not None]
        if sem in self.dma_sems:
            deps.append(("D" + sem, self.dma_sems[sem]))
        self.dma_sems[sem] = self.dma_sems.get(sem, 0) + 16
        t = ("D" + sem, self.dma_sems[sem])
        self.q[eng].append(("dmaf", fn, deps, t))
        self._mark(t, reads, writes)
        return t

    def wait(self, eng, deps):
        self.q[eng].append(("wait", None, [d for d in deps if d is not None], None))

    def emit(self):
        nc = self.nc
        keys = ["E" + e for e in ENGS] + ["D" + s for s in self.dma_sems]
        with contextlib.ExitStack() as st:
            for k in keys:
                self.sem_handles[k] = st.enter_context(nc.semaphore(k))
            blk = st.enter_context(nc.Block())
            engmap = {"pe": blk.tensor, "act": blk.scalar, "dve": blk.vector,
                      "pool": blk.gpsimd, "sp": blk.sync}
            sh = self.sem_handles
            for e in ENGS:
                items = self.q[e]

                def body(engine, items=items):
                    waited = {}
                    for kind, payload, deps, t in items:
                        for (k, v) in deps:
                            if waited.get(k, 0) >= v:
                                continue
                            engine.wait_ge(sh[k], v)
                            waited[k] = v
                        if kind == "wait":
                            continue
                        if kind == "op":
                            ins = None
                            for f in payload:
                                ins = f(engine)
                            ins.then_inc(sh[t[0]], 1)
                        elif kind == "dmaf":
                            payload(engine).then_inc(sh[t[0]], 16)
                        else:
                            out, in_, kw = payload
                            engine.dma_start(out=out, in_=in_, **kw).then_inc(sh[t[0]], 16)
                engmap[e](body)


def mm(out, lhsT, rhs, start=True, stop=True):
    return lambda e: e.matmul(out, lhsT, rhs, start=start, stop=stop)


def tp(out, in_, ident):
    return lambda e: e.transpose(out, in_, ident)


def actf(out, in_, func, bias=None, scale=None, accum=None):
    def f(e):
        kw = {}
        if bias is not None:
            kw["bias"] = bias
        if scale is not None:
            kw["scale"] = scale
        if accum is not None:
            kw["accum_out"] = accum
        return e.activation(out, in_, func, **kw)
    return f


def ts(out, in0, s1, s2, op0, op1=None):
    if op1 is None:
        return lambda e: e.tensor_scalar(out, in0, s1, None, op0)
    return lambda e: e.tensor_scalar(out, in0, s1, s2, op0, op1)


def tt(out, in0, in1, op):
    return lambda e: e.tensor_tensor(out, in0, in1, op)


def stt(out, in0, scalar, in1, op0, op1, accum=None):
    if accum is None:
        return lambda e: e.scalar_tensor_tensor(out, in0, scalar, in1, op0, op1)
    return lambda e: e.scalar_tensor_tensor(out, in0, scalar, in1, op0, op1, accum_out=accum)


def cp(out, in_):
    return lambda e: e.tensor_copy(out, in_)


class Ring:
    def __init__(self, bufs):
        self.bufs = bufs
        self.regs = [Reg() for _ in bufs]
        self.i = 0

    def next(self):
        s = self.i % len(self.bufs)
        self.i += 1
        return self.bufs[s], self.regs[s]


def build(dbg=None):
    nc = bass.Bass("TRN2", target_bir_lowering=False)
    P = Prog(nc)

    def din(name, shape, dt=F32):
        return nc.dram_tensor(name, list(shape), dt, kind="ExternalInput").ap()

    x_all = din("x_all", [S, D])
    x_ext = din("x_ext", [NEXT, D])
    ctx_in = din("ctx", [CTX, D])
    c2 = din("c2", [2, D])
    ada_w = din("ada_w", [2, D, 6 * D])
    ada_b = din("ada_b", [2, 6 * D])
    norm_mix = din("norm_mix", [2, D])
    norm_ffn = din("norm_ffn", [2, D])
    wqkv = din("wqkv", [D, 1536])
    qk_g = din("qk_g", [2, 128])
    wo = din("wo", [D, D])
    pool_w = din("pool_w", [4, 256, 256])
    pool_scale = din("pool_scale", [D])
    router_w = din("router_w", [2, D, E])
    router_b = din("router_b", [2, E])
    w_gu = din("w_gu", [2, E, D, 2 * D])
    b_gu = din("b_gu", [2, E, 2 * D])
    w_down = din("w_down", [2, E, D, D])
    b_down = din("b_down", [2, E, D])
    final_norm = din("final_norm", [D])
    ropek = din("ropek", [2, 128, S])
    ropeq = din("ropeq", [2, 128, NEXT])
    consts = din("consts", [7, 128, 128])
    bands = din("bands", [128, 36, 128], BF16)
    hmask = din("hmask", [128, 1])
    out = nc.dram_tensor("out", [OWN, D], F32, kind="ExternalOutput").ap()
    dbg_out = None
    if dbg is not None:
        dbg_out = nc.dram_tensor("dbg", list(dbg[1]), F32, kind="ExternalOutput").ap()

    mvec = nc.dram_tensor("mvec", [2, 2, 6 * D], F32).ap()
    bvec = nc.dram_tensor("bvec", [2, 1536], F32).ap()
    xspill = nc.dram_tensor("xspill", [NEXT, D], F32).ap()
    R_mvec = [Reg(), Reg()]
    R_bvec = Reg()
    R_xspill = Reg()

    def col(ap1d, n=8):
        return ap1d.rearrange("(k p) -> p k", p=128)

    def bc(ap1d, parts=128):
        return ap1d.rearrange("(o n) -> o n", o=1).partition_broadcast(parts)

    NC_KW = dict(allow_slow_non_contiguous=True)
    final_waits = []

    with contextlib.ExitStack() as top:
        def sb(name, shape, dt=F32, st=top):
            return st.enter_context(nc.sbuf_tensor(name, list(shape), dt))

        def ps(name, shape, dt=F32, st=top):
            return st.enter_context(nc.psum_tensor(name, list(shape), dt))

        cst = sb("cst", [128, 7, 128])
        R_cst = Reg()
        P.dma("sp", cst[:], consts.rearrange("c p n -> p c n"), "c0", writes=[R_cst])
        identf = cst[:, 0, :]
        cstb = sb("cstb", [128, 7, 128], BF16)
        R_cstb = Reg()
        P.op("dve", cp(cstb[:], cst[:]), reads=[R_cst], writes=[R_cstb])
        identb = cstb[:, 0, :]
        onesmb = cstb[:, 1, :]
        rotb = cstb[:, 2, :]
        onesb = cstb[:, 3, :]
        ustrb = cstb[:, 4, :]
        iota_e = cst[:, 5, 0:32]
        vmask = cst[:, 5, 32:33]
        notvbig = cst[:, 5, 33:34]
        kpc = cst[:, 5, 40:48]
        epsc = sb("epsc", [128, 1])
        R_eps = Reg()
        P.op("pool", lambda e: e.memset(epsc[:], EPS), writes=[R_eps])
        dmy = sb("dmy", [128, 1])
        R_dmy = Reg()
        hm = sb("hm", [128, 1])
        R_hm = Reg()
        P.dma("sp", hm[:], hmask, "c0", writes=[R_hm])

        hs_d = [nc.dram_tensor(f"hs{l}", [NSLOT, D], BF16).ap() for l in range(2)]
        ys_d = [nc.dram_tensor(f"ys{l}", [NSLOT, D], F32).ap() for l in range(2)]
        zt = sb("zt", [128, D], BF16)
        R_zt = Reg()
        P.op("pool", lambda e: e.memset(zt[:], 0.0), writes=[R_zt])
        R_hs0 = [[Reg() for _ in range(NTILE)] for _ in range(2)]
        for l_ in range(2):
            for e_ in range(NTILE):
                P.dma("pool", hs_d[l_][e_ * CAP:(e_ + 1) * CAP, :].rearrange("(s p) d -> p s d", p=128),
                      zt[:].unsqueeze(1).to_broadcast([128, CAP // 128, D]), f"zf{(l_ * NTILE + e_) % 8}", reads=[R_zt], writes=[R_hs0[l_][e_]])

        qT_d = nc.dram_tensor("qT_d", [128, 8, NEXT], BF16).ap()
        R_qTd = [Reg() for _ in BLKS]

        with contextlib.ExitStack() as sa:
            c2col = sb("c2col", [128, 8, 2], st=sa)
            R_c2 = Reg()
            for r in range(2):
                P.dma("sp", c2col[:, :, r], col(c2[r]), "c0", writes=[R_c2], **NC_KW)
            sc2 = sb("sc2", [128, 8, 2], st=sa)
            R_sc2 = Reg()
            P.op("act", actf(sc2[:], c2col[:], AF.Silu), reads=[R_c2], writes=[R_sc2])
            adab = sb("adab", [2, 2, 6 * D], st=sa)
            R_adab = Reg()
            for l in range(2):
                for r in range(2):
                    P.dma("sp", adab[r:r + 1, l, :], ada_b[l].rearrange("(o n) -> o n", o=1), "c0", writes=[R_adab])
            mrow = sb("mrow", [2, 2, 6 * D], st=sa)
            awr = Ring([sb(f"aw{i}", [128, 8, 512], st=sa) for i in range(2)])
            psA = Ring([ps(f"psA{i}", [128, 512], st=sa) for i in range(2)])
            for l in range(2):
                R_m = Reg()
                for nb in range(12):
                    wbuf, wreg = awr.next()
                    P.dma("sp", wbuf[:], ada_w[l][:, nb * 512:(nb + 1) * 512].rearrange("(k p) n -> p k n", p=128),
                          f"aw{(awr.i - 1) % 2}", writes=[wreg])
                    pb, preg = psA.next()
                    P.op("pe", [mm(pb[0:2, :], sc2[:, k, :], wbuf[:, k, :], k == 0, k == 7) for k in range(8)],
                         reads=[R_sc2, wreg], writes=[preg])
                    P.op("dve", tt(mrow[0:2, l, nb * 512:(nb + 1) * 512], pb[0:2, :], adab[0:2, l, nb * 512:(nb + 1) * 512], ALU.add),
                         reads=[preg, R_adab], writes=[R_m])
                P.dma("sp", mvec[l], mrow[0:2, l, :], "c1", reads=[R_m], writes=[R_mvec[l]])

        def mv(l, r, j):
            return mvec[l, r, j * D:(j + 1) * D]

        vecs = sb("vecs", [128, 16, 8])
        R_vecs = Reg()

        def load_cols(idx, ap1d, dep_regs):
            P.dma("sp", vecs[:, idx, :], col(ap1d), "c0", reads=dep_regs, writes=[R_vecs], **NC_KW)

        load_cols(0, norm_mix[0], [])
        load_cols(1, mv(0, 0, 1), [R_mvec[0]])
        load_cols(2, mv(0, 0, 0), [R_mvec[0]])
        load_cols(3, mv(0, 1, 1), [R_mvec[0]])
        load_cols(4, mv(0, 1, 0), [R_mvec[0]])
        load_cols(5, norm_ffn[0], [])
        load_cols(6, mv(0, 0, 4), [R_mvec[0]])
        load_cols(7, mv(0, 0, 3), [R_mvec[0]])
        load_cols(8, norm_ffn[1], [])
        load_cols(9, mv(1, 0, 4), [R_mvec[1]])
        load_cols(10, mv(1, 0, 3), [R_mvec[1]])
        AB = sb("AB", [128, 4, 8])
        R_AB = Reg()
        for i, (scx, nmx) in enumerate([(1, 0), (3, 0), (6, 5), (9, 8)]):
            P.op("dve", stt(AB[:, i, :], vecs[:, scx, :], 1.0, vecs[:, nmx, :], ALU.add, ALU.mult),
                 reads=[R_vecs], writes=[R_AB])

        if dbg is not None and dbg[0] == "A":
            P.dma("sp", dbg_out[:, 0:32].rearrange("p (i k) -> p i k", k=8), AB[:], "dbg", reads=[R_AB])
            final_waits.append(P.dma("sp", dbg_out[:, 32:160].rearrange("p (i k) -> p i k", k=8), vecs[:], "dbg", reads=[R_vecs]))
            P.wait("sp", final_waits)
            P.emit()
            return nc


        with contextlib.ExitStack() as sattn:
            KT = sb("KT", [128, 2, S + CTX], BF16, st=sattn)
            Vs = sb("Vs", [128, NKT, 256], BF16, st=sattn)
            R_KT = [[Reg() for _ in range(33)] for _ in range(2)]
            R_V = [Reg() for _ in range(NKT)]
            R_Wl, R_Wc = Reg(), Reg()
            bcol = sb("bcol", [128, 12], st=sattn)
            bvb = sb("bvb", [128, 2, 256], st=sattn)
            gcol = sb("gcol", [128, 2], st=sattn)
            negB = sb("negB", [128, 1], st=sattn)
            sw = contextlib.ExitStack()
            Wkv = sb("Wkv", [128, 8, 512], BF16, st=sw)
            Wc = sb("Wc", [128, 8, 512], BF16, st=sw)
            sq_ = contextlib.ExitStack()
            Wq = sb("Wq", [128, 8, 1024], BF16, st=sq_)
            R_bcol, R_bvb, R_gcol, R_negB = Reg(), Reg(), Reg(), Reg()

            with contextlib.ExitStack() as sbp:
                wst_ring = Ring([sb(f"wst{i}", [128, 8, 512], st=sbp) for i in range(1)])
                Bc2 = sb("Bc2", [128, 8, 2], st=sbp)
                R_Bc2 = Reg()
                P.op("dve", cp(Bc2[:, :, 0], vecs[:, 2, :]), reads=[R_vecs], writes=[R_Bc2])
                P.op("dve", cp(Bc2[:, :, 1], vecs[:, 4, :]), reads=[R_vecs], writes=[R_Bc2])
                brow = sb("brow", [2, 1536], st=sbp)
                R_brow = Reg()
                psB = ps("psB", [128, 512], st=sbp)
                R_psB = Reg()
                for nb in range(3):
                    wst, R_wst = wst_ring.next()
                    P.dma("sp", wst[:], wqkv[:, nb * 512:(nb + 1) * 512].rearrange("(k p) n -> p k n", p=128), "wst0", writes=[R_wst])
                    P.op("pe", [mm(psB[0:2, :], Bc2[:, k, :], wst[:, k, :], k == 0, k == 7) for k in range(8)],
                         reads=[R_Bc2, R_wst], writes=[R_psB])
                    P.op("dve", cp(brow[0:2, nb * 512:(nb + 1) * 512], psB[0:2, :]), reads=[R_psB], writes=[R_brow])
                    for k in range(8):
                        P.op("dve", ts(Wq[:, k, nb * 512:(nb + 1) * 512] if nb < 2 else Wkv[:, k, :], wst[:, k, :], AB[:, 0, k:k + 1], None, ALU.mult), reads=[R_wst, R_AB], writes=[R_Wl])
                        if nb == 2:
                            P.op("dve", ts(Wc[:, k, :], wst[:, k, :], AB[:, 1, k:k + 1], None, ALU.mult), reads=[R_wst, R_AB], writes=[R_Wc])
                P.dma("sp", bvec, brow[:], "c1", reads=[R_brow], writes=[R_bvec])
                P.dma("sp", bcol[:, 0:10], col(bvec[0, 0:1280], 10), "c0", reads=[R_bvec], writes=[R_bcol], **NC_KW)
                P.dma("sp", bcol[:, 10:12], col(bvec[1, 1024:1280], 2), "c0", reads=[R_bvec], writes=[R_bcol], **NC_KW)
                P.dma("sp", bvb[:, 0, :], bc(bvec[0, 1280:1536]), "c0", reads=[R_bvec], writes=[R_bvb])
                P.dma("sp", bvb[:, 1, :], bc(bvec[1, 1280:1536]), "c0", reads=[R_bvec], writes=[R_bvb])
                P.dma("sp", gcol[:], qk_g.rearrange("r p -> p r"), "c0", writes=[R_gcol], **NC_KW)
                gb = sb("gb", [128, 2, 128], st=sbp)
                R_gb = Reg()
                for r in range(2):
                    P.dma("sp", gb[:, r, :], bc(qk_g[r]), "c0", writes=[R_gb])
                gb2 = sb("gb2", [128, 2, 128], st=sbp)
                P.op("dve", stt(gb2[:].rearrange("p a b -> p (a b)"), gb[:].rearrange("p a b -> p (a b)"), -1.0, gb[:].rearrange("p a b -> p (a b)"), ALU.mult, ALU.max), reads=[R_gb], writes=[R_gb])
                mx = sb("mx", [128, 2], st=sbp)
                R_mx = Reg()
                for r in range(2):
                    P.op("dve", lambda e, r=r: e.reduce_max(mx[:, r:r + 1], gb2[:, r, :], mybir.AxisListType.X), reads=[R_gb], writes=[R_mx])
                P.op("dve", stt(negB[:], mx[:, 0:1], -math.sqrt(128.0), mx[:, 1:2], ALU.mult, ALU.mult), reads=[R_mx], writes=[R_negB])
                phaseB_done = [R_Wl.w, R_Wc.w, R_negB.w, R_bvec.w]

            if dbg is not None and dbg[0] == "B":
                t = P.dma("sp", dbg_out[:, 0:12], bcol[:], "dbg", reads=[R_bcol])
                t2 = P.dma("sp", dbg_out[:, 12:13], negB[:], "dbg", reads=[R_negB], **NC_KW)
                t3 = P.dma("sp", dbg_out[:, 16:528], bvb[:].rearrange("p a b -> p (a b)"), "dbg", reads=[R_bvb])
                P.wait("sp", [t, t2, t3])
                P.emit()
                return nc

            for phase_ in ("D", "C"):
                deep = phase_ == "C"
                with contextlib.ExitStack() as scd:
                    xs_ring = Ring([sb(f"xs{phase_}{i}", [128, D], st=scd) for i in range(3 if deep else 2)])
                    xn_ring = Ring([sb(f"xn{phase_}{i}", [128, D], BF16, st=scd) for i in range(2)])
                    st_ring = Ring([sb(f"st{phase_}{i}", [128, 2], st=scd) for i in range(4 if deep else 3)])
                    pT_ring = Ring([ps(f"pT{phase_}{i}", [128, 8, 128], BF16, st=scd) for i in range(2)])
                    xnT_ring = Ring([sb(f"xnT{phase_}{i}", [128, 8, 512], BF16, st=scd) for i in range(2 if deep else 1)])
                    cs_ring = Ring([sb(f"cs{phase_}{i}", [128, 2, 512], st=scd) for i in range(1 if deep else 1)])
                    psK_ring = Ring([ps(f"psK{phase_}{i}", [128, 512], st=scd) for i in range(2)])
                    psVb = [ps(f"psV{phase_}{i}", [128, 512], st=scd) for i in range(2)]
                    R_psV = [Reg(), Reg()]
                    psM = ps("psM" + phase_, [128, 512], st=scd)
                    psR = ps("psR" + phase_, [128, 512], st=scd)
                    R_psM, R_psR = Reg(), Reg()
                    kb_ring = Ring([sb(f"kb{phase_}{i}", [128, 512], st=scd) for i in range(2 if deep else 1)])
                    sq_ring = Ring([sb(f"sq{phase_}{i}", [128, 512], BF16, st=scd) for i in range(1 if deep else 1)])
                    rk_ring = Ring([sb(f"rk{phase_}{i}", [128, 512], st=scd) for i in range(2 if deep else 1)])
                    kn_ring = Ring([sb(f"kn{phase_}{i}", [128, 512], BF16, st=scd) for i in range(1 if deep else 1)])
                    t1_ring = Ring([sb(f"t1{phase_}{i}", [128, 512], st=scd) for i in range(1 if deep else 1)])
                    t2_ring = Ring([sb(f"t2{phase_}{i}", [128, 512], st=scd) for i in range(1 if deep else 1)])
                    xs_cnt = [0]

                    def norm_transpose_tile(src_rows, dst, R_dst, first_extra=()):
                        xs, R_xs = xs_ring.next()
                        P.dma("sp", xs[:], src_rows, f"xs{xs_cnt[0] % 3}", writes=[R_xs], extra=first_extra)
                        xs_cnt[0] += 1
                        xn, R_xn = xn_ring.next()
                        stt_, R_st = st_ring.next()
                        P.op("act", actf(xn[:], xs[:], AF.Square, accum=stt_[:, 0:1]), reads=[R_xs], writes=[R_xn, R_st])
                        P.op("act", actf(stt_[:, 1:2], stt_[:, 0:1], AF.Ln, bias=epsc[:, 0:1], scale=1.0 / D), reads=[R_st, R_eps], writes=[R_st])
                        P.op("act", actf(stt_[:, 1:2], stt_[:, 1:2], AF.Exp, scale=-0.5), reads=[R_st], writes=[R_st])
                        P.op("act", actf(xn[:], xs[:], AF.Copy, scale=stt_[:, 1:2]), reads=[R_xs, R_st], writes=[R_xn])
                        pT, R_pT = pT_ring.next()
                        P.op("pe", [tp(pT[:, k, :], xn[:, k * 128:(k + 1) * 128], identb) for k in range(8)],
                             reads=[R_xn, R_cstb], writes=[R_pT])
                        P.op("dve", cp(dst, pT[:]), reads=[R_pT], writes=[R_dst])

                    def qk_post(psX, R_psX, n, bias_ap, g_ap, cs, R_cs, out_ap, R_out):
                        kb, R_kb = kb_ring.next()
                        sq, R_sq = sq_ring.next()
                        P.op("act", actf(kb[:, :n], psX[:, :n], AF.Identity, bias=bias_ap), reads=[R_psX, R_bcol], writes=[R_kb])
                        P.op("act", actf(sq[:, :n], psX[:, :n], AF.Square, bias=bias_ap), reads=[R_psX, R_bcol], writes=[R_sq])
                        P.op("pe", mm(psM[:, :n], onesmb, sq[:, :n]), reads=[R_sq, R_cstb], writes=[R_psM])
                        rk, R_rk = rk_ring.next()
                        P.op("act", actf(rk[:, :n], psM[:, :n], AF.Ln, bias=epsc[:, 0:1]), reads=[R_psM, R_eps], writes=[R_rk])
                        P.op("act", actf(rk[:, :n], rk[:, :n], AF.Exp, scale=-0.5), reads=[R_rk], writes=[R_rk])
                        if cs is None:
                            P.op("dve", stt(out_ap, kb[:, :n], g_ap, rk[:, :n], ALU.mult, ALU.mult), reads=[R_kb, R_rk, R_gcol], writes=[R_out])
                            return
                        kn, R_kn = kn_ring.next()
                        P.op("dve", stt(kn[:, :n], kb[:, :n], g_ap, rk[:, :n], ALU.mult, ALU.mult), reads=[R_kb, R_rk, R_gcol], writes=[R_kn])
                        P.op("pe", mm(psR[:, :n], rotb, kn[:, :n]), reads=[R_kn, R_cstb], writes=[R_psR])
                        t1, R_t1 = t1_ring.next()
                        t2, R_t2 = t2_ring.next()
                        P.op("pool", tt(t1[:, :n], kn[:, :n], cs[:, 0, :n], ALU.mult), reads=[R_kn, R_cs], writes=[R_t1])
                        P.op("dve", tt(t2[:, :n], psR[:, :n], cs[:, 1, :n], ALU.mult), reads=[R_psR, R_cs], writes=[R_t2])
                        P.op("pool", tt(out_ap, t1[:, :n], t2[:, :n], ALU.add), reads=[R_t1, R_t2], writes=[R_out])

                    if phase_ == "C":
                        nblk_c = 33 if not (dbg and dbg[0] == "Csmall") else 2
                        for b in range(nblk_c):
                            isctx = b == 32
                            ntile = 2 if isctx else 4
                            n = ntile * 128
                            xnT, R_xnT = xnT_ring.next()
                            for j in range(ntile):
                                rows = ctx_in[j * 128:(j + 1) * 128, :] if isctx else x_all[b * 512 + j * 128: b * 512 + (j + 1) * 128, :]
                                norm_transpose_tile(rows, xnT[:, :, j * 128:(j + 1) * 128], R_xnT, first_extra=())
                            Wsrc, R_W, koff, voff = (Wc, R_Wc, 0, 256) if isctx else (Wkv, R_Wl, 0, 256)
                            cs, R_cs = (None, None)
                            if not isctx:
                                cs, R_cs = cs_ring.next()
                                P.dma("sp", cs[:], ropek[:, :, b * 512:(b + 1) * 512].rearrange("c p n -> p c n"), "cs0", writes=[R_cs])
                            for j in range(ntile):
                                kt = b * 4 + j
                                hv = j % 2
                                P.op("pe", [mm(psVb[hv][:, 0:256], xnT[:, k, j * 128:(j + 1) * 128], Wsrc[:, k, voff:voff + 256], k == 0, k == 7) for k in range(8)],
                                     reads=[R_xnT, R_W], writes=[R_psV[hv]])
                                P.op("dve", tt(Vs[:, kt, :], psVb[hv][:, 0:256], bvb[:, 1 if isctx else 0, :], ALU.add), reads=[R_psV[hv], R_bvb], writes=[R_V[kt]])
                            for g in range(2):
                                psK, R_psK = psK_ring.next()
                                P.op("pe", [mm(psK[:, :n], Wsrc[:, k, koff + g * 128: koff + (g + 1) * 128], xnT[:, k, :n], k == 0, k == 7) for k in range(8)],
                                     reads=[R_xnT, R_W], writes=[R_psK])
                                bidx = (10 if isctx else 8) + g
                                qk_post(psK, R_psK, n, bcol[:, bidx:bidx + 1], gcol[:, 1:2], cs, R_cs,
                                        KT[:, g, b * 512: b * 512 + n], R_KT[g][b])

                    else:
                        qst_ring = Ring([sb(f"qst{i}", [128, 512], BF16, st=scd) for i in range(1)])
                        for bi, (b0, n) in enumerate(BLKS):
                            ntile = max(1, n // 128)
                            xnT, R_xnT = xnT_ring.next()
                            for j in range(ntile):
                                norm_transpose_tile(x_ext[b0 + j * 128: b0 + (j + 1) * 128, :], xnT[:, :, j * 128:(j + 1) * 128], R_xnT, first_extra=phaseB_done if (bi == 0 and j == 0) else ())
                            cs, R_cs = cs_ring.next()
                            P.dma("sp", cs[:, :, :n], ropeq[:, :, b0:b0 + n].rearrange("c p n -> p c n"), "cs0", writes=[R_cs], **NC_KW)
                            for h in range(8):
                                psK, R_psK = psK_ring.next()
                                P.op("pe", [mm(psK[:, :n], Wq[:, k, h * 128:(h + 1) * 128], xnT[:, k, :n], k == 0, k == 7) for k in range(8)],
                                     reads=[R_xnT, R_Wl], writes=[R_psK])
                                qst, R_qst = qst_ring.next()
                                qk_post(psK, R_psK, n, bcol[:, h:h + 1], gcol[:, 0:1], cs, R_cs, qst[:, :n], R_qst)
                                P.dma("sp", qT_d[:, h, b0:b0 + n], qst[:, :n], "qst0", reads=[R_qst], writes=[R_qTd[bi]], **NC_KW)
                    ph_done = [("E" + e_, P.cnt[e_]) for e_ in ("pe", "act", "dve", "pool")] + [("D" + k_, v_) for k_, v_ in P.dma_sems.items()]
                    for e_ in ENGS:
                        P.wait(e_, ph_done)
                if phase_ == "D":
                    sq_.close()
            cd_done = [P.op("dve", cp(dmy[:, 0:1], epsc[:, 0:1]), writes=[R_dmy], reads=[R_eps] + R_V + [r for rr in R_KT for r in rr] + R_qTd)]
            sw.close()

            with contextlib.ExitStack() as se:
                g0b = sb("g0b", [128, D], st=se)
                R_g0b = Reg()
                P.dma("sp", g0b[:], bc(mv(0, 0, 2)), "c0", reads=[R_mvec[0]], writes=[R_g0b], extra=cd_done)
                Wob = sb("Wob", [128, 8, D], BF16, st=se)
                R_Wob = Reg()
                wos_ring = Ring([sb(f"wos{i}", [128, D], st=se) for i in range(1)])
                for k in range(8):
                    wos, R_wos = wos_ring.next()
                    P.dma("sp", wos[:], wo[k * 128:(k + 1) * 128, :], "wos0", writes=[R_wos], extra=cd_done)
                    P.op("dve", tt(Wob[:, k, :], wos[:], g0b[:], ALU.mult), reads=[R_wos, R_g0b], writes=[R_Wob])
                QT_ring = Ring([sb(f"QTb{i}", [128, 8, 512], BF16, st=se) for i in range(2)])
                PT_ring = Ring([sb(f"PT{i}", [128, 512], BF16, st=se) for i in range(4)])
                attnT = sb("attnT", [128, 8, 512], BF16, st=se)
                R_attnT = Reg()
                rc = sb("rc", [128, 512], st=se)
                R_rc = Reg()
                xt_ring = Ring([sb(f"xt{i}", [128, D], st=se) for i in range(1)])
                x1_ring = Ring([sb(f"x1{i}", [128, D], st=se) for i in range(2)])
                psS_ring = Ring([ps(f"psS{i}", [128, 512], st=se) for i in range(4)])
                pO = [ps(f"pO{i}", [128, 512], st=se) for i in range(2)]
                pD = [ps(f"pD{i}", [128, 512], st=se) for i in range(2)]
                R_pO = [Reg(), Reg()]
                LA = 2
                for bi, (b0, n) in enumerate(BLKS):
                    QTb, R_QTb = QT_ring.next()
                    P.dma("sp", QTb[:, :, :n], qT_d[:, :, b0:b0 + n], f"qtb{bi % 2}", reads=[R_qTd[bi]], writes=[R_QTb], **NC_KW)
                    for hp in range(4):
                        g = hp // 2
                        units = [(kt, hh) for kt in range(NKT) for hh in range(2)]
                        pend = []
                        for i in range(len(units) + LA):
                            if i < len(units):
                                kt, hh = units[i]
                                h = hp * 2 + hh
                                pS, R_pS = psS_ring.next()
                                P.op("pe", mm(pS[:, :n], KT[:, g, kt * 128:(kt + 1) * 128], QTb[:, h, :n]),
                                     reads=[R_KT[g][kt // 4], R_QTb], writes=[R_pS])
                                PT, R_PT = PT_ring.next()
                                P.op("act", actf(PT[:, :n], pS[:, :n], AF.Exp, bias=negB[:, 0:1], scale=1.0 / math.sqrt(128.0)),
                                     reads=[R_pS, R_negB], writes=[R_PT])
                                pend.append((kt, hh, PT, R_PT))
                            if i >= LA:
                                kt, hh, PT, R_PT = pend[i - LA]
                                P.op("pe", [mm(pO[hh][:, :n], Vs[:, kt, g * 128:(g + 1) * 128], PT[:, :n], kt == 0, kt == NKT - 1),
                                            mm(pD[hh][:, :n], onesb, PT[:, :n], kt == 0, kt == NKT - 1)],
                                     reads=[R_PT, R_V[kt], R_cstb], writes=[R_pO[hh]])
                        for hh in range(2):
                            h = hp * 2 + hh
                            P.op("dve", lambda e, hh=hh, n=n: e.reciprocal(rc[:, :n], pD[hh][:, :n]), reads=[R_pO[hh]], writes=[R_rc])
                            P.op("dve", tt(attnT[:, h, :n], pO[hh][:, :n], rc[:, :n], ALU.mult), reads=[R_pO[hh], R_rc], writes=[R_attnT])
                    for j in range(max(1, n // 128)):
                        m = min(128, n)
                        ti = b0 // 128 + j
                        xt, R_xt = xt_ring.next()
                        P.dma("sp", xt[:], x_ext[ti * 128:(ti + 1) * 128, :], "xt0", writes=[R_xt])
                        x1, R_x1 = x1_ring.next()
                        if m < 128:
                            P.op("dve", cp(x1[:], xt[:]), reads=[R_xt], writes=[R_x1])
                        for half in range(2):
                            pY, R_pY = psS_ring.next()
                            P.op("pe", [mm(pY[:m, :], attnT[:, h, j * 128:j * 128 + m], Wob[:, h, half * 512:(half + 1) * 512], h == 0, h == 7) for h in range(8)],
                                 reads=[R_attnT, R_Wob], writes=[R_pY])
                            P.op("dve", tt(x1[:m, half * 512:(half + 1) * 512], pY[:m, :], xt[:m, half * 512:(half + 1) * 512], ALU.add),
                                 reads=[R_pY, R_xt], writes=[R_x1])
                        P.dma("sp", xspill[ti * 128:(ti + 1) * 128, :], x1[:], f"x1{(x1_ring.i - 1) % 2}", reads=[R_x1], writes=[R_xspill])
                attn_done = [P.op("dve", cp(dmy[:, 0:1], epsc[:, 0:1]), writes=[R_dmy], reads=[R_eps, R_attnT, R_rc] + x1_ring.regs + xt_ring.regs + PT_ring.regs)]
                attn_done.append(R_xspill.w)
                attn_done.append(("Epe", P.cnt["pe"]))
                attn_done.append(("Eact", P.cnt["act"]))

        XB = sb("XB", [128, NT, D])
        R_XB = [Reg() for _ in range(NT)]
        for t in range(NT):
            P.dma("sp", XB[:, t, :], xspill[t * 128:(t + 1) * 128, :], f"xb{t % 4}", reads=[R_xspill], writes=[R_XB[t]], extra=attn_done)

        C7 = (LIM * ALPHA) / (1.0 + math.exp(-LIM * ALPHA))

        def rstd_tile(src_ap, junk, R_junk, stt_, R_st, rd):
            P.op("act", actf(junk, src_ap, AF.Square, accum=stt_[:, 0:1]), reads=rd, writes=[R_junk, R_st])
            P.op("act", actf(stt_[:, 1:2], stt_[:, 0:1], AF.Ln, bias=epsc[:, 0:1], scale=1.0 / D), reads=[R_st, R_eps], writes=[R_st])
            P.op("act", actf(stt_[:, 1:2], stt_[:, 1:2], AF.Exp, scale=-0.5), reads=[R_st], writes=[R_st])

        def moe(l):
            with contextlib.ExitStack() as sm:
                hfT = sb(f"hfT{l}", [128, 8, NEXT], BF16, st=sm)
                R_hf = [Reg() for _ in BLKS]
                Gt = sb(f"G{l}", [128, NT, E], st=sm)
                R_G = [Reg() for _ in range(NT)]
                GT = sb(f"GT{l}", [E, NEXT], st=sm)
                R_GT = [Reg() for _ in range(NT)]
                bd_sb = sb(f"bd{l}", [E, D], st=sm)
                R_bd = Reg()
                P.dma("sp", bd_sb[:], b_down[l], "c0", writes=[R_bd])
                bguc = sb(f"bguc{l}", [128, 16, E], st=sm)
                bgs = sb(f"bgs{l}", [128, 8, E], st=sm)
                R_bguc = Reg()
                ifirst = vecs[:, 7 if l == 0 else 10, :]
                Afc = AB[:, 2 + l, :]
                with contextlib.ExitStack() as s1:
                    bgr = sb(f"bgr{l}", [E, 2 * D], st=s1)
                    R_bgr = Reg()
                    P.dma("sp", bgr[:], b_gu[l], "c0", writes=[R_bgr])
                    psT = ps(f"psT{l}", [128, 16, E], st=s1)
                    R_psT = Reg()
                    P.op("pe", [tp(psT[:, c, :], bgr[0:E, c * 128:(c + 1) * 128], identf[0:E, 0:E]) for c in range(16)], reads=[R_bgr, R_cst], writes=[R_psT])
                    P.op("dve", cp(bguc[:], psT[:]), reads=[R_psT], writes=[R_bguc])
                    P.op("dve", ts(bgs[:].rearrange("p a b -> p (a b)"), bguc[:, 0:8, :].rearrange("p a b -> p (a b)"), ALPHA, None, ALU.mult), reads=[R_bguc], writes=[R_bguc])
                    wr = sb(f"wr{l}", [128, 8, E], st=s1)
                    R_wr = Reg()
                    P.dma("sp", wr[:], router_w[l].rearrange("(k p) e -> p k e", p=128), "c0", writes=[R_wr])
                    rbb = sb(f"rbb{l}", [128, E], st=s1)
                    R_rbb = Reg()
                    P.dma("sp", rbb[:], bc(router_b[l]), "c0", writes=[R_rbb])
                    xnf_ring = Ring([sb(f"xnf{l}{i}", [128, D], st=s1) for i in range(2)])
                    st_ring = Ring([sb(f"mst{l}{i}", [128, 2], st=s1) for i in range(2)])
                    h32_ring = Ring([sb(f"h32{l}{i}", [128, 8, 128], st=s1) for i in range(2)])
                    p32_ring = Ring([ps(f"p32{l}{i}", [128, 8, 128], st=s1) for i in range(2)])
                    psL = ps(f"psL{l}", [128, 512], st=s1)
                    R_psL = Reg()
                    psG2 = ps(f"psG2{l}", [128, 512], st=s1)
                    R_psG2 = Reg()
                    sm_ring = Ring([sb(f"smx{l}{i}", [128, 4, E], st=s1) for i in range(2)])
                    s8_ring = Ring([sb(f"s8{l}{i}", [128, 12], st=s1) for i in range(2)])
                    for t in range(NT):
                        xnf, R_xnf = xnf_ring.next()
                        stt_, R_st = st_ring.next()
                        rstd_tile(XB[:, t, :], xnf[:], R_xnf, stt_, R_st, [R_XB[t]])
                        P.op("act", actf(xnf[:], XB[:, t, :], AF.Copy, scale=stt_[:, 1:2]), reads=[R_XB[t], R_st], writes=[R_xnf])
                        p32, R_p32 = p32_ring.next()
                        P.op("pe", [tp(p32[:, k, :], xnf[:, k * 128:(k + 1) * 128], identf) for k in range(8)], reads=[R_xnf, R_cst], writes=[R_p32])
                        h32, R_h32 = h32_ring.next()
                        for k in range(8):
                            P.op("dve", ts(h32[:, k, :], p32[:, k, :], Afc[:, k:k + 1], ifirst[:, k:k + 1], ALU.mult, ALU.add), reads=[R_p32, R_AB, R_vecs], writes=[R_h32])
                        bi = min(t // 4, 4)
                        P.op("pool", cp(hfT[:, :, t * 128:(t + 1) * 128], h32[:]), reads=[R_h32], writes=[R_hf[bi]])
                        P.op("pe", [mm(psL[:, 0:E], h32[:, k, :], wr[:, k, :], k == 0, k == 7) for k in range(8)], reads=[R_h32, R_wr], writes=[R_psL])
                        sx, R_sx = sm_ring.next()
                        s8, R_s8 = s8_ring.next()
                        P.op("dve", tt(sx[:, 0, :], psL[:, 0:E], rbb[:], ALU.add), reads=[R_psL, R_rbb], writes=[R_sx])
                        P.op("dve", lambda e, s8=s8, sx=sx: e.max(s8[:, 0:8], sx[:, 0, :]), reads=[R_sx], writes=[R_s8])
                        P.op("dve", ts(s8[:, 8:9], s8[:, 0:1], -1.0, None, ALU.mult), reads=[R_s8], writes=[R_s8])
                        P.op("act", actf(sx[:, 1, :], sx[:, 0, :], AF.Exp, bias=s8[:, 8:9]), reads=[R_sx, R_s8], writes=[R_sx])
                        P.op("dve", ts(sx[:, 2, :], sx[:, 0, :], s8[:, 3:4], None, ALU.is_ge), reads=[R_sx, R_s8], writes=[R_sx])
                        P.op("dve", stt(sx[:, 3, :], sx[:, 1, :], 1.0, sx[:, 2, :], ALU.mult, ALU.mult, accum=s8[:, 9:10]), reads=[R_sx], writes=[R_sx, R_s8])
                        P.op("dve", lambda e, s8=s8: e.reciprocal(s8[:, 10:11], s8[:, 9:10]), reads=[R_s8], writes=[R_s8])
                        P.op("dve", ts(Gt[:, t, :], sx[:, 3, :], s8[:, 10:11], None, ALU.mult), reads=[R_sx, R_s8], writes=[R_G[t]])
                        P.op("pe", tp(psG2[0:E, 0:128], Gt[:, t, :], identf), reads=[R_G[t], R_cst], writes=[R_psG2])
                        P.op("dve", cp(GT[0:E, t * 128:(t + 1) * 128], psG2[0:E, 0:128]), reads=[R_psG2], writes=[R_GT[t]])
                    g1_done = [P.op("dve", cp(dmy[:, 0:1], epsc[:, 0:1]), writes=[R_dmy], reads=[R_eps] + R_hf + R_GT + R_XB + [R_bguc]),
                               ("Epe", P.cnt["pe"]), ("Eact", P.cnt["act"]), ("Epool", P.cnt["pool"])]
                with contextlib.ExitStack() as s3:
                    actT = sb(f"actT{l}", [128, 8, NEXT], BF16, st=s3)
                    R_act = [[Reg() for _ in BLKS] for _ in range(8)]
                    wring = Ring([sb(f"wp{l}{i}", [128, 8, 512], BF16, st=s3) for i in range(4)])
                    sS_ring = Ring([sb(f"sS{l}{i}", [128, 512], st=s3) for i in range(2)])
                    uS_ring = Ring([sb(f"uS{l}{i}", [128, 512], st=s3) for i in range(2)])
                    psG_ring = Ring([ps(f"psG{l}{i}", [128, 512], st=s3) for i in range(2)])
                    psU_ring = Ring([ps(f"psU{l}{i}", [128, 512], st=s3) for i in range(2)])
                    psY_ring = Ring([ps(f"psY{l}{i}", [128, 512], st=s3) for i in range(3)])
                    for t in range(NT):
                        for half in range(2):
                            pY, R_pY = psY_ring.next()
                            P.op("pe", mm(pY[:, :], GT[0:E, t * 128:(t + 1) * 128], bd_sb[0:E, half * 512:(half + 1) * 512]),
                                 reads=[R_GT[t], R_bd], writes=[R_pY], extra=g1_done)
                            P.op("act", actf(XB[:, t, half * 512:(half + 1) * 512], pY[:, :], AF.Copy), reads=[R_pY], writes=[R_XB[t]], extra=g1_done)
                    wcnt = [0]

                    def wload(parts):
                        wb, R_wb = wring.next()
                        slot = (wring.i - 1) % 4
                        for dst, src in parts:
                            P.dma("pool", dst(wb), src, f"w{slot}_{wcnt[0] % 2}", writes=[R_wb], extra=g1_done if wcnt[0] < 8 else ())
                            wcnt[0] += 1
                        return wb, R_wb

                    for e in range(E):
                        wge = w_gu[l, e].rearrange("(k p) n -> p k n", p=128)
                        for pi in range(4):
                            wb, R_wb = wload([(lambda b: b[:, :, 0:256], wge[:, :, pi * 256:(pi + 1) * 256]),
                                              (lambda b: b[:, :, 256:512], wge[:, :, D + pi * 256: D + (pi + 1) * 256])])
                            for jj in range(2):
                                fc = pi * 2 + jj
                                for bi, (b0, n) in enumerate(BLKS):
                                    psG, R_psG = psG_ring.next()
                                    psU, R_psU = psU_ring.next()
                                    P.op("pe", [mm(psG[:, :n], wb[:, k, jj * 128:(jj + 1) * 128], hfT[:, k, b0:b0 + n], k == 0, k == 7) for k in range(8)],
                                         reads=[R_wb, R_hf[bi]], writes=[R_psG])
                                    P.op("pe", [mm(psU[:, :n], wb[:, k, 256 + jj * 128:256 + (jj + 1) * 128], hfT[:, k, b0:b0 + n], k == 0, k == 7) for k in range(8)],
                                         reads=[R_wb, R_hf[bi]], writes=[R_psU])
                                    sS, R_sS = sS_ring.next()
                                    uS, R_uS = uS_ring.next()
                                    P.op("act", actf(sS[:, :n], psG[:, :n], AF.Silu, bias=bgs[:, fc, e:e + 1], scale=ALPHA), reads=[R_psG, R_bguc], writes=[R_sS])
                                    P.op("act", actf(uS[:, :n], psU[:, :n], AF.Identity, bias=bguc[:, 8 + fc, e:e + 1]), reads=[R_psU, R_bguc], writes=[R_uS])
                                    P.op("dve", ts(sS[:, :n], sS[:, :n], C7, 1.0 / ALPHA, ALU.min, ALU.mult), reads=[R_sS], writes=[R_sS])
                                    P.op("dve", ts(uS[:, :n], uS[:, :n], LIM, -LIM, ALU.min, ALU.max), reads=[R_uS], writes=[R_uS])
                                    P.op("dve", stt(actT[:, fc, b0:b0 + n], uS[:, :n], 1.0, sS[:, :n], ALU.add, ALU.mult), reads=[R_uS, R_sS], writes=[R_act[fc][bi]])
                        wde = w_down[l, e].rearrange("(k p) n -> p k n", p=128)
                        for half in range(2):
                            wb, R_wb = wload([(lambda b: b[:, :, :], wde[:, :, half * 512:(half + 1) * 512])])
                            for t in range(NT):
                                m = 128 if t < 16 else 16
                                bi = min(t // 4, 4)
                                pY, R_pY = psY_ring.next()
                                P.op("pe", [mm(pY[:m, :], actT[:, fc, t * 128:t * 128 + m], wb[:, fc, :], fc == 0, fc == 7) for fc in range(8)],
                                     reads=[R_wb] + [R_act[fc][bi] for fc in range(8)], writes=[R_pY])
                                P.op("dve", stt(XB[:m, t, half * 512:(half + 1) * 512], pY[:m, :], Gt[:m, t, e:e + 1], XB[:m, t, half * 512:(half + 1) * 512], ALU.mult, ALU.add),
                                     reads=[R_pY, R_G[t]], writes=[R_XB[t]])
                    g3_done = [("Epe", P.cnt["pe"]), ("Eact", P.cnt["act"]), ("Edve", P.cnt["dve"])]
                with contextlib.ExitStack() as s4:
                    gfb = sb(f"gfb{l}", [128, D], st=s4)
                    R_gfb = Reg()
                    P.dma("sp", gfb[:], bc(mv(l, 0, 5)), "c0", reads=[R_mvec[l]], writes=[R_gfb], extra=g3_done)
                    xt_ring = Ring([sb(f"mxt{l}{i}", [128, D], st=s4) for i in range(2)])
                    for t in range(NT):
                        xt, R_xt = xt_ring.next()
                        P.dma("sp", xt[:], xspill[t * 128:(t + 1) * 128, :], f"mxt{t % 2}", reads=[R_xspill], writes=[R_xt], extra=g3_done)
                        P.op("dve", tt(XB[:, t, :], XB[:, t, :], gfb[:], ALU.mult), reads=[R_XB[t], R_gfb], writes=[R_XB[t]])
                        P.op("dve", tt(XB[:, t, :], XB[:, t, :], xt[:], ALU.add), reads=[R_XB[t], R_xt], writes=[R_XB[t]])
                    return [P.op("dve", cp(dmy[:, 0:1], epsc[:, 0:1]), writes=[R_dmy], reads=[R_eps] + R_XB + xt_ring.regs),
                            ("Epe", P.cnt["pe"]), ("Eact", P.cnt["act"]), ("Edve", P.cnt["dve"])]


        I32 = mybir.dt.int32
        U32 = mybir.dt.uint32

        _bnd = {}

        def bnd_reg(engine, which="r"):
            if which not in _bnd:
                _bnd[which] = engine.to_reg(NSLOT - 1 if which == "r" else (int(which[1:]) + 1) * E * D - 1)
            return _bnd[which]

        def moe_sparse(l):
            hs, ys = hs_d[l], ys_d[l]
            wgu_rows = w_gu.rearrange("l e r n -> (l e r) n")
            wdn_rows = w_down.rearrange("l e r n -> (l e r) n")
            with contextlib.ExitStack() as sm:
                Gt = sb(f"G{l}", [128, NT, E], st=sm)
                R_G = [Reg() for _ in range(NT)]
                GT = sb(f"GT{l}", [E, NEXT], st=sm)
                R_GT = [Reg() for _ in range(NT)]
                bd_sb = sb(f"bd{l}", [E, D], st=sm)
                R_bd = Reg()
                P.dma("sp", bd_sb[:], b_down[l], "c0", writes=[R_bd])
                SLOT = sb(f"SLOT{l}", [128, NT, 4], I32, st=sm)
                GK = sb(f"GK{l}", [128, NT, 4], st=sm)
                R_SLOT = [Reg() for _ in range(NT)]
                R_GK = [Reg() for _ in range(NT)]
                R_hs = [[Reg() for _ in range(4)] for _ in range(NT)]
                IDXW = sb(f"IDXW{l}", [128, NTILE, 8], I32, st=sm)
                bsel = sb(f"bsel{l}", [128, NTILE, 16], st=sm)
                bgsel = sb(f"bgsel{l}", [128, NTILE, 8], st=sm)
                R_IDXW, R_bsel = Reg(), Reg()
                ifirst = vecs[:, 7 if l == 0 else 10, :]
                Afc = AB[:, 2 + l, :]
                with contextlib.ExitStack() as s1:
                    bguc = sb(f"bguc{l}", [128, 16, E], st=s1)
                    R_bguc = Reg()
                    bgr = sb(f"bgr{l}", [E, 2 * D], st=s1)
                    R_bgr = Reg()
                    P.dma("sp", bgr[:], b_gu[l], "c0", writes=[R_bgr])
                    psT = ps(f"psT{l}", [128, 16, E], st=s1)
                    R_psT = Reg()
                    P.op("pe", [tp(psT[:, c, :], bgr[0:E, c * 128:(c + 1) * 128], identf[0:E, 0:E]) for c in range(16)], reads=[R_bgr, R_cst], writes=[R_psT])
                    P.op("dve", cp(bguc[:], psT[:]), reads=[R_psT], writes=[R_bguc])
                    wr = sb(f"wr{l}", [128, 8, E], st=s1)
                    R_wr = Reg()
                    P.dma("sp", wr[:], router_w[l].rearrange("(k p) e -> p k e", p=128), "c0", writes=[R_wr])
                    rbb = sb(f"rbb{l}", [128, E], st=s1)
                    R_rbb = Reg()
                    P.dma("sp", rbb[:], bc(router_b[l]), "c0", writes=[R_rbb])
                    Afr = sb(f"Afr{l}", [128, D], st=s1)
                    Bfr = sb(f"Bfr{l}", [128, D], st=s1)
                    tmr = sb(f"tmr{l}", [128, D], st=s1)
                    R_Afr, R_Bfr, R_tmr = Reg(), Reg(), Reg()
                    P.dma("sp", Afr[:], bc(mv(l, 0, 4)), "c0", reads=[R_mvec[l]], writes=[R_Afr])
                    P.dma("sp", tmr[:], bc(norm_ffn[l]), "c0", writes=[R_tmr])
                    P.op("dve", stt(Afr[:], Afr[:], 1.0, tmr[:], ALU.add, ALU.mult), reads=[R_Afr, R_tmr], writes=[R_Afr])
                    P.dma("sp", Bfr[:], bc(mv(l, 0, 3)), "c0", reads=[R_mvec[l]], writes=[R_Bfr])
                    MSK = sb(f"MSK{l}", [128, NT, E], BF16, st=s1)
                    R_MSK = [Reg() for _ in range(NT)]
                    RK4 = sb(f"RK4{l}", [128, NT, 4], st=s1)
                    E4 = sb(f"E4{l}", [128, NT, 4], st=s1)
                    R_RK4 = [Reg() for _ in range(NT)]
                    HFT = sb(f"HFT{l}", [128, NT, D], BF16, st=s1)
                    R_HFT = [Reg() for _ in range(NT)]
                    xnf_ring = Ring([sb(f"xnf{l}{i}", [128, D], st=s1) for i in range(2)])
                    st_ring = Ring([sb(f"mst{l}{i}", [128, 2], st=s1) for i in range(2)])
                    h32_ring = Ring([sb(f"h32{l}{i}", [128, 8, 128], st=s1) for i in range(2)])
                    p32_ring = Ring([ps(f"p32{l}{i}", [128, 8, 128], st=s1) for i in range(2)])
                    psL = ps(f"psL{l}", [128, 512], st=s1)
                    R_psL = Reg()
                    psG2 = ps(f"psG2{l}", [128, 512], st=s1)
                    R_psG2 = Reg()
                    psRk = ps(f"psRk{l}", [128, 512], st=s1)
                    R_psRk = Reg()
                    sm_ring = Ring([sb(f"smx{l}{i}", [128, 6, E], st=s1) for i in range(2)])
                    s8_ring = Ring([sb(f"s8{l}{i}", [128, 40], st=s1) for i in range(2)])
                    i8_ring = Ring([sb(f"i8{l}{i}", [128, 8], U32, st=s1) for i in range(2)])
                    for t in range(NT):
                        xnf, R_xnf = xnf_ring.next()
                        stt_, R_st = st_ring.next()
                        rstd_tile(XB[:, t, :], xnf[:], R_xnf, stt_, R_st, [R_XB[t]])
                        P.op("act", actf(xnf[:], XB[:, t, :], AF.Copy, scale=stt_[:, 1:2]), reads=[R_XB[t], R_st], writes=[R_xnf])
                        p32, R_p32 = p32_ring.next()
                        P.op("pe", [tp(p32[:, k, :], xnf[:, k * 128:(k + 1) * 128], identf) for k in range(8)], reads=[R_xnf, R_cst], writes=[R_p32])
                        h32, R_h32 = h32_ring.next()
                        for k in range(8):
                            P.op("dve", ts(h32[:, k, :], p32[:, k, :], Afc[:, k:k + 1], ifirst[:, k:k + 1], ALU.mult, ALU.add), reads=[R_p32, R_AB, R_vecs], writes=[R_h32])
                        P.op("pool", tt(tmr[:], xnf[:], Afr[:], ALU.mult), reads=[R_xnf, R_Afr, R_tmr], writes=[R_tmr])
                        P.op("pool", tt(HFT[:, t, :], tmr[:], Bfr[:], ALU.add), reads=[R_tmr, R_Bfr], writes=[R_HFT[t]])
                        P.op("pe", [mm(psL[:, 0:E], h32[:, k, :], wr[:, k, :], k == 0, k == 7) for k in range(8)], reads=[R_h32, R_wr], writes=[R_psL])
                        sx, R_sx = sm_ring.next()
                        s8, R_s8 = s8_ring.next()
                        i8, R_i8 = i8_ring.next()
                        P.op("dve", tt(sx[:, 0, :], psL[:, 0:E], rbb[:], ALU.add), reads=[R_psL, R_rbb], writes=[R_sx])
                        P.op("dve", lambda e, s8=s8, sx=sx: e.max(s8[:, 0:8], sx[:, 0, :]), reads=[R_sx], writes=[R_s8])
                        P.op("dve", lambda e, s8=s8, sx=sx, i8=i8: e.max_index(i8[:, 0:8], s8[:, 0:8], sx[:, 0, :]), reads=[R_sx, R_s8], writes=[R_i8])
                        P.op("dve", ts(s8[:, 8:9], s8[:, 0:1], -1.0, None, ALU.mult), reads=[R_s8], writes=[R_s8])
                        P.op("act", actf(s8[:, 12:16], s8[:, 0:4], AF.Exp, bias=s8[:, 8:9], accum=s8[:, 9:10]), reads=[R_s8], writes=[R_s8])
                        P.op("dve", lambda e, s8=s8: e.reciprocal(s8[:, 10:11], s8[:, 9:10]), reads=[R_s8], writes=[R_s8])
                        P.op("dve", ts(GK[:, t, :], s8[:, 12:16], s8[:, 10:11], None, ALU.mult), reads=[R_s8], writes=[R_GK[t]])
                        P.op("dve", cp(E4[:, t, :], i8[:, 0:4]), reads=[R_i8], writes=[R_RK4[t]])
                        for k in range(4):
                            P.op("dve", ts(sx[:, 2 + k, :], iota_e, E4[:, t, k:k + 1], None, ALU.is_equal), reads=[R_RK4[t], R_cst], writes=[R_sx])
                        P.op("dve", tt(sx[:, 1, :], sx[:, 2, :], sx[:, 3, :], ALU.add), reads=[R_sx], writes=[R_sx])
                        P.op("dve", tt(sx[:, 1, :], sx[:, 1, :], sx[:, 4, :], ALU.add), reads=[R_sx], writes=[R_sx])
                        P.op("dve", tt(sx[:, 1, :], sx[:, 1, :], sx[:, 5, :], ALU.add), reads=[R_sx], writes=[R_sx])
                        if t == 16:
                            P.op("dve", ts(sx[:, 1, :], sx[:, 1, :], vmask, None, ALU.mult), reads=[R_sx, R_cst], writes=[R_sx])
                        P.op("dve", cp(MSK[:, t, :], sx[:, 1, :]), reads=[R_sx], writes=[R_MSK[t]])
                        P.op("dve", ts(Gt[:, t, :], sx[:, 2, :], GK[:, t, 0:1], None, ALU.mult), reads=[R_sx, R_GK[t]], writes=[R_G[t]])
                        for k in range(1, 4):
                            P.op("dve", stt(Gt[:, t, :], sx[:, 2 + k, :], GK[:, t, k:k + 1], Gt[:, t, :], ALU.mult, ALU.add), reads=[R_sx, R_GK[t], R_G[t]], writes=[R_G[t]])
                        P.op("pe", tp(psG2[0:E, 0:128], Gt[:, t, :], identf), reads=[R_G[t], R_cst], writes=[R_psG2])
                        P.op("dve", cp(GT[0:E, t * 128:(t + 1) * 128], psG2[0:E, 0:128]), reads=[R_psG2], writes=[R_GT[t]])
                        fns = [mm(psRk[:, 0:E], onesb, MSK[:, tp_, :], tp_ == 0, False) for tp_ in range(t)]
                        fns.append(mm(psRk[:, 0:E], ustrb, MSK[:, t, :], t == 0, True))
                        P.op("pe", fns, reads=R_MSK[0:t + 1] + [R_cstb], writes=[R_psRk])
                        for k in range(4):
                            P.op("dve", stt(sx[:, 2 + k, :], sx[:, 2 + k, :], 1.0, psRk[:, 0:E], ALU.mult, ALU.mult, accum=RK4[:, t, k:k + 1]),
                                 reads=[R_sx, R_psRk], writes=[R_sx, R_RK4[t]])
                    gl = sb(f"gl{l}", [128, 8, E], st=s1)
                    R_gl = Reg()
                    P.op("pe", [mm(psRk[:, 0:E], onesb, MSK[:, tp_, :], tp_ == 0, tp_ == NT - 1) for tp_ in range(NT)], reads=R_MSK + [R_cstb], writes=[R_psRk])
                    P.op("dve", cp(gl[:, 0, :], psRk[:, 0:E]), reads=[R_psRk], writes=[R_gl])
                    P.op("dve", ts(gl[:, 3, :], gl[:, 0, :], 0.0, None, ALU.is_gt), reads=[R_gl], writes=[R_gl])
                    for m_ in range(1, 5):
                        P.op("dve", stt(gl[:, 3, :], gl[:, 0, :], float(m_ * CAP), gl[:, 3, :], ALU.is_gt, ALU.add), reads=[R_gl], writes=[R_gl])
                    P.op("dve", cp(gl[:, 4, :], gl[:, 3, :]), reads=[R_gl], writes=[R_gl])
                    cur, nxt = 4, 5
                    for sft in (1, 2, 4, 8, 16):
                        P.op("dve", cp(gl[:, nxt, 0:sft], gl[:, cur, 0:sft]), reads=[R_gl], writes=[R_gl])
                        P.op("dve", tt(gl[:, nxt, sft:E], gl[:, cur, sft:E], gl[:, cur, 0:E - sft], ALU.add), reads=[R_gl], writes=[R_gl])
                        cur, nxt = nxt, cur
                    cum = gl[:, cur, :]
                    P.op("dve", tt(gl[:, 6, :], cum, gl[:, 3, :], ALU.subtract), reads=[R_gl], writes=[R_gl])
                    P.op("dve", ts(gl[:, 6, :], gl[:, 6, :], float(CAP), None, ALU.mult), reads=[R_gl], writes=[R_gl])
                    ejt = sb(f"ejt{l}", [128, NTILE], st=s1)
                    R_ej = Reg()
                    for j in range(NTILE):
                        P.op("dve", stt(gl[:, 7, :], cum, float(j), cst[:, 3, 0:E], ALU.is_le, ALU.mult, accum=ejt[:, j:j + 1]), reads=[R_gl, R_cst], writes=[R_gl, R_ej])
                    P.op("dve", ts(ejt[:], ejt[:], float(E - 1), None, ALU.min), reads=[R_ej], writes=[R_ej])
                    idxf = sb(f"idxf{l}", [128, NTILE, 8], st=s1)
                    R_idxf = Reg()
                    for k in range(8):
                        P.op("dve", ts(idxf[:, :, k], ejt[:], float(D), kpc[:, k:k + 1], ALU.mult, ALU.add), reads=[R_ej, R_cst], writes=[R_idxf])
                        if l == 1:
                            P.op("dve", ts(idxf[:, :, k], idxf[:, :, k], float(E * D), None, ALU.add), reads=[R_idxf], writes=[R_idxf])
                    P.op("dve", cp(IDXW[:], idxf[:]), reads=[R_idxf], writes=[R_IDXW])
                    ohj = sb(f"ohj{l}", [128, E], st=s1)
                    tmpb2 = sb(f"tmpb2{l}", [128, 16, E], st=s1)
                    R_ohj, R_tmpb2 = Reg(), Reg()
                    for j in range(NTILE):
                        P.op("dve", ts(ohj[:], iota_e, ejt[:, j:j + 1], None, ALU.is_equal), reads=[R_ej, R_cst], writes=[R_ohj])
                        P.op("dve", tt(tmpb2[:], bguc[:], ohj[:].unsqueeze(1).to_broadcast([128, 16, E]), ALU.mult), reads=[R_ohj, R_bguc], writes=[R_tmpb2])
                        P.op("dve", lambda e_, j=j: e_.reduce_sum(bsel[:, j, :], tmpb2[:], mybir.AxisListType.X), reads=[R_tmpb2], writes=[R_bsel])
                    P.op("dve", ts(bgsel[:].rearrange("p a b -> p (a b)").rearrange("p (a b) -> p a b", b=8), bsel[:, :, 0:8], ALPHA, None, ALU.mult), reads=[R_bsel], writes=[R_bsel])
                    s8b_ring = Ring([sb(f"s8b{l}{i}", [128, 8], st=s1) for i in range(2)])
                    ohk_ring = Ring([sb(f"ohk{l}{i}", [128, E], st=s1) for i in range(2)])
                    for t in range(NT):
                        s8b, R_s8b = s8b_ring.next()
                        for k in range(4):
                            ohk, R_ohk = ohk_ring.next()
                            P.op("dve", ts(ohk[:], iota_e, E4[:, t, k:k + 1], None, ALU.is_equal), reads=[R_RK4[t], R_cst], writes=[R_ohk])
                            P.op("dve", stt(ohk[:], ohk[:], 1.0, gl[:, 6, :], ALU.mult, ALU.mult, accum=s8b[:, k:k + 1]), reads=[R_ohk, R_gl], writes=[R_ohk, R_s8b])
                        P.op("dve", tt(s8b[:, 4:8], s8b[:, 0:4], RK4[:, t, :], ALU.add), reads=[R_s8b, R_RK4[t]], writes=[R_s8b])
                        if t == 16:
                            P.op("dve", ts(s8b[:, 4:8], s8b[:, 4:8], notvbig, None, ALU.add), reads=[R_s8b, R_cst], writes=[R_s8b])
                        P.op("dve", cp(SLOT[:, t, :], s8b[:, 4:8]), reads=[R_s8b], writes=[R_SLOT[t]])
                        for k in range(4):
                            P.dmaf("pool", lambda e, t=t, k=k: e.indirect_dma_start(
                                out=hs[:, :], out_offset=bass.IndirectOffsetOnAxis(ap=SLOT[:, t, k:k + 1], axis=0),
                                in_=HFT[:, t, :], in_offset=None, bounds_check=bnd_reg(e), oob_is_err=False),
                                f"sc{(t * 4 + k) % 8}", reads=[R_HFT[t], R_SLOT[t]], writes=[R_hs[t][k]], extra=[r.w for r in R_hs0[l]])
                    g1_done = [P.op("dve", cp(dmy[:, 0:1], epsc[:, 0:1]), writes=[R_dmy], reads=[R_eps] + R_GT + R_XB + [R_bsel, R_IDXW] + R_SLOT + R_GK),
                               ("Epe", P.cnt["pe"]), ("Eact", P.cnt["act"]), ("Epool", P.cnt["pool"])]
                    all_hs = [r for rr in R_hs for r in rr]
                    for t in range(4):
                        pass
                    g1_done += [r.w for r in all_hs]
                with contextlib.ExitStack() as s3:
                    hg_ring = Ring([sb(f"hg{l}{i}", [128, 4, D], BF16, st=s3) for i in range(1)])
                    hgT_ring = Ring([sb(f"hgT{l}{i}", [128, 8, CAP], BF16, st=s3) for i in range(2)])
                    actT_ring = Ring([sb(f"actT{l}{i}", [128, 8, CAP], BF16, st=s3) for i in range(2)])
                    wgu_ring = Ring([XB[:, 0:8, :].bitcast(BF16), XB[:, 8:16, :].bitcast(BF16)])
                    wdn_ring = Ring([sb(f"wdn{l}{i}", [128, 8, D], BF16, st=s3) for i in range(2)])
                    sS_ring = Ring([sb(f"sS{l}{i}", [128, 512], st=s3) for i in range(2)])
                    uS_ring = Ring([sb(f"uS{l}{i}", [128, 512], st=s3) for i in range(2)])
                    ysb_ring = Ring([sb(f"ysb{l}{i}", [128, D], st=s3) for i in range(4)])
                    pTe_ring = Ring([ps(f"pTe{l}{i}", [128, 8, 128], BF16, st=s3) for i in range(2)])
                    psG_ring = Ring([ps(f"psG{l}{i}", [128, 512], st=s3) for i in range(2)])
                    psU_ring = Ring([ps(f"psU{l}{i}", [128, 512], st=s3) for i in range(2)])
                    psY_ring = Ring([ps(f"psY{l}{i}", [128, 512], st=s3) for i in range(2)])
                    R_ys = [Reg() for _ in range(NTILE * 4)]
                    evi = 0
                    wq = 0
                    R_wguk = [[Reg() for _ in range(8)] for _ in range(2)]
                    R_wdnk = [[Reg() for _ in range(8)] for _ in range(2)]
                    for j in range(NTILE):
                        wgu, _ = wgu_ring.next()
                        wdn, _ = wdn_ring.next()
                        R_wgu = R_wguk[j % 2]
                        R_wdn = R_wdnk[j % 2]
                        for k in range(8):
                            P.dmaf("pool", lambda e_, j=j, k=k, wgu=wgu: e_.indirect_dma_start(
                                out=wgu[:, k, :], out_offset=None, in_=wgu_rows[:, :],
                                in_offset=bass.IndirectOffsetOnAxis(ap=IDXW[:, j, k:k + 1], axis=0), bounds_check=bnd_reg(e_, "w1"), oob_is_err=False),
                                f"wg{wq % 16}", reads=[R_IDXW], writes=[R_wgu[k]], extra=g1_done)
                            wq += 1
                        for k in range(8):
                            P.dmaf("pool", lambda e_, j=j, k=k, wdn=wdn: e_.indirect_dma_start(
                                out=wdn[:, k, :], out_offset=None, in_=wdn_rows[:, :],
                                in_offset=bass.IndirectOffsetOnAxis(ap=IDXW[:, j, k:k + 1], axis=0), bounds_check=bnd_reg(e_, "w1"), oob_is_err=False),
                                f"wg{wq % 16}", reads=[R_IDXW], writes=[R_wdn[k]], extra=g1_done)
                            wq += 1
                        hg, R_hg = hg_ring.bufs[0], hg_ring.regs[0]
                        if j == 0:
                            P.dma("sp", hg[:], hs[0:CAP, :].rearrange("(s p) d -> p s d", p=128), "hg0", reads=all_hs, writes=[R_hg], extra=g1_done)
                        hgT, R_hgT = hgT_ring.next()
                        for s_ in range(4):
                            pTe, R_pTe = pTe_ring.next()
                            P.op("pe", [tp(pTe[:, k, :], hg[:, s_, k * 128:(k + 1) * 128], identb) for k in range(8)], reads=[R_hg, R_cstb], writes=[R_pTe])
                            if s_ % 2 == 0:
                                P.op("dve", cp(hgT[:, :, s_ * 128:(s_ + 1) * 128], pTe[:]), reads=[R_pTe], writes=[R_hgT])
                            else:
                                P.op("act", actf(hgT[:, :, s_ * 128:(s_ + 1) * 128], pTe[:], AF.Copy), reads=[R_pTe], writes=[R_hgT])
                        if j + 1 < NTILE:
                            P.dma("sp", hg[:], hs[(j + 1) * CAP:(j + 2) * CAP, :].rearrange("(s p) d -> p s d", p=128), "hg0", reads=all_hs, writes=[R_hg], extra=g1_done)
                        actT, R_actT = actT_ring.next()
                        for fc in range(8):
                            psG, R_psG = psG_ring.next()
                            psU, R_psU = psU_ring.next()
                            P.op("pe", [mm(psG[:, :], wgu[:, k, fc * 128:(fc + 1) * 128], hgT[:, k, :], k == 0, k == 7) for k in range(8)],
                                 reads=R_wgu + [R_hgT], writes=[R_psG])
                            P.op("pe", [mm(psU[:, :], wgu[:, k, D + fc * 128:D + (fc + 1) * 128], hgT[:, k, :], k == 0, k == 7) for k in range(8)],
                                 reads=R_wgu + [R_hgT], writes=[R_psU])
                            sS, R_sS = sS_ring.next()
                            uS, R_uS = uS_ring.next()
                            P.op("act", actf(sS[:], psG[:], AF.Silu, bias=bgsel[:, j, fc:fc + 1], scale=ALPHA), reads=[R_psG, R_bsel], writes=[R_sS])
                            P.op("act", actf(uS[:], psU[:], AF.Identity, bias=bsel[:, j, 8 + fc:9 + fc]), reads=[R_psU, R_bsel], writes=[R_uS])
                            P.op("dve", ts(sS[:], sS[:], C7, 1.0 / ALPHA, ALU.min, ALU.mult), reads=[R_sS], writes=[R_sS])
                            P.op("dve", ts(uS[:], uS[:], LIM, -LIM, ALU.min, ALU.max), reads=[R_uS], writes=[R_uS])
                            P.op("dve", stt(actT[:, fc, :], uS[:], 1.0, sS[:], ALU.add, ALU.mult), reads=[R_uS, R_sS], writes=[R_actT])
                        for s_ in range(4):
                            ysb, R_ysb = ysb_ring.next()
                            for half in range(2):
                                pY, R_pY = psY_ring.next()
                                P.op("pe", [mm(pY[:, :], actT[:, fc, s_ * 128:(s_ + 1) * 128], wdn[:, fc, half * 512:(half + 1) * 512], fc == 0, fc == 7) for fc in range(8)],
                                     reads=R_wdn + [R_actT], writes=[R_pY])
                                if evi % 2 == 0:
                                    P.op("act", actf(ysb[:, half * 512:(half + 1) * 512], pY[:, :], AF.Copy), reads=[R_pY], writes=[R_ysb])
                                else:
                                    P.op("dve", cp(ysb[:, half * 512:(half + 1) * 512], pY[:, :]), reads=[R_pY], writes=[R_ysb])
                                evi += 1
                            P.dma("sp", ys[j * CAP + s_ * 128: j * CAP + (s_ + 1) * 128, :], ysb[:], f"ysb{(ysb_ring.i - 1) % 4}", reads=[R_ysb], writes=[R_ys[j * 4 + s_]])
                    e_done0 = [("Epe", P.cnt["pe"]), ("Eact", P.cnt["act"]), ("Edve", P.cnt["dve"])] + [r.w for r in R_ys]
                    for t in range(NT):
                        for half in range(2):
                            pY, R_pY = psY_ring.next()
                            P.op("pe", mm(pY[:, :], GT[0:E, t * 128:(t + 1) * 128], bd_sb[0:E, half * 512:(half + 1) * 512]),
                                 reads=[R_GT[t], R_bd], writes=[R_pY], extra=e_done0)
                            P.op("act", actf(XB[:, t, half * 512:(half + 1) * 512], pY[:, :], AF.Copy), reads=[R_pY], writes=[R_XB[t]], extra=e_done0)
                    e_done = [("Epe", P.cnt["pe"]), ("Eact", P.cnt["act"]), ("Edve", P.cnt["dve"])] + [r.w for r in R_ys]
                with contextlib.ExitStack() as s5:
                    yg_ring = Ring([sb(f"yg{l}{i}", [128, D], st=s5) for i in range(4)])
                    for yg, R_yg in zip(yg_ring.bufs, yg_ring.regs):
                        P.op("dve", lambda e, yg=yg: e.memset(yg[:], 0.0), writes=[R_yg], extra=e_done)
                    for t in range(NT):
                        for k in range(4):
                            yg, R_yg = yg_ring.next()
                            P.dmaf("pool", lambda e_, t=t, k=k, yg=yg: e_.indirect_dma_start(
                                out=yg[:, :], out_offset=None, in_=ys[:, :],
                                in_offset=bass.IndirectOffsetOnAxis(ap=SLOT[:, t, k:k + 1], axis=0), bounds_check=bnd_reg(e_), oob_is_err=False),
                                f"yg{(yg_ring.i - 1) % 4}", reads=R_ys + [R_SLOT[t]], writes=[R_yg], extra=e_done)
                            P.op("dve", stt(XB[:, t, :], yg[:], GK[:, t, k:k + 1], XB[:, t, :], ALU.mult, ALU.add), reads=[R_yg, R_GK[t], R_XB[t]], writes=[R_XB[t]])
                    g3_done = [("Epe", P.cnt["pe"]), ("Eact", P.cnt["act"]), ("Edve", P.cnt["dve"]), ("Epool", P.cnt["pool"])] + [r.w for r in yg_ring.regs]
                with contextlib.ExitStack() as s4:
                    gfb = sb(f"gfb{l}", [128, D], st=s4)
                    R_gfb = Reg()
                    P.dma("sp", gfb[:], bc(mv(l, 0, 5)), "c0", reads=[R_mvec[l]], writes=[R_gfb], extra=g3_done)
                    xt_ring = Ring([sb(f"mxt{l}{i}", [128, D], st=s4) for i in range(2)])
                    for t in range(NT):
                        xt, R_xt = xt_ring.next()
                        P.dma("sp", xt[:], xspill[t * 128:(t + 1) * 128, :], f"mxt{t % 2}", reads=[R_xspill], writes=[R_xt], extra=g3_done)
                        P.op("dve", tt(XB[:, t, :], XB[:, t, :], gfb[:], ALU.mult), reads=[R_XB[t], R_gfb], writes=[R_XB[t]])
                        P.op("dve", tt(XB[:, t, :], XB[:, t, :], xt[:], ALU.add), reads=[R_XB[t], R_xt], writes=[R_XB[t]])
                    return [P.op("dve", cp(dmy[:, 0:1], epsc[:, 0:1]), writes=[R_dmy], reads=[R_eps] + R_XB + xt_ring.regs),
                            ("Epe", P.cnt["pe"]), ("Eact", P.cnt["act"]), ("Edve", P.cnt["dve"])]

        moe0_done = (moe_sparse if SPARSE else moe)(0)

        with contextlib.ExitStack() as sh:
            hP = sb("hP", [128, NT, D], BF16, st=sh)
            R_hP = [Reg() for _ in range(NT)]
            bandsb = sb("bandsb", [128, 36, 128], BF16, st=sh)
            R_bands = Reg()
            P.dma("sp", bandsb[:], bands, "c0", writes=[R_bands], extra=moe0_done)
            A1b = sb("A1b", [128, D], st=sh)
            B1b = sb("B1b", [128, D], st=sh)
            tmpb = sb("tmpb", [128, D], st=sh)
            gpb = sb("gpb", [128, D], st=sh)
            R_A1b, R_B1b, R_tmpb, R_gpb = Reg(), Reg(), Reg(), Reg()
            P.dma("sp", A1b[:], bc(mv(1, 0, 1)), "c0", reads=[R_mvec[1]], writes=[R_A1b], extra=moe0_done)
            P.dma("sp", tmpb[:], bc(norm_mix[1]), "c0", writes=[R_tmpb], extra=moe0_done)
            P.op("dve", stt(A1b[:], A1b[:], 1.0, tmpb[:], ALU.add, ALU.mult), reads=[R_A1b, R_tmpb], writes=[R_A1b])
            P.dma("sp", B1b[:], bc(mv(1, 0, 0)), "c0", reads=[R_mvec[1]], writes=[R_B1b], extra=moe0_done)
            P.dma("sp", gpb[:], bc(mv(1, 0, 2)), "c0", reads=[R_mvec[1]], writes=[R_gpb], extra=moe0_done)
            P.dma("sp", tmpb[:], bc(pool_scale), "c0", writes=[R_tmpb], reads=[R_tmpb])
            P.op("dve", tt(gpb[:], gpb[:], tmpb[:], ALU.mult), reads=[R_gpb, R_tmpb], writes=[R_gpb])
            Wpb = sb("Wpb", [128, 4, 2, 256], BF16, st=sh)
            R_Wpb = Reg()
            pws = sb("pws", [128, 4, 2, 256], st=sh)
            R_pws = Reg()
            for gi in range(4):
                P.dma("sp", pws[:, gi, :, :], pool_w[gi].rearrange("(c p) n -> p c n", p=128), "c0", writes=[R_pws], extra=moe0_done)
            for gi in range(4):
                for cc in range(2):
                    P.op("dve", tt(Wpb[:, gi, cc, :], pws[:, gi, cc, :], gpb[:, gi * 256:(gi + 1) * 256], ALU.mult), reads=[R_pws, R_gpb], writes=[R_Wpb])
            st_ring = Ring([sb(f"pst{i}", [128, 2], st=sh) for i in range(2)])
            junk = sb("pjunk", [128, D], BF16, st=sh)
            R_junk = Reg()
            for t in range(NT):
                stt_, R_st = st_ring.next()
                rstd_tile(XB[:, t, :], junk[:], R_junk, stt_, R_st, [R_XB[t]])
                P.op("dve", stt(tmpb[:], XB[:, t, :], stt_[:, 1:2], A1b[:], ALU.mult, ALU.mult), reads=[R_XB[t], R_st, R_A1b, R_tmpb], writes=[R_tmpb])
                P.op("dve", tt(hP[:, t, :], tmpb[:], B1b[:], ALU.add), reads=[R_tmpb, R_B1b], writes=[R_hP[t]])
                if t == 16:
                    P.op("dve", ts(hP[:, t, :], hP[:, t, :], hm[:, 0:1], None, ALU.mult), reads=[R_hP[t], R_hm], writes=[R_hP[t]])
            psP_ring = Ring([ps(f"psP{i}", [128, 8, 128], st=sh) for i in range(2)])
            psY2_ring = Ring([ps(f"psY2{i}", [128, D], st=sh) for i in range(2)])
            pT_ring2 = Ring([sb(f"plT{i}", [128, 8, 128], BF16, st=sh) for i in range(2)])
            for t in range(16):
                if t == 0:
                    srcs = [(16, 7), (0, 3), (1, 4)]
                elif t == 15:
                    srcs = [(14, 5), (15, 6), (16, 8)]
                else:
                    srcs = [(t - 1, 0), (t, 1), (t + 1, 2)]
                psP, R_psP = psP_ring.next()
                fns = []
                for dc in range(8):
                    gi = dc // 2
                    for si, (st_, kind) in enumerate(srcs):
                        fns.append(mm(psP[:, dc, :], hP[:, st_, dc * 128:(dc + 1) * 128], bandsb[:, gi * 9 + kind, :], si == 0, si == 2))
                P.op("pe", fns, reads=[R_hP[s_] for s_, _ in srcs] + [R_bands], writes=[R_psP])
                plT, R_plT = pT_ring2.next()
                P.op("act", actf(plT[:], psP[:], AF.Copy), reads=[R_psP], writes=[R_plT])
                psY2, R_psY2 = psY2_ring.next()
                fns = []
                for gi in range(4):
                    for cc in range(2):
                        fns.append(mm(psY2[:, gi * 256:(gi + 1) * 256], plT[:, gi * 2 + cc, :], Wpb[:, gi, cc, :], cc == 0, cc == 1))
                P.op("pe", fns, reads=[R_plT, R_Wpb], writes=[R_psY2])
                for half in range(2):
                    P.op("dve", tt(XB[:, t, half * 512:(half + 1) * 512], XB[:, t, half * 512:(half + 1) * 512], psY2[:, half * 512:(half + 1) * 512], ALU.add),
                         reads=[R_psY2, R_XB[t]], writes=[R_XB[t]])
            for t in range(NT):
                P.dma("sp", xspill[t * 128:(t + 1) * 128, :], XB[:, t, :], f"xb{t % 4}", reads=[R_XB[t]], writes=[R_xspill])
            pool_done = [P.op("dve", cp(dmy[:, 0:1], epsc[:, 0:1]), writes=[R_dmy], reads=[R_eps] + R_XB + R_hP),
                         ("Epe", P.cnt["pe"]), ("Eact", P.cnt["act"]), R_xspill.w]
        for t in range(4):
            pool_done.append(("Dxb%d" % t, P.dma_sems["xb%d" % t]))
        P.wait("pool", pool_done)
        P.wait("pe", pool_done)
        P.wait("act", pool_done)
        P.wait("dve", pool_done)
        P.wait("sp", pool_done)

        moe1_done = (moe_sparse if SPARSE else moe)(1)

        with contextlib.ExitStack() as sj:
            fnb = sb("fnb", [128, D], st=sj)
            R_fnb = Reg()
            P.dma("sp", fnb[:], bc(final_norm), "c0", writes=[R_fnb], extra=moe1_done)
            st_ring = Ring([sb(f"jst{i}", [128, 2], st=sj) for i in range(2)])
            junk = sb("jjunk", [128, D], BF16, st=sj)
            R_junk = Reg()
            ot_ring = Ring([sb(f"ot{i}", [128, D], st=sj) for i in range(2)])
            outs = []
            for t in range(16):
                stt_, R_st = st_ring.next()
                rstd_tile(XB[:, t, :], junk[:], R_junk, stt_, R_st, [R_XB[t]])
                ot, R_ot = ot_ring.next()
                P.op("dve", stt(ot[:], XB[:, t, :], stt_[:, 1:2], fnb[:], ALU.mult, ALU.mult), reads=[R_XB[t], R_st, R_fnb], writes=[R_ot])
                outs.append(P.dma("sp", out[t * 128:(t + 1) * 128, :], ot[:], f"ot{t % 2}", reads=[R_ot]))
            P.wait("sp", outs[-2:])
        P.emit()
    return nc


def _rope_tables(pos_row, pos_col):
    half = 32
    inv = (np.float32(10000.0) ** (-(np.arange(half, dtype=np.float32) / np.float32(half)))).astype(np.float32)
    ang_r = pos_row.astype(np.float32)[None, :] * inv[:, None]
    ang_c = pos_col.astype(np.float32)[None, :] * inv[:, None]
    cos = np.concatenate([np.cos(ang_r), np.cos(ang_r), np.cos(ang_c), np.cos(ang_c)], 0).astype(np.float32)
    sin = np.concatenate([np.sin(ang_r), np.sin(ang_r), np.sin(ang_c), np.sin(ang_c)], 0).astype(np.float32)
    return np.stack([cos, sin], 0)


def _coef(s, t, w):
    lo = np.clip(t - w // 2, 0, S)
    hi = np.clip(t - w // 2 + w, 0, S)
    cnt = (hi - lo).astype(np.float32)
    inside = (s >= lo) & (s < hi) & (s >= 0) & (s < S)
    return np.where(inside, np.float32(1.0) / cnt, np.float32(0.0)).astype(np.float32) - (s == t).astype(np.float32)


def _bands(c):
    base = OWN * c
    out = np.zeros((128, 36, 128), np.float32)
    i = np.arange(128)[:, None]
    j = np.arange(128)[None, :]
    for gi, w in enumerate((2, 4, 8, 16)):
        gb = OWN * 3 + 128 * 5
        def blk(src0, dst0):
            return _coef(src0 + i + 0 * j, dst0 + j + 0 * i, w)
        kinds = [
            blk(gb - 128, gb), blk(gb, gb), blk(gb + 128, gb),
            blk(base, base), blk(base + 128, base),
            blk(base + 128 * 14, base + 128 * 15), blk(base + 128 * 15, base + 128 * 15),
        ]
        hl = np.zeros((128, 128), np.float32)
        hl[0:8] = _coef(base - 8 + np.arange(8)[:, None] + 0 * j, base + j + 0 * np.arange(8)[:, None], w)
        hr = np.zeros((128, 128), np.float32)
        hr[8:16] = _coef(base + OWN + np.arange(8)[:, None] + 0 * j, base + 128 * 15 + j + 0 * np.arange(8)[:, None], w)
        kinds += [hl, hr]
        for k, m in enumerate(kinds):
            out[:, gi * 9 + k, :] = m
    return out.astype(ml_dtypes.bfloat16)


def _consts():
    cs = np.zeros((7, 128, 128), np.float32)
    cs[5, :, 40:48] = (np.arange(8)[None, :] * 128 + np.arange(128)[:, None]).astype(np.float32)
    cs[4] = (np.arange(128)[:, None] < np.arange(128)[None, :]).astype(np.float32)
    cs[5, :, 0:32] = np.arange(32, dtype=np.float32)[None, :]
    cs[5, :16, 32] = 1.0
    cs[5, 16:, 33] = 1.0e6
    cs[0] = np.eye(128, dtype=np.float32)
    cs[1] = 1.0 / 128.0
    rot = np.zeros((128, 128), np.float32)
    for m in range(128):
        if (m % 64) < 32:
            rot[m + 32, m] = -1.0
        else:
            rot[m - 32, m] = 1.0
    cs[2] = rot
    cs[3] = 1.0
    return cs


def prep(inputs):
    f = lambda a: np.ascontiguousarray(np.asarray(a, dtype=np.float32))
    x = f(inputs["x"])[0]
    ctx = f(inputs["ctx"])[0]
    tpos = np.arange(S)
    ropek = _rope_tables(tpos // 64, tpos % 64)
    shared = {
        "x_all": x, "ctx": ctx,
        "c2": np.stack([f(inputs["c"])[0], f(inputs["c_ctx"])], 0),
        "ada_w": f(inputs["ada_w"]), "ada_b": f(inputs["ada_b"]),
        "norm_mix": f(inputs["norm_mix"]), "norm_ffn": f(inputs["norm_ffn"]),
        "wqkv": f(inputs["attn_w_qkv"])[0],
        "qk_g": np.stack([f(inputs["attn_q_norm"])[0], f(inputs["attn_k_norm"])[0]], 0),
        "wo": f(inputs["attn_w_o"])[0], "pool_w": f(inputs["pool_w"])[0], "pool_scale": f(inputs["pool_scale"])[0],
        "router_w": f(inputs["moe_router_w"]), "router_b": f(inputs["moe_router_b"]),
        "w_gu": f(inputs["moe_w_gu"]), "b_gu": f(inputs["moe_b_gu"]),
        "w_down": f(inputs["moe_w_down"]), "b_down": f(inputs["moe_b_down"]),
        "final_norm": f(inputs["final_norm"]), "ropek": ropek, "consts": _consts(),
    }
    maps = []
    for c in range(NCORES):
        base = OWN * c
        idx = np.zeros(NEXT, np.int64)
        idx[:OWN] = base + np.arange(OWN)
        idx[OWN:OWN + 8] = base - 8 + np.arange(8) if c > 0 else base + np.arange(8)
        idx[OWN + 8:OWN + 16] = base + OWN + np.arange(8) if c < NCORES - 1 else base + np.arange(8)
        idx[OWN + 16:] = base
        x_ext = x[idx].copy()
        x_ext[OWN + 16:] = 0.0
        hmask = np.zeros((128, 1), np.float32)
        if c > 0:
            hmask[0:8] = 1.0
        if c < NCORES - 1:
            hmask[8:16] = 1.0
        m = dict(shared)
        m.update({"x_ext": x_ext, "ropeq": np.ascontiguousarray(ropek[:, :, idx]), "bands": _bands(c), "hmask": hmask})
        maps.append(m)
    return maps


_NC_CACHE = {}


def kernel(**inputs):
    maps = prep(inputs)
    if "nc" not in _NC_CACHE:
        _NC_CACHE["nc"] = build()
    res = run_bass_kernel_spmd(_NC_CACHE["nc"], maps, core_ids=list(range(NCORES)))
    return np.concatenate([r["out"] for r in res.results], axis=0)[None].astype(np.float32)
```

```python
import contextlib
import math
import numpy as np
import ml_dtypes
import concourse.bass as bass
import concourse.mybir as mybir
from concourse.bass_utils import run_bass_kernel_spmd

F32 = mybir.dt.float32
BF16 = mybir.dt.bfloat16
ALU = mybir.AluOpType
AF = mybir.ActivationFunctionType

NCORES = 8
D = 1024
S = 16384
CTX = 256
OWN = S // NCORES
NT = 17
NEXT = NT * 128
NKT = (S + CTX) // 128
E = 32
EPS = 1e-6
ALPHA = 1.702
LIM = 7.0
ENGS = ("pe", "act", "dve", "pool", "sp")
SPARSE = True
CAP = 512
NTILE = 48
NSLOT = NTILE * CAP
BLKS = [(0, 512), (512, 512), (1024, 512), (1536, 512), (2048, 16)]


class Reg:
    __slots__ = ("w", "r")

    def __init__(self):
        self.w = None
        self.r = {}


class Prog:
    def __init__(self, nc):
        self.nc = nc
        self.q = {e: [] for e in ENGS}
        self.cnt = {e: 0 for e in ENGS}
        self.dma_sems = {}
        self.sem_handles = {}

    def _deps(self, eng, reads, writes):
        deps = {}

        def add(t):
            if t is None:
                return
            k, v = t
            if eng == "pe" and k == "Epe":
                return
            if deps.get(k, 0) < v:
                deps[k] = v
        for x in reads:
            add(x.w)
        for x in writes:
            add(x.w)
            for k, v in x.r.items():
                add((k, v))
        return list(deps.items())

    def _mark(self, t, reads, writes):
        k, v = t
        for x in reads:
            if x.r.get(k, 0) < v:
                x.r[k] = v
        for x in writes:
            x.w = t
            x.r = {}

    def op(self, eng, fns, reads=(), writes=(), extra=()):
        if callable(fns):
            fns = [fns]
        deps = self._deps(eng, reads, writes) + [d for d in extra if d is not None]
        self.cnt[eng] += 1
        t = ("E" + eng, self.cnt[eng])
        self.q[eng].append(("op", fns, deps, t))
        self._mark(t, reads, writes)
        return t

    def dma(self, eng, out, in_, sem, reads=(), writes=(), extra=(), **kw):
        if sem in ("c0", "c1", "dbg"):
            self.auto_i = getattr(self, "auto_i", 0) + 1
            sem = f"a{self.auto_i % 24}"
        deps = self._deps(eng, reads, writes) + [d for d in extra if d is not None]
        if sem in self.dma_sems:
            deps.append(("D" + sem, self.dma_sems[sem]))
        self.dma_sems[sem] = self.dma_sems.get(sem, 0) + 16
        t = ("D" + sem, self.dma_sems[sem])
        self.q[eng].append(("dma", (out, in_, kw), deps, t))
        self._mark(t, reads, writes)
        return t

    def dmaf(self, eng, fn, sem, reads=(), writes=(), extra=()):
        deps = self._deps(eng, reads, writes) + [d for d in extra if d is not None]
        if sem in self.dma_sems:
            deps.append(("D" + sem, self.dma_sems[sem]))
        self.dma_sems[sem] = self.dma_sems.get(sem, 0) + 16
        t = ("D" + sem, self.dma_sems[sem])
        self.q[eng].append(("dmaf", fn, deps, t))
        self._mark(t, reads, writes)
        return t

    def wait(self, eng, deps):
        self.q[eng].append(("wait", None, [d for d in deps if d is not None], None))

    def emit(self):
        nc = self.nc
        keys = ["E" + e for e in ENGS] + ["D" + s for s in self.dma_sems]
        with contextlib.ExitStack() as st:
            for k in keys:
                self.sem_handles[k] = st.enter_context(nc.semaphore(k))
            blk = st.enter_context(nc.Block())
            engmap = {"pe": blk.tensor, "act": blk.scalar, "dve": blk.vector,
                      "pool": blk.gpsimd, "sp": blk.sync}
            sh = self.sem_handles
            for e in ENGS:
                items = self.q[e]

                def body(engine, items=items):
                    waited = {}
                    for kind, payload, deps, t in items:
                        for (k, v) in deps:
                            if waited.get(k, 0) >= v:
                                continue
                            engine.wait_ge(sh[k], v)
                            waited[k] = v
                        if kind == "wait":
                            continue
                        if kind == "op":
                            ins = None
                            for f in payload:
                                ins = f(engine)
                            ins.then_inc(sh[t[0]], 1)
                        elif kind == "dmaf":
                            payload(engine).then_inc(sh[t[0]], 16)
                        else:
                            out, in_, kw = payload
                            engine.dma_start(out=out, in_=in_, **kw).then_inc(sh[t[0]], 16)
                engmap[e](body)


def mm(out, lhsT, rhs, start=True, stop=True):
    return lambda e: e.matmul(out, lhsT, rhs, start=start, stop=stop)


def tp(out, in_, ident):
    return lambda e: e.transpose(out, in_, ident)


def actf(out, in_, func, bias=None, scale=None, accum=None):
    def f(e):
        kw = {}
        if bias is not None:
            kw["bias"] = bias
        if scale is not None:
            kw["scale"] = scale
        if accum is not None:
            kw["accum_out"] = accum
        return e.activation(out, in_, func, **kw)
    return f


def ts(out, in0, s1, s2, op0, op1=None):
    if op1 is None:
        return lambda e: e.tensor_scalar(out, in0, s1, None, op0)
    return lambda e: e.tensor_scalar(out, in0, s1, s2, op0, op1)


def tt(out, in0, in1, op):
    return lambda e: e.tensor_tensor(out, in0, in1, op)


def stt(out, in0, scalar, in1, op0, op1, accum=None):
    if accum is None:
        return lambda e: e.scalar_tensor_tensor(out, in0, scalar, in1, op0, op1)
    return lambda e: e.scalar_tensor_tensor(out, in0, scalar, in1, op0, op1, accum_out=accum)


def cp(out, in_):
    return lambda e: e.tensor_copy(out, in_)


class Ring:
    def __init__(self, bufs):
        self.bufs = bufs
        self.regs = [Reg() for _ in bufs]
        self.i = 0

    def next(self):
        s = self.i % len(self.bufs)
        self.i += 1
        return self.bufs[s], self.regs[s]


def build(dbg=None):
    nc = bass.Bass("TRN2", target_bir_lowering=False)
    P = Prog(nc)

    def din(name, shape, dt=F32):
        return nc.dram_tensor(name, list(shape), dt, kind="ExternalInput").ap()

    x_all = din("x_all", [S, D])
    x_ext = din("x_ext", [NEXT, D])
    ctx_in = din("ctx", [CTX, D])
    c2 = din("c2", [2, D])
    ada_w = din("ada_w", [2, D, 6 * D])
    ada_b = din("ada_b", [2, 6 * D])
    norm_mix = din("norm_mix", [2, D])
    norm_ffn = din("norm_ffn", [2, D])
    wqkv = din("wqkv", [D, 1536])
    qk_g = din("qk_g", [2, 128])
    wo = din("wo", [D, D])
    pool_w = din("pool_w", [4, 256, 256])
    pool_scale = din("pool_scale", [D])
    router_w = din("router_w", [2, D, E])
    router_b = din("router_b", [2, E])
    w_gu = din("w_gu", [2, E, D, 2 * D])
    b_gu = din("b_gu", [2, E, 2 * D])
    w_down = din("w_down", [2, E, D, D])
    b_down = din("b_down", [2, E, D])
    final_norm = din("final_norm", [D])
    ropek = din("ropek", [2, 128, S])
    ropeq = din("ropeq", [2, 128, NEXT])
    consts = din("consts", [7, 128, 128])
    bands = din("bands", [128, 36, 128], BF16)
    hmask = din("hmask", [128, 1])
    out = nc.dram_tensor("out", [OWN, D], F32, kind="ExternalOutput").ap()
    dbg_out = None
    if dbg is not None:
        dbg_out = nc.dram_tensor("dbg", list(dbg[1]), F32, kind="ExternalOutput").ap()

    mvec = nc.dram_tensor("mvec", [2, 2, 6 * D], F32).ap()
    bvec = nc.dram_tensor("bvec", [2, 1536], F32).ap()
    xspill = nc.dram_tensor("xspill", [NEXT, D], F32).ap()
    R_mvec = [Reg(), Reg()]
    R_bvec = Reg()
    R_xspill = Reg()

    def col(ap1d, n=8):
        return ap1d.rearrange("(k p) -> p k", p=128)

    def bc(ap1d, parts=128):
        return ap1d.rearrange("(o n) -> o n", o=1).partition_broadcast(parts)

    NC_KW = dict(allow_slow_non_contiguous=True)
    final_waits = []

    with contextlib.ExitStack() as top:
        def sb(name, shape, dt=F32, st=top):
            return st.enter_context(nc.sbuf_tensor(name, list(shape), dt))

        def ps(name, shape, dt=F32, st=top):
            return st.enter_context(nc.psum_tensor(name, list(shape), dt))

        cst = sb("cst", [128, 7, 128])
        R_cst = Reg()
        P.dma("sp", cst[:], consts.rearrange("c p n -> p c n"), "c0", writes=[R_cst])
        identf = cst[:, 0, :]
        cstb = sb("cstb", [128, 7, 128], BF16)
        R_cstb = Reg()
        P.op("dve", cp(cstb[:], cst[:]), reads=[R_cst], writes=[R_cstb])
        identb = cstb[:, 0, :]
        onesmb = cstb[:, 1, :]
        rotb = cstb[:, 2, :]
        onesb = cstb[:, 3, :]
        ustrb = cstb[:, 4, :]
        iota_e = cst[:, 5, 0:32]
        vmask = cst[:, 5, 32:33]
        notvbig = cst[:, 5, 33:34]
        kpc = cst[:, 5, 40:48]
        epsc = sb("epsc", [128, 1])
        R_eps = Reg()
        P.op("pool", lambda e: e.memset(epsc[:], EPS), writes=[R_eps])
        dmy = sb("dmy", [128, 1])
        R_dmy = Reg()
        hm = sb("hm", [128, 1])
        R_hm = Reg()
        P.dma("sp", hm[:], hmask, "c0", writes=[R_hm])

        hs_d = [nc.dram_tensor(f"hs{l}", [NSLOT, D], BF16).ap() for l in range(2)]
        ys_d = [nc.dram_tensor(f"ys{l}", [NSLOT, D], F32).ap() for l in range(2)]
        zt = sb("zt", [128, D], BF16)
        R_zt = Reg()
        P.op("pool", lambda e: e.memset(zt[:], 0.0), writes=[R_zt])
        R_hs0 = [[Reg() for _ in range(NTILE)] for _ in range(2)]
        for l_ in range(2):
            for e_ in range(NTILE):
                P.dma("pool", hs_d[l_][e_ * CAP:(e_ + 1) * CAP, :].rearrange("(s p) d -> p s d", p=128),
                      zt[:].unsqueeze(1).to_broadcast([128, CAP // 128, D]), f"zf{(l_ * NTILE + e_) % 8}", reads=[R_zt], writes=[R_hs0[l_][e_]])

        qT_d = nc.dram_tensor("qT_d", [128, 8, NEXT], BF16).ap()
        R_qTd = [Reg() for _ in BLKS]

        with contextlib.ExitStack() as sa:
            c2col = sb("c2col", [128, 8, 2], st=sa)
            R_c2 = Reg()
            for r in range(2):
                P.dma("sp", c2col[:, :, r], col(c2[r]), "c0", writes=[R_c2], **NC_KW)
            sc2 = sb("sc2", [128, 8, 2], st=sa)
            R_sc2 = Reg()
            P.op("act", actf(sc2[:], c2col[:], AF.Silu), reads=[R_c2], writes=[R_sc2])
            adab = sb("adab", [2, 2, 6 * D], st=sa)
            R_adab = Reg()
            for l in range(2):
                for r in range(2):
                    P.dma("sp", adab[r:r + 1, l, :], ada_b[l].rearrange("(o n) -> o n", o=1), "c0", writes=[R_adab])
            mrow = sb("mrow", [2, 2, 6 * D], st=sa)
            awr = Ring([sb(f"aw{i}", [128, 8, 512], st=sa) for i in range(2)])
            psA = Ring([ps(f"psA{i}", [128, 512], st=sa) for i in range(2)])
            for l in range(2):
                R_m = Reg()
                for nb in range(12):
                    wbuf, wreg = awr.next()
                    P.dma("sp", wbuf[:], ada_w[l][:, nb * 512:(nb + 1) * 512].rearrange("(k p) n -> p k n", p=128),
                          f"aw{(awr.i - 1) % 2}", writes=[wreg])
                    pb, preg = psA.next()
                    P.op("pe", [mm(pb[0:2, :], sc2[:, k, :], wbuf[:, k, :], k == 0, k == 7) for k in range(8)],
                         reads=[R_sc2, wreg], writes=[preg])
                    P.op("dve", tt(mrow[0:2, l, nb * 512:(nb + 1) * 512], pb[0:2, :], adab[0:2, l, nb * 512:(nb + 1) * 512], ALU.add),
                         reads=[preg, R_adab], writes=[R_m])
                P.dma("sp", mvec[l], mrow[0:2, l, :], "c1", reads=[R_m], writes=[R_mvec[l]])

        def mv(l, r, j):
            return mvec[l, r, j * D:(j + 1) * D]

        vecs = sb("vecs", [128, 16, 8])
        R_vecs = Reg()

        def load_cols(idx, ap1d, dep_regs):
            P.dma("sp", vecs[:, idx, :], col(ap1d), "c0", reads=dep_regs, writes=[R_vecs], **NC_KW)

        load_cols(0, norm_mix[0], [])
        load_cols(1, mv(0, 0, 1), [R_mvec[0]])
        load_cols(2, mv(0, 0, 0), [R_mvec[0]])
        load_cols(3, mv(0, 1, 1), [R_mvec[0]])
        load_cols(4, mv(0, 1, 0), [R_mvec[0]])
        load_cols(5, norm_ffn[0], [])
        load_cols(6, mv(0, 0, 4), [R_mvec[0]])
        load_cols(7, mv(0, 0, 3), [R_mvec[0]])
        load_cols(8, norm_ffn[1], [])
        load_cols(9, mv(1, 0, 4), [R_mvec[1]])
        load_cols(10, mv(1, 0, 3), [R_mvec[1]])
        AB = sb("AB", [128, 4, 8])
        R_AB = Reg()
        for i, (scx, nmx) in enumerate([(1, 0), (3, 0), (6, 5), (9, 8)]):
            P.op("dve", stt(AB[:, i, :], vecs[:, scx, :], 1.0, vecs[:, nmx, :], ALU.add, ALU.mult),
                 reads=[R_vecs], writes=[R_AB])

        if dbg is not None and dbg[0] == "A":
            P.dma("sp", dbg_out[:, 0:32].rearrange("p (i k) -> p i k", k=8), AB[:], "dbg", reads=[R_AB])
            final_waits.append(P.dma("sp", dbg_out[:, 32:160].rearrange("p (i k) -> p i k", k=8), vecs[:], "dbg", reads=[R_vecs]))
            P.wait("sp", final_waits)
            P.emit()
            return nc


        with contextlib.ExitStack() as sattn:
            KT = sb("KT", [128, 2, S + CTX], BF16, st=sattn)
            Vs = sb("Vs", [128, NKT, 256], BF16, st=sattn)
            R_KT = [[Reg() for _ in range(33)] for _ in range(2)]
            R_V = [Reg() for _ in range(NKT)]
            R_Wl, R_Wc = Reg(), Reg()
            bcol = sb("bcol", [128, 12], st=sattn)
            bvb = sb("bvb", [128, 2, 256], st=sattn)
            gcol = sb("gcol", [128, 2], st=sattn)
            negB = sb("negB", [128, 1], st=sattn)
            sw = contextlib.ExitStack()
            Wkv = sb("Wkv", [128, 8, 512], BF16, st=sw)
            Wc = sb("Wc", [128, 8, 512], BF16, st=sw)
            sq_ = contextlib.ExitStack()
            Wq = sb("Wq", [128, 8, 1024], BF16, st=sq_)
            R_bcol, R_bvb, R_gcol, R_negB = Reg(), Reg(), Reg(), Reg()

            with contextlib.ExitStack() as sbp:
                wst_ring = Ring([sb(f"wst{i}", [128, 8, 512], st=sbp) for i in range(1)])
                Bc2 = sb("Bc2", [128, 8, 2], st=sbp)
                R_Bc2 = Reg()
                P.op("dve", cp(Bc2[:, :, 0], vecs[:, 2, :]), reads=[R_vecs], writes=[R_Bc2])
                P.op("dve", cp(Bc2[:, :, 1], vecs[:, 4, :]), reads=[R_vecs], writes=[R_Bc2])
                brow = sb("brow", [2, 1536], st=sbp)
                R_brow = Reg()
                psB = ps("psB", [128, 512], st=sbp)
                R_psB = Reg()
                for nb in range(3):
                    wst, R_wst = wst_ring.next()
                    P.dma("sp", wst[:], wqkv[:, nb * 512:(nb + 1) * 512].rearrange("(k p) n -> p k n", p=128), "wst0", writes=[R_wst])
                    P.op("pe", [mm(psB[0:2, :], Bc2[:, k, :], wst[:, k, :], k == 0, k == 7) for k in range(8)],
                         reads=[R_Bc2, R_wst], writes=[R_psB])
                    P.op("dve", cp(brow[0:2, nb * 512:(nb + 1) * 512], psB[0:2, :]), reads=[R_psB], writes=[R_brow])
                    for k in range(8):
                        P.op("dve", ts(Wq[:, k, nb * 512:(nb + 1) * 512] if nb < 2 else Wkv[:, k, :], wst[:, k, :], AB[:, 0, k:k + 1], None, ALU.mult), reads=[R_wst, R_AB], writes=[R_Wl])
                        if nb == 2:
                            P.op("dve", ts(Wc[:, k, :], wst[:, k, :], AB[:, 1, k:k + 1], None, ALU.mult), reads=[R_wst, R_AB], writes=[R_Wc])
                P.dma("sp", bvec, brow[:], "c1", reads=[R_brow], writes=[R_bvec])
                P.dma("sp", bcol[:, 0:10], col(bvec[0, 0:1280], 10), "c0", reads=[R_bvec], writes=[R_bcol], **NC_KW)
                P.dma("sp", bcol[:, 10:12], col(bvec[1, 1024:1280], 2), "c0", reads=[R_bvec], writes=[R_bcol], **NC_KW)
                P.dma("sp", bvb[:, 0, :], bc(bvec[0, 1280:1536]), "c0", reads=[R_bvec], writes=[R_bvb])
                P.dma("sp", bvb[:, 1, :], bc(bvec[1, 1280:1536]), "c0", reads=[R_bvec], writes=[R_bvb])
                P.dma("sp", gcol[:], qk_g.rearrange("r p -> p r"), "c0", writes=[R_gcol], **NC_KW)
                gb = sb("gb", [128, 2, 128], st=sbp)
                R_gb = Reg()
                for r in range(2):
                    P.dma("sp", gb[:, r, :], bc(qk_g[r]), "c0", writes=[R_gb])
                gb2 = sb("gb2", [128, 2, 128], st=sbp)
                P.op("dve", stt(gb2[:].rearrange("p a b -> p (a b)"), gb[:].rearrange("p a b -> p (a b)"), -1.0, gb[:].rearrange("p a b -> p (a b)"), ALU.mult, ALU.max), reads=[R_gb], writes=[R_gb])
                mx = sb("mx", [128, 2], st=sbp)
                R_mx = Reg()
                for r in range(2):
                    P.op("dve", lambda e, r=r: e.reduce_max(mx[:, r:r + 1], gb2[:, r, :], mybir.AxisListType.X), reads=[R_gb], writes=[R_mx])
                P.op("dve", stt(negB[:], mx[:, 0:1], -math.sqrt(128.0), mx[:, 1:2], ALU.mult, ALU.mult), reads=[R_mx], writes=[R_negB])
                phaseB_done = [R_Wl.w, R_Wc.w, R_negB.w, R_bvec.w]

            if dbg is not None and dbg[0] == "B":
                t = P.dma("sp", dbg_out[:, 0:12], bcol[:], "dbg", reads=[R_bcol])
                t2 = P.dma("sp", dbg_out[:, 12:13], negB[:], "dbg", reads=[R_negB], **NC_KW)
                t3 = P.dma("sp", dbg_out[:, 16:528], bvb[:].rearrange("p a b -> p (a b)"), "dbg", reads=[R_bvb])
                P.wait("sp", [t, t2, t3])
                P.emit()
                return nc

            for phase_ in ("D", "C"):
                deep = phase_ == "C"
                with contextlib.ExitStack() as scd:
                    xs_ring = Ring([sb(f"xs{phase_}{i}", [128, D], st=scd) for i in range(3 if deep else 2)])
                    xn_ring = Ring([sb(f"xn{phase_}{i}", [128, D], BF16, st=scd) for i in range(2)])
                    st_ring = Ring([sb(f"st{phase_}{i}", [128, 2], st=scd) for i in range(4 if deep else 3)])
                    pT_ring = Ring([ps(f"pT{phase_}{i}", [128, 8, 128], BF16, st=scd) for i in range(2)])
                    xnT_ring = Ring([sb(f"xnT{phase_}{i}", [128, 8, 512], BF16, st=scd) for i in range(2 if deep else 1)])
                    cs_ring = Ring([sb(f"cs{phase_}{i}", [128, 2, 512], st=scd) for i in range(1 if deep else 1)])
                    psK_ring = Ring([ps(f"psK{phase_}{i}", [128, 512], st=scd) for i in range(2)])
                    psVb = [ps(f"psV{phase_}{i}", [128, 512], st=scd) for i in range(2)]
                    R_psV = [Reg(), Reg()]
                    psM = ps("psM" + phase_, [128, 512], st=scd)
                    psR = ps("psR" + phase_, [128, 512], st=scd)
                    R_psM, R_psR = Reg(), Reg()
                    kb_ring = Ring([sb(f"kb{phase_}{i}", [128, 512], st=scd) for i in range(2 if deep else 1)])
                    sq_ring = Ring([sb(f"sq{phase_}{i}", [128, 512], BF16, st=scd) for i in range(1 if deep else 1)])
                    rk_ring = Ring([sb(f"rk{phase_}{i}", [128, 512], st=scd) for i in range(2 if deep else 1)])
                    kn_ring = Ring([sb(f"kn{phase_}{i}", [128, 512], BF16, st=scd) for i in range(1 if deep else 1)])
                    t1_ring = Ring([sb(f"t1{phase_}{i}", [128, 512], st=scd) for i in range(1 if deep else 1)])
                    t2_ring = Ring([sb(f"t2{phase_}{i}", [128, 512], st=scd) for i in range(1 if deep else 1)])
                    xs_cnt = [0]

                    def norm_transpose_tile(src_rows, dst, R_dst, first_extra=()):
                        xs, R_xs = xs_ring.next()
                        P.dma("sp", xs[:], src_rows, f"xs{xs_cnt[0] % 3}", writes=[R_xs], extra=first_extra)
                        xs_cnt[0] += 1
                        xn, R_xn = xn_ring.next()
                        stt_, R_st = st_ring.next()
                        P.op("act", actf(xn[:], xs[:], AF.Square, accum=stt_[:, 0:1]), reads=[R_xs], writes=[R_xn, R_st])
                        P.op("act", actf(stt_[:, 1:2], stt_[:, 0:1], AF.Ln, bias=epsc[:, 0:1], scale=1.0 / D), reads=[R_st, R_eps], writes=[R_st])
                        P.op("act", actf(stt_[:, 1:2], stt_[:, 1:2], AF.Exp, scale=-0.5), reads=[R_st], writes=[R_st])
                        P.op("act", actf(xn[:], xs[:], AF.Copy, scale=stt_[:, 1:2]), reads=[R_xs, R_st], writes=[R_xn])
                        pT, R_pT = pT_ring.next()
                        P.op("pe", [tp(pT[:, k, :], xn[:, k * 128:(k + 1) * 128], identb) for k in range(8)],
                             reads=[R_xn, R_cstb], writes=[R_pT])
                        P.op("dve", cp(dst, pT[:]), reads=[R_pT], writes=[R_dst])

                    def qk_post(psX, R_psX, n, bias_ap, g_ap, cs, R_cs, out_ap, R_out):
                        kb, R_kb = kb_ring.next()
                        sq, R_sq = sq_ring.next()
                        P.op("act", actf(kb[:, :n], psX[:, :n], AF.Identity, bias=bias_ap), reads=[R_psX, R_bcol], writes=[R_kb])
                        P.op("act", actf(sq[:, :n], psX[:, :n], AF.Square, bias=bias_ap), reads=[R_psX, R_bcol], writes=[R_sq])
                        P.op("pe", mm(psM[:, :n], onesmb, sq[:, :n]), reads=[R_sq, R_cstb], writes=[R_psM])
                        rk, R_rk = rk_ring.next()
                        P.op("act", actf(rk[:, :n], psM[:, :n], AF.Ln, bias=epsc[:, 0:1]), reads=[R_psM, R_eps], writes=[R_rk])
                        P.op("act", actf(rk[:, :n], rk[:, :n], AF.Exp, scale=-0.5), reads=[R_rk], writes=[R_rk])
                        if cs is None:
                            P.op("dve", stt(out_ap, kb[:, :n], g_ap, rk[:, :n], ALU.mult, ALU.mult), reads=[R_kb, R_rk, R_gcol], writes=[R_out])
                            return
                        kn, R_kn = kn_ring.next()
                        P.op("dve", stt(kn[:, :n], kb[:, :n], g_ap, rk[:, :n], ALU.mult, ALU.mult), reads=[R_kb, R_rk, R_gcol], writes=[R_kn])
                        P.op("pe", mm(psR[:, :n], rotb, kn[:, :n]), reads=[R_kn, R_cstb], writes=[R_psR])
                        t1, R_t1 = t1_ring.next()
                        t2, R_t2 = t2_ring.next()
                        P.op("pool", tt(t1[:, :n], kn[:, :n], cs[:, 0, :n], ALU.mult), reads=[R_kn, R_cs], writes=[R_t1])
                        P.op("dve", tt(t2[:, :n], psR[:, :n], cs[:, 1, :n], ALU.mult), reads=[R_psR, R_cs], writes=[R_t2])
                        P.op("pool", tt(out_ap, t1[:, :n], t2[:, :n], ALU.add), reads=[R_t1, R_t2], writes=[R_out])

                    if phase_ == "C":
                        nblk_c = 33 if not (dbg and dbg[0] == "Csmall") else 2
                        def c_stage1(b):
                            isctx = b == 32
                            ntile = 2 if isctx else 4
                            xnT, R_xnT = xnT_ring.next()
                            for j in range(ntile):
                                rows = ctx_in[j * 128:(j + 1) * 128, :] if isctx else x_all[b * 512 + j * 128: b * 512 + (j + 1) * 128, :]
                                norm_transpose_tile(rows, xnT[:, :, j * 128:(j + 1) * 128], R_xnT, first_extra=())
                            return xnT, R_xnT

                        c_pend = {0: c_stage1(0)}
                        for b in range(nblk_c):
                            isctx = b == 32
                            ntile = 2 if isctx else 4
                            n = ntile * 128
                            if b + 1 < nblk_c:
                                c_pend[b + 1] = c_stage1(b + 1)
                            xnT, R_xnT = c_pend.pop(b)
                            Wsrc, R_W, koff, voff = (Wc, R_Wc, 0, 256) if isctx else (Wkv, R_Wl, 0, 256)
                            cs, R_cs = (None, None)
                            if not isctx:
                                cs, R_cs = cs_ring.next()
                                P.dma("sp", cs[:], ropek[:, :, b * 512:(b + 1) * 512].rearrange("c p n -> p c n"), "cs0", writes=[R_cs])
                            for j in range(ntile):
                                kt = b * 4 + j
                                hv = j % 2
                                P.op("pe", [mm(psVb[hv][:, 0:256], xnT[:, k, j * 128:(j + 1) * 128], Wsrc[:, k, voff:voff + 256], k == 0, k == 7) for k in range(8)],
                                     reads=[R_xnT, R_W], writes=[R_psV[hv]])
                                P.op("dve", tt(Vs[:, kt, :], psVb[hv][:, 0:256], bvb[:, 1 if isctx else 0, :], ALU.add), reads=[R_psV[hv], R_bvb], writes=[R_V[kt]])
                            for g in range(2):
                                psK, R_psK = psK_ring.next()
                                P.op("pe", [mm(psK[:, :n], Wsrc[:, k, koff + g * 128: koff + (g + 1) * 128], xnT[:, k, :n], k == 0, k == 7) for k in range(8)],
                                     reads=[R_xnT, R_W], writes=[R_psK])
                                bidx = (10 if isctx else 8) + g
                                qk_post(psK, R_psK, n, bcol[:, bidx:bidx + 1], gcol[:, 1:2], cs, R_cs,
                                        KT[:, g, b * 512: b * 512 + n], R_KT[g][b])

                    else:
                        qst_ring = Ring([sb(f"qst{i}", [128, 512], BF16, st=scd) for i in range(1)])
                        for bi, (b0, n) in enumerate(BLKS):
                            ntile = max(1, n // 128)
                            xnT, R_xnT = xnT_ring.next()
                            for j in range(ntile):
                                norm_transpose_tile(x_ext[b0 + j * 128: b0 + (j + 1) * 128, :], xnT[:, :, j * 128:(j + 1) * 128], R_xnT, first_extra=phaseB_done if (bi == 0 and j == 0) else ())
                            cs, R_cs = cs_ring.next()
                            P.dma("sp", cs[:, :, :n], ropeq[:, :, b0:b0 + n].rearrange("c p n -> p c n"), "cs0", writes=[R_cs], **NC_KW)
                            for h in range(8):
                                psK, R_psK = psK_ring.next()
                                P.op("pe", [mm(psK[:, :n], Wq[:, k, h * 128:(h + 1) * 128], xnT[:, k, :n], k == 0, k == 7) for k in range(8)],
                                     reads=[R_xnT, R_Wl], writes=[R_psK])
                                qst, R_qst = qst_ring.next()
                                qk_post(psK, R_psK, n, bcol[:, h:h + 1], gcol[:, 0:1], cs, R_cs, qst[:, :n], R_qst)
                                P.dma("sp", qT_d[:, h, b0:b0 + n], qst[:, :n], "qst0", reads=[R_qst], writes=[R_qTd[bi]], **NC_KW)
                    ph_done = [("E" + e_, P.cnt[e_]) for e_ in ("pe", "act", "dve", "pool")] + [("D" + k_, v_) for k_, v_ in P.dma_sems.items()]
                    for e_ in ENGS:
                        P.wait(e_, ph_done)
                if phase_ == "D":
                    sq_.close()
            cd_done = [P.op("dve", cp(dmy[:, 0:1], epsc[:, 0:1]), writes=[R_dmy], reads=[R_eps] + R_V + [r for rr in R_KT for r in rr] + R_qTd)]
            sw.close()

            with contextlib.ExitStack() as se:
                g0b = sb("g0b", [128, D], st=se)
                R_g0b = Reg()
                P.dma("sp", g0b[:], bc(mv(0, 0, 2)), "c0", reads=[R_mvec[0]], writes=[R_g0b], extra=cd_done)
                Wob = sb("Wob", [128, 8, D], BF16, st=se)
                R_Wob = Reg()
                wos_ring = Ring([sb(f"wos{i}", [128, D], st=se) for i in range(1)])
                for k in range(8):
                    wos, R_wos = wos_ring.next()
                    P.dma("sp", wos[:], wo[k * 128:(k + 1) * 128, :], "wos0", writes=[R_wos], extra=cd_done)
                    P.op("dve", tt(Wob[:, k, :], wos[:], g0b[:], ALU.mult), reads=[R_wos, R_g0b], writes=[R_Wob])
                QT_ring = Ring([sb(f"QTb{i}", [128, 8, 512], BF16, st=se) for i in range(2)])
                PT_ring = Ring([sb(f"PT{i}", [128, 512], BF16, st=se) for i in range(4)])
                attnT = sb("attnT", [128, 8, 512], BF16, st=se)
                R_attnT = Reg()
                rc = sb("rc", [128, 512], st=se)
                R_rc = Reg()
                xt_ring = Ring([sb(f"xt{i}", [128, D], st=se) for i in range(1)])
                x1_ring = Ring([sb(f"x1{i}", [128, D], st=se) for i in range(2)])
                psS_ring = Ring([ps(f"psS{i}", [128, 512], st=se) for i in range(4)])
                pO = [ps(f"pO{i}", [128, 512], st=se) for i in range(2)]
                pD = [ps(f"pD{i}", [128, 512], st=se) for i in range(2)]
                R_pO = [Reg(), Reg()]
                LA = 2
                for bi, (b0, n) in enumerate(BLKS):
                    QTb, R_QTb = QT_ring.next()
                    P.dma("sp", QTb[:, :, :n], qT_d[:, :, b0:b0 + n], f"qtb{bi % 2}", reads=[R_qTd[bi]], writes=[R_QTb], **NC_KW)
                    for hp in range(4):
                        g = hp // 2
                        units = [(kt, hh) for kt in range(NKT) for hh in range(2)]
                        pend = []
                        for i in range(len(units) + LA):
                            if i < len(units):
                                kt, hh = units[i]
                                h = hp * 2 + hh
                                pS, R_pS = psS_ring.next()
                                P.op("pe", mm(pS[:, :n], KT[:, g, kt * 128:(kt + 1) * 128], QTb[:, h, :n]),
                                     reads=[R_KT[g][kt // 4], R_QTb], writes=[R_pS])
                                PT, R_PT = PT_ring.next()
                                P.op("act", actf(PT[:, :n], pS[:, :n], AF.Exp, bias=negB[:, 0:1], scale=1.0 / math.sqrt(128.0)),
                                     reads=[R_pS, R_negB], writes=[R_PT])
                                pend.append((kt, hh, PT, R_PT))
                            if i >= LA:
                                kt, hh, PT, R_PT = pend[i - LA]
                                P.op("pe", [mm(pO[hh][:, :n], Vs[:, kt, g * 128:(g + 1) * 128], PT[:, :n], kt == 0, kt == NKT - 1),
                                            mm(pD[hh][:, :n], onesb, PT[:, :n], kt == 0, kt == NKT - 1)],
                                     reads=[R_PT, R_V[kt], R_cstb], writes=[R_pO[hh]])
                        for hh in range(2):
                            h = hp * 2 + hh
                            P.op("dve", lambda e, hh=hh, n=n: e.reciprocal(rc[:, :n], pD[hh][:, :n]), reads=[R_pO[hh]], writes=[R_rc])
                            P.op("dve", tt(attnT[:, h, :n], pO[hh][:, :n], rc[:, :n], ALU.mult), reads=[R_pO[hh], R_rc], writes=[R_attnT])
                    for j in range(max(1, n // 128)):
                        m = min(128, n)
                        ti = b0 // 128 + j
                        xt, R_xt = xt_ring.next()
                        P.dma("sp", xt[:], x_ext[ti * 128:(ti + 1) * 128, :], "xt0", writes=[R_xt])
                        x1, R_x1 = x1_ring.next()
                        if m < 128:
                            P.op("dve", cp(x1[:], xt[:]), reads=[R_xt], writes=[R_x1])
                        for half in range(2):
                            pY, R_pY = psS_ring.next()
                            P.op("pe", [mm(pY[:m, :], attnT[:, h, j * 128:j * 128 + m], Wob[:, h, half * 512:(half + 1) * 512], h == 0, h == 7) for h in range(8)],
                                 reads=[R_attnT, R_Wob], writes=[R_pY])
                            P.op("dve", tt(x1[:m, half * 512:(half + 1) * 512], pY[:m, :], xt[:m, half * 512:(half + 1) * 512], ALU.add),
                                 reads=[R_pY, R_xt], writes=[R_x1])
                        P.dma("sp", xspill[ti * 128:(ti + 1) * 128, :], x1[:], f"x1{(x1_ring.i - 1) % 2}", reads=[R_x1], writes=[R_xspill])
                attn_done = [P.op("dve", cp(dmy[:, 0:1], epsc[:, 0:1]), writes=[R_dmy], reads=[R_eps, R_attnT, R_rc] + x1_ring.regs + xt_ring.regs + PT_ring.regs)]
                attn_done.append(R_xspill.w)
                attn_done.append(("Epe", P.cnt["pe"]))
                attn_done.append(("Eact", P.cnt["act"]))

        XB = sb("XB", [128, NT, D])
        R_XB = [Reg() for _ in range(NT)]
        for t in range(NT):
            P.dma("sp", XB[:, t, :], xspill[t * 128:(t + 1) * 128, :], f"xb{t % 4}", reads=[R_xspill], writes=[R_XB[t]], extra=attn_done)

        C7 = (LIM * ALPHA) / (1.0 + math.exp(-LIM * ALPHA))

        def rstd_tile(src_ap, junk, R_junk, stt_, R_st, rd):
            P.op("act", actf(junk, src_ap, AF.Square, accum=stt_[:, 0:1]), reads=rd, writes=[R_junk, R_st])
            P.op("act", actf(stt_[:, 1:2], stt_[:, 0:1], AF.Ln, bias=epsc[:, 0:1], scale=1.0 / D), reads=[R_st, R_eps], writes=[R_st])
            P.op("act", actf(stt_[:, 1:2], stt_[:, 1:2], AF.Exp, scale=-0.5), reads=[R_st], writes=[R_st])

        def moe(l):
            with contextlib.ExitStack() as sm:
                hfT = sb(f"hfT{l}", [128, 8, NEXT], BF16, st=sm)
                R_hf = [Reg() for _ in BLKS]
                Gt = sb(f"G{l}", [128, NT, E], st=sm)
                R_G = [Reg() for _ in range(NT)]
                GT = sb(f"GT{l}", [E, NEXT], st=sm)
                R_GT = [Reg() for _ in range(NT)]
                bd_sb = sb(f"bd{l}", [E, D], st=sm)
                R_bd = Reg()
                P.dma("sp", bd_sb[:], b_down[l], "c0", writes=[R_bd])
                bguc = sb(f"bguc{l}", [128, 16, E], st=sm)
                bgs = sb(f"bgs{l}", [128, 8, E], st=sm)
                R_bguc = Reg()
                ifirst = vecs[:, 7 if l == 0 else 10, :]
                Afc = AB[:, 2 + l, :]
                with contextlib.ExitStack() as s1:
                    bgr = sb(f"bgr{l}", [E, 2 * D], st=s1)
                    R_bgr = Reg()
                    P.dma("sp", bgr[:], b_gu[l], "c0", writes=[R_bgr])
                    psT = ps(f"psT{l}", [128, 16, E], st=s1)
                    R_psT = Reg()
                    P.op("pe", [tp(psT[:, c, :], bgr[0:E, c * 128:(c + 1) * 128], identf[0:E, 0:E]) for c in range(16)], reads=[R_bgr, R_cst], writes=[R_psT])
                    P.op("dve", cp(bguc[:], psT[:]), reads=[R_psT], writes=[R_bguc])
                    P.op("dve", ts(bgs[:].rearrange("p a b -> p (a b)"), bguc[:, 0:8, :].rearrange("p a b -> p (a b)"), ALPHA, None, ALU.mult), reads=[R_bguc], writes=[R_bguc])
                    wr = sb(f"wr{l}", [128, 8, E], st=s1)
                    R_wr = Reg()
                    P.dma("sp", wr[:], router_w[l].rearrange("(k p) e -> p k e", p=128), "c0", writes=[R_wr])
                    rbb = sb(f"rbb{l}", [128, E], st=s1)
                    R_rbb = Reg()
                    P.dma("sp", rbb[:], bc(router_b[l]), "c0", writes=[R_rbb])
                    xnf_ring = Ring([sb(f"xnf{l}{i}", [128, D], st=s1) for i in range(2)])
                    st_ring = Ring([sb(f"mst{l}{i}", [128, 2], st=s1) for i in range(2)])
                    h32_ring = Ring([sb(f"h32{l}{i}", [128, 8, 128], st=s1) for i in range(2)])
                    p32_ring = Ring([ps(f"p32{l}{i}", [128, 8, 128], st=s1) for i in range(2)])
                    psL = ps(f"psL{l}", [128, 512], st=s1)
                    R_psL = Reg()
                    psG2 = ps(f"psG2{l}", [128, 512], st=s1)
                    R_psG2 = Reg()
                    sm_ring = Ring([sb(f"smx{l}{i}", [128, 4, E], st=s1) for i in range(2)])
                    s8_ring = Ring([sb(f"s8{l}{i}", [128, 12], st=s1) for i in range(2)])
                    for t in range(NT):
                        xnf, R_xnf = xnf_ring.next()
                        stt_, R_st = st_ring.next()
                        rstd_tile(XB[:, t, :], xnf[:], R_xnf, stt_, R_st, [R_XB[t]])
                        P.op("act", actf(xnf[:], XB[:, t, :], AF.Copy, scale=stt_[:, 1:2]), reads=[R_XB[t], R_st], writes=[R_xnf])
                        p32, R_p32 = p32_ring.next()
                        P.op("pe", [tp(p32[:, k, :], xnf[:, k * 128:(k + 1) * 128], identf) for k in range(8)], reads=[R_xnf, R_cst], writes=[R_p32])
                        h32, R_h32 = h32_ring.next()
                        for k in range(8):
                            P.op("dve", ts(h32[:, k, :], p32[:, k, :], Afc[:, k:k + 1], ifirst[:, k:k + 1], ALU.mult, ALU.add), reads=[R_p32, R_AB, R_vecs], writes=[R_h32])
                        bi = min(t // 4, 4)
                        P.op("pool", cp(hfT[:, :, t * 128:(t + 1) * 128], h32[:]), reads=[R_h32], writes=[R_hf[bi]])
                        P.op("pe", [mm(psL[:, 0:E], h32[:, k, :], wr[:, k, :], k == 0, k == 7) for k in range(8)], reads=[R_h32, R_wr], writes=[R_psL])
                        sx, R_sx = sm_ring.next()
                        s8, R_s8 = s8_ring.next()
                        P.op("dve", tt(sx[:, 0, :], psL[:, 0:E], rbb[:], ALU.add), reads=[R_psL, R_rbb], writes=[R_sx])
                        P.op("dve", lambda e, s8=s8, sx=sx: e.max(s8[:, 0:8], sx[:, 0, :]), reads=[R_sx], writes=[R_s8])
                        P.op("dve", ts(s8[:, 8:9], s8[:, 0:1], -1.0, None, ALU.mult), reads=[R_s8], writes=[R_s8])
                        P.op("act", actf(sx[:, 1, :], sx[:, 0, :], AF.Exp, bias=s8[:, 8:9]), reads=[R_sx, R_s8], writes=[R_sx])
                        P.op("dve", ts(sx[:, 2, :], sx[:, 0, :], s8[:, 3:4], None, ALU.is_ge), reads=[R_sx, R_s8], writes=[R_sx])
                        P.op("dve", stt(sx[:, 3, :], sx[:, 1, :], 1.0, sx[:, 2, :], ALU.mult, ALU.mult, accum=s8[:, 9:10]), reads=[R_sx], writes=[R_sx, R_s8])
                        P.op("dve", lambda e, s8=s8: e.reciprocal(s8[:, 10:11], s8[:, 9:10]), reads=[R_s8], writes=[R_s8])
                        P.op("dve", ts(Gt[:, t, :], sx[:, 3, :], s8[:, 10:11], None, ALU.mult), reads=[R_sx, R_s8], writes=[R_G[t]])
                        P.op("pe", tp(psG2[0:E, 0:128], Gt[:, t, :], identf), reads=[R_G[t], R_cst], writes=[R_psG2])
                        P.op("dve", cp(GT[0:E, t * 128:(t + 1) * 128], psG2[0:E, 0:128]), reads=[R_psG2], writes=[R_GT[t]])
                    g1_done = [P.op("dve", cp(dmy[:, 0:1], epsc[:, 0:1]), writes=[R_dmy], reads=[R_eps] + R_hf + R_GT + R_XB + [R_bguc]),
                               ("Epe", P.cnt["pe"]), ("Eact", P.cnt["act"]), ("Epool", P.cnt["pool"])]
                with contextlib.ExitStack() as s3:
                    actT = sb(f"actT{l}", [128, 8, NEXT], BF16, st=s3)
                    R_act = [[Reg() for _ in BLKS] for _ in range(8)]
                    wring = Ring([sb(f"wp{l}{i}", [128, 8, 512], BF16, st=s3) for i in range(4)])
                    sS_ring = Ring([sb(f"sS{l}{i}", [128, 512], st=s3) for i in range(2)])
                    uS_ring = Ring([sb(f"uS{l}{i}", [128, 512], st=s3) for i in range(2)])
                    psG_ring = Ring([ps(f"psG{l}{i}", [128, 512], st=s3) for i in range(2)])
                    psU_ring = Ring([ps(f"psU{l}{i}", [128, 512], st=s3) for i in range(2)])
                    psY_ring = Ring([ps(f"psY{l}{i}", [128, 512], st=s3) for i in range(3)])
                    for t in range(NT):
                        for half in range(2):
                            pY, R_pY = psY_ring.next()
                            P.op("pe", mm(pY[:, :], GT[0:E, t * 128:(t + 1) * 128], bd_sb[0:E, half * 512:(half + 1) * 512]),
                                 reads=[R_GT[t], R_bd], writes=[R_pY], extra=g1_done)
                            P.op("act", actf(XB[:, t, half * 512:(half + 1) * 512], pY[:, :], AF.Copy), reads=[R_pY], writes=[R_XB[t]], extra=g1_done)
                    wcnt = [0]

                    def wload(parts):
                        wb, R_wb = wring.next()
                        slot = (wring.i - 1) % 4
                        for dst, src in parts:
                            P.dma("pool", dst(wb), src, f"w{slot}_{wcnt[0] % 2}", writes=[R_wb], extra=g1_done if wcnt[0] < 8 else ())
                            wcnt[0] += 1
                        return wb, R_wb

                    for e in range(E):
                        wge = w_gu[l, e].rearrange("(k p) n -> p k n", p=128)
                        for pi in range(4):
                            wb, R_wb = wload([(lambda b: b[:, :, 0:256], wge[:, :, pi * 256:(pi + 1) * 256]),
                                              (lambda b: b[:, :, 256:512], wge[:, :, D + pi * 256: D + (pi + 1) * 256])])
                            for jj in range(2):
                                fc = pi * 2 + jj
                                for bi, (b0, n) in enumerate(BLKS):
                                    psG, R_psG = psG_ring.next()
                                    psU, R_psU = psU_ring.next()
                                    P.op("pe", [mm(psG[:, :n], wb[:, k, jj * 128:(jj + 1) * 128], hfT[:, k, b0:b0 + n], k == 0, k == 7) for k in range(8)],
                                         reads=[R_wb, R_hf[bi]], writes=[R_psG])
                                    P.op("pe", [mm(psU[:, :n], wb[:, k, 256 + jj * 128:256 + (jj + 1) * 128], hfT[:, k, b0:b0 + n], k == 0, k == 7) for k in range(8)],
                                         reads=[R_wb, R_hf[bi]], writes=[R_psU])
                                    sS, R_sS = sS_ring.next()
                                    uS, R_uS = uS_ring.next()
                                    P.op("act", actf(sS[:, :n], psG[:, :n], AF.Silu, bias=bgs[:, fc, e:e + 1], scale=ALPHA), reads=[R_psG, R_bguc], writes=[R_sS])
                                    P.op("act", actf(uS[:, :n], psU[:, :n], AF.Identity, bias=bguc[:, 8 + fc, e:e + 1]), reads=[R_psU, R_bguc], writes=[R_uS])
                                    P.op("dve", ts(sS[:, :n], sS[:, :n], C7, 1.0 / ALPHA, ALU.min, ALU.mult), reads=[R_sS], writes=[R_sS])
                                    P.op("dve", ts(uS[:, :n], uS[:, :n], LIM, -LIM, ALU.min, ALU.max), reads=[R_uS], writes=[R_uS])
                                    P.op("dve", stt(actT[:, fc, b0:b0 + n], uS[:, :n], 1.0, sS[:, :n], ALU.add, ALU.mult), reads=[R_uS, R_sS], writes=[R_act[fc][bi]])
                        wde = w_down[l, e].rearrange("(k p) n -> p k n", p=128)
                        for half in range(2):
                            wb, R_wb = wload([(lambda b: b[:, :, :], wde[:, :, half * 512:(half + 1) * 512])])
                            for t in range(NT):
                                m = 128 if t < 16 else 16
                                bi = min(t // 4, 4)
                                pY, R_pY = psY_ring.next()
                                P.op("pe", [mm(pY[:m, :], actT[:, fc, t * 128:t * 128 + m], wb[:, fc, :], fc == 0, fc == 7) for fc in range(8)],
                                     reads=[R_wb] + [R_act[fc][bi] for fc in range(8)], writes=[R_pY])
                                P.op("dve", stt(XB[:m, t, half * 512:(half + 1) * 512], pY[:m, :], Gt[:m, t, e:e + 1], XB[:m, t, half * 512:(half + 1) * 512], ALU.mult, ALU.add),
                                     reads=[R_pY, R_G[t]], writes=[R_XB[t]])
                    g3_done = [("Epe", P.cnt["pe"]), ("Eact", P.cnt["act"]), ("Edve", P.cnt["dve"])]
                with contextlib.ExitStack() as s4:
                    gfb = sb(f"gfb{l}", [128, D], st=s4)
                    R_gfb = Reg()
                    P.dma("sp", gfb[:], bc(mv(l, 0, 5)), "c0", reads=[R_mvec[l]], writes=[R_gfb], extra=g3_done)
                    xt_ring = Ring([sb(f"mxt{l}{i}", [128, D], st=s4) for i in range(2)])
                    for t in range(NT):
                        xt, R_xt = xt_ring.next()
                        P.dma("sp", xt[:], xspill[t * 128:(t + 1) * 128, :], f"mxt{t % 2}", reads=[R_xspill], writes=[R_xt], extra=g3_done)
                        P.op("dve", tt(XB[:, t, :], XB[:, t, :], gfb[:], ALU.mult), reads=[R_XB[t], R_gfb], writes=[R_XB[t]])
                        P.op("dve", tt(XB[:, t, :], XB[:, t, :], xt[:], ALU.add), reads=[R_XB[t], R_xt], writes=[R_XB[t]])
                    return [P.op("dve", cp(dmy[:, 0:1], epsc[:, 0:1]), writes=[R_dmy], reads=[R_eps] + R_XB + xt_ring.regs),
                            ("Epe", P.cnt["pe"]), ("Eact", P.cnt["act"]), ("Edve", P.cnt["dve"])]


        I32 = mybir.dt.int32
        U32 = mybir.dt.uint32

        _bnd = {}

        def bnd_reg(engine, which="r"):
            if which not in _bnd:
                _bnd[which] = engine.to_reg(NSLOT - 1 if which == "r" else (int(which[1:]) + 1) * E * D - 1)
            return _bnd[which]

        def moe_sparse(l):
            hs, ys = hs_d[l], ys_d[l]
            wgu_rows = w_gu.rearrange("l e r n -> (l e r) n")
            wdn_rows = w_down.rearrange("l e r n -> (l e r) n")
            with contextlib.ExitStack() as sm:
                Gt = sb(f"G{l}", [128, NT, E], st=sm)
                R_G = [Reg() for _ in range(NT)]
                GT = sb(f"GT{l}", [E, NEXT], st=sm)
                R_GT = [Reg() for _ in range(NT)]
                bd_sb = sb(f"bd{l}", [E, D], st=sm)
                R_bd = Reg()
                P.dma("sp", bd_sb[:], b_down[l], "c0", writes=[R_bd])
                SLOT = sb(f"SLOT{l}", [128, NT, 4], I32, st=sm)
                GK = sb(f"GK{l}", [128, NT, 4], st=sm)
                R_SLOT = [Reg() for _ in range(NT)]
                R_GK = [Reg() for _ in range(NT)]
                R_hs = [[Reg() for _ in range(4)] for _ in range(NT)]
                IDXW = sb(f"IDXW{l}", [128, NTILE, 8], I32, st=sm)
                bsel = sb(f"bsel{l}", [128, NTILE, 16], st=sm)
                bgsel = sb(f"bgsel{l}", [128, NTILE, 8], st=sm)
                R_IDXW, R_bsel = Reg(), Reg()
                ifirst = vecs[:, 7 if l == 0 else 10, :]
                Afc = AB[:, 2 + l, :]
                with contextlib.ExitStack() as s1:
                    bguc = sb(f"bguc{l}", [128, 16, E], st=s1)
                    R_bguc = Reg()
                    bgr = sb(f"bgr{l}", [E, 2 * D], st=s1)
                    R_bgr = Reg()
                    P.dma("sp", bgr[:], b_gu[l], "c0", writes=[R_bgr])
                    psT = ps(f"psT{l}", [128, 16, E], st=s1)
                    R_psT = Reg()
                    P.op("pe", [tp(psT[:, c, :], bgr[0:E, c * 128:(c + 1) * 128], identf[0:E, 0:E]) for c in range(16)], reads=[R_bgr, R_cst], writes=[R_psT])
                    P.op("dve", cp(bguc[:], psT[:]), reads=[R_psT], writes=[R_bguc])
                    wr = sb(f"wr{l}", [128, 8, E], st=s1)
                    R_wr = Reg()
                    P.dma("sp", wr[:], router_w[l].rearrange("(k p) e -> p k e", p=128), "c0", writes=[R_wr])
                    rbb = sb(f"rbb{l}", [128, E], st=s1)
                    R_rbb = Reg()
                    P.dma("sp", rbb[:], bc(router_b[l]), "c0", writes=[R_rbb])
                    Afr = sb(f"Afr{l}", [128, D], st=s1)
                    Bfr = sb(f"Bfr{l}", [128, D], st=s1)
                    tmr = sb(f"tmr{l}", [128, D], st=s1)
                    R_Afr, R_Bfr, R_tmr = Reg(), Reg(), Reg()
                    P.dma("sp", Afr[:], bc(mv(l, 0, 4)), "c0", reads=[R_mvec[l]], writes=[R_Afr])
                    P.dma("sp", tmr[:], bc(norm_ffn[l]), "c0", writes=[R_tmr])
                    P.op("dve", stt(Afr[:], Afr[:], 1.0, tmr[:], ALU.add, ALU.mult), reads=[R_Afr, R_tmr], writes=[R_Afr])
                    P.dma("sp", Bfr[:], bc(mv(l, 0, 3)), "c0", reads=[R_mvec[l]], writes=[R_Bfr])
                    MSK = sb(f"MSK{l}", [128, NT, E], BF16, st=s1)
                    R_MSK = [Reg() for _ in range(NT)]
                    RK4 = sb(f"RK4{l}", [128, NT, 4], st=s1)
                    E4 = sb(f"E4{l}", [128, NT, 4], st=s1)
                    R_RK4 = [Reg() for _ in range(NT)]
                    HFT = sb(f"HFT{l}", [128, NT, D], BF16, st=s1)
                    R_HFT = [Reg() for _ in range(NT)]
                    xnf_ring = Ring([sb(f"xnf{l}{i}", [128, D], st=s1) for i in range(2)])
                    st_ring = Ring([sb(f"mst{l}{i}", [128, 2], st=s1) for i in range(2)])
                    h32_ring = Ring([sb(f"h32{l}{i}", [128, 8, 128], st=s1) for i in range(2)])
                    p32_ring = Ring([ps(f"p32{l}{i}", [128, 8, 128], st=s1) for i in range(2)])
                    psL = ps(f"psL{l}", [128, 512], st=s1)
                    R_psL = Reg()
                    psG2 = ps(f"psG2{l}", [128, 512], st=s1)
                    R_psG2 = Reg()
                    psRk = ps(f"psRk{l}", [128, 512], st=s1)
                    R_psRk = Reg()
                    sm_ring = Ring([sb(f"smx{l}{i}", [128, 6, E], st=s1) for i in range(2)])
                    s8_ring = Ring([sb(f"s8{l}{i}", [128, 40], st=s1) for i in range(2)])
                    i8_ring = Ring([sb(f"i8{l}{i}", [128, 8], U32, st=s1) for i in range(2)])
                    for t in range(NT):
                        xnf, R_xnf = xnf_ring.next()
                        stt_, R_st = st_ring.next()
                        rstd_tile(XB[:, t, :], xnf[:], R_xnf, stt_, R_st, [R_XB[t]])
                        P.op("act", actf(xnf[:], XB[:, t, :], AF.Copy, scale=stt_[:, 1:2]), reads=[R_XB[t], R_st], writes=[R_xnf])
                        p32, R_p32 = p32_ring.next()
                        P.op("pe", [tp(p32[:, k, :], xnf[:, k * 128:(k + 1) * 128], identf) for k in range(8)], reads=[R_xnf, R_cst], writes=[R_p32])
                        h32, R_h32 = h32_ring.next()
                        for k in range(8):
                            P.op("dve", ts(h32[:, k, :], p32[:, k, :], Afc[:, k:k + 1], ifirst[:, k:k + 1], ALU.mult, ALU.add), reads=[R_p32, R_AB, R_vecs], writes=[R_h32])
                        P.op("pool", tt(tmr[:], xnf[:], Afr[:], ALU.mult), reads=[R_xnf, R_Afr, R_tmr], writes=[R_tmr])
                        P.op("pool", tt(HFT[:, t, :], tmr[:], Bfr[:], ALU.add), reads=[R_tmr, R_Bfr], writes=[R_HFT[t]])
                        P.op("pe", [mm(psL[:, 0:E], h32[:, k, :], wr[:, k, :], k == 0, k == 7) for k in range(8)], reads=[R_h32, R_wr], writes=[R_psL])
                        sx, R_sx = sm_ring.next()
                        s8, R_s8 = s8_ring.next()
                        i8, R_i8 = i8_ring.next()
                        P.op("dve", tt(sx[:, 0, :], psL[:, 0:E], rbb[:], ALU.add), reads=[R_psL, R_rbb], writes=[R_sx])
                        P.op("dve", lambda e, s8=s8, sx=sx: e.max(s8[:, 0:8], sx[:, 0, :]), reads=[R_sx], writes=[R_s8])
                        P.op("dve", lambda e, s8=s8, sx=sx, i8=i8: e.max_index(i8[:, 0:8], s8[:, 0:8], sx[:, 0, :]), reads=[R_sx, R_s8], writes=[R_i8])
                        P.op("dve", ts(s8[:, 8:9], s8[:, 0:1], -1.0, None, ALU.mult), reads=[R_s8], writes=[R_s8])
                        P.op("act", actf(s8[:, 12:16], s8[:, 0:4], AF.Exp, bias=s8[:, 8:9], accum=s8[:, 9:10]), reads=[R_s8], writes=[R_s8])
                        P.op("dve", lambda e, s8=s8: e.reciprocal(s8[:, 10:11], s8[:, 9:10]), reads=[R_s8], writes=[R_s8])
                        P.op("dve", ts(GK[:, t, :], s8[:, 12:16], s8[:, 10:11], None, ALU.mult), reads=[R_s8], writes=[R_GK[t]])
                        P.op("dve", cp(E4[:, t, :], i8[:, 0:4]), reads=[R_i8], writes=[R_RK4[t]])
                        for k in range(4):
                            P.op("dve", ts(sx[:, 2 + k, :], iota_e, E4[:, t, k:k + 1], None, ALU.is_equal), reads=[R_RK4[t], R_cst], writes=[R_sx])
                        P.op("dve", tt(sx[:, 1, :], sx[:, 2, :], sx[:, 3, :], ALU.add), reads=[R_sx], writes=[R_sx])
                        P.op("dve", tt(sx[:, 1, :], sx[:, 1, :], sx[:, 4, :], ALU.add), reads=[R_sx], writes=[R_sx])
                        P.op("dve", tt(sx[:, 1, :], sx[:, 1, :], sx[:, 5, :], ALU.add), reads=[R_sx], writes=[R_sx])
                        if t == 16:
                            P.op("dve", ts(sx[:, 1, :], sx[:, 1, :], vmask, None, ALU.mult), reads=[R_sx, R_cst], writes=[R_sx])
                        P.op("dve", cp(MSK[:, t, :], sx[:, 1, :]), reads=[R_sx], writes=[R_MSK[t]])
                        P.op("dve", ts(Gt[:, t, :], sx[:, 2, :], GK[:, t, 0:1], None, ALU.mult), reads=[R_sx, R_GK[t]], writes=[R_G[t]])
                        for k in range(1, 4):
                            P.op("dve", stt(Gt[:, t, :], sx[:, 2 + k, :], GK[:, t, k:k + 1], Gt[:, t, :], ALU.mult, ALU.add), reads=[R_sx, R_GK[t], R_G[t]], writes=[R_G[t]])
                        P.op("pe", tp(psG2[0:E, 0:128], Gt[:, t, :], identf), reads=[R_G[t], R_cst], writes=[R_psG2])
                        P.op("dve", cp(GT[0:E, t * 128:(t + 1) * 128], psG2[0:E, 0:128]), reads=[R_psG2], writes=[R_GT[t]])
                        fns = [mm(psRk[:, 0:E], onesb, MSK[:, tp_, :], tp_ == 0, False) for tp_ in range(t)]
                        fns.append(mm(psRk[:, 0:E], ustrb, MSK[:, t, :], t == 0, True))
                        P.op("pe", fns, reads=R_MSK[0:t + 1] + [R_cstb], writes=[R_psRk])
                        for k in range(4):
                            P.op("dve", stt(sx[:, 2 + k, :], sx[:, 2 + k, :], 1.0, psRk[:, 0:E], ALU.mult, ALU.mult, accum=RK4[:, t, k:k + 1]),
                                 reads=[R_sx, R_psRk], writes=[R_sx, R_RK4[t]])
                    gl = sb(f"gl{l}", [128, 8, E], st=s1)
                    R_gl = Reg()
                    P.op("pe", [mm(psRk[:, 0:E], onesb, MSK[:, tp_, :], tp_ == 0, tp_ == NT - 1) for tp_ in range(NT)], reads=R_MSK + [R_cstb], writes=[R_psRk])
                    P.op("dve", cp(gl[:, 0, :], psRk[:, 0:E]), reads=[R_psRk], writes=[R_gl])
                    P.op("dve", ts(gl[:, 3, :], gl[:, 0, :], 0.0, None, ALU.is_gt), reads=[R_gl], writes=[R_gl])
                    for m_ in range(1, 5):
                        P.op("dve", stt(gl[:, 3, :], gl[:, 0, :], float(m_ * CAP), gl[:, 3, :], ALU.is_gt, ALU.add), reads=[R_gl], writes=[R_gl])
                    P.op("dve", cp(gl[:, 4, :], gl[:, 3, :]), reads=[R_gl], writes=[R_gl])
                    cur, nxt = 4, 5
                    for sft in (1, 2, 4, 8, 16):
                        P.op("dve", cp(gl[:, nxt, 0:sft], gl[:, cur, 0:sft]), reads=[R_gl], writes=[R_gl])
                        P.op("dve", tt(gl[:, nxt, sft:E], gl[:, cur, sft:E], gl[:, cur, 0:E - sft], ALU.add), reads=[R_gl], writes=[R_gl])
                        cur, nxt = nxt, cur
                    cum = gl[:, cur, :]
                    P.op("dve", tt(gl[:, 6, :], cum, gl[:, 3, :], ALU.subtract), reads=[R_gl], writes=[R_gl])
                    P.op("dve", ts(gl[:, 6, :], gl[:, 6, :], float(CAP), None, ALU.mult), reads=[R_gl], writes=[R_gl])
                    ejt = sb(f"ejt{l}", [128, NTILE], st=s1)
                    R_ej = Reg()
                    for j in range(NTILE):
                        P.op("dve", stt(gl[:, 7, :], cum, float(j), cst[:, 3, 0:E], ALU.is_le, ALU.mult, accum=ejt[:, j:j + 1]), reads=[R_gl, R_cst], writes=[R_gl, R_ej])
                    P.op("dve", ts(ejt[:], ejt[:], float(E - 1), None, ALU.min), reads=[R_ej], writes=[R_ej])
                    idxf = sb(f"idxf{l}", [128, NTILE, 8], st=s1)
                    R_idxf = Reg()
                    for k in range(8):
                        P.op("dve", ts(idxf[:, :, k], ejt[:], float(D), kpc[:, k:k + 1], ALU.mult, ALU.add), reads=[R_ej, R_cst], writes=[R_idxf])
                        if l == 1:
                            P.op("dve", ts(idxf[:, :, k], idxf[:, :, k], float(E * D), None, ALU.add), reads=[R_idxf], writes=[R_idxf])
                    P.op("dve", cp(IDXW[:], idxf[:]), reads=[R_idxf], writes=[R_IDXW])
                    ohj = sb(f"ohj{l}", [128, E], st=s1)
                    tmpb2 = sb(f"tmpb2{l}", [128, 16, E], st=s1)
                    R_ohj, R_tmpb2 = Reg(), Reg()
                    for j in range(NTILE):
                        P.op("dve", ts(ohj[:], iota_e, ejt[:, j:j + 1], None, ALU.is_equal), reads=[R_ej, R_cst], writes=[R_ohj])
                        P.op("dve", tt(tmpb2[:], bguc[:], ohj[:].unsqueeze(1).to_broadcast([128, 16, E]), ALU.mult), reads=[R_ohj, R_bguc], writes=[R_tmpb2])
                        P.op("dve", lambda e_, j=j: e_.reduce_sum(bsel[:, j, :], tmpb2[:], mybir.AxisListType.X), reads=[R_tmpb2], writes=[R_bsel])
                    P.op("dve", ts(bgsel[:].rearrange("p a b -> p (a b)").rearrange("p (a b) -> p a b", b=8), bsel[:, :, 0:8], ALPHA, None, ALU.mult), reads=[R_bsel], writes=[R_bsel])
                    s8b_ring = Ring([sb(f"s8b{l}{i}", [128, 8], st=s1) for i in range(2)])
                    ohk_ring = Ring([sb(f"ohk{l}{i}", [128, E], st=s1) for i in range(2)])
                    for t in range(NT):
                        s8b, R_s8b = s8b_ring.next()
                        for k in range(4):
                            ohk, R_ohk = ohk_ring.next()
                            P.op("dve", ts(ohk[:], iota_e, E4[:, t, k:k + 1], None, ALU.is_equal), reads=[R_RK4[t], R_cst], writes=[R_ohk])
                            P.op("dve", stt(ohk[:], ohk[:], 1.0, gl[:, 6, :], ALU.mult, ALU.mult, accum=s8b[:, k:k + 1]), reads=[R_ohk, R_gl], writes=[R_ohk, R_s8b])
                        P.op("dve", tt(s8b[:, 4:8], s8b[:, 0:4], RK4[:, t, :], ALU.add), reads=[R_s8b, R_RK4[t]], writes=[R_s8b])
                        if t == 16:
                            P.op("dve", ts(s8b[:, 4:8], s8b[:, 4:8], notvbig, None, ALU.add), reads=[R_s8b, R_cst], writes=[R_s8b])
                        P.op("dve", cp(SLOT[:, t, :], s8b[:, 4:8]), reads=[R_s8b], writes=[R_SLOT[t]])
                        for k in range(4):
                            P.dmaf("pool", lambda e, t=t, k=k: e.indirect_dma_start(
                                out=hs[:, :], out_offset=bass.IndirectOffsetOnAxis(ap=SLOT[:, t, k:k + 1], axis=0),
                                in_=HFT[:, t, :], in_offset=None, bounds_check=bnd_reg(e), oob_is_err=False),
                                f"sc{(t * 4 + k) % 8}", reads=[R_HFT[t], R_SLOT[t]], writes=[R_hs[t][k]], extra=[r.w for r in R_hs0[l]])
                    g1_done = [P.op("dve", cp(dmy[:, 0:1], epsc[:, 0:1]), writes=[R_dmy], reads=[R_eps] + R_GT + R_XB + [R_bsel, R_IDXW] + R_SLOT + R_GK),
                               ("Epe", P.cnt["pe"]), ("Eact", P.cnt["act"]), ("Epool", P.cnt["pool"])]
                    all_hs = [r for rr in R_hs for r in rr]
                    for t in range(4):
                        pass
                    g1_done += [r.w for r in all_hs]
                with contextlib.ExitStack() as s3:
                    hg_ring = Ring([sb(f"hg{l}{i}", [128, 4, D], BF16, st=s3) for i in range(1)])
                    hgT_ring = Ring([sb(f"hgT{l}{i}", [128, 8, CAP], BF16, st=s3) for i in range(2)])
                    actT_ring = Ring([sb(f"actT{l}{i}", [128, 8, CAP], BF16, st=s3) for i in range(2)])
                    wgu_ring = Ring([XB[:, 0:8, :].bitcast(BF16), XB[:, 8:16, :].bitcast(BF16)])
                    wdn_ring = Ring([sb(f"wdn{l}{i}", [128, 8, D], BF16, st=s3) for i in range(2)])
                    sS_ring = Ring([sb(f"sS{l}{i}", [128, 512], st=s3) for i in range(2)])
                    uS_ring = Ring([sb(f"uS{l}{i}", [128, 512], st=s3) for i in range(2)])
                    ysb_ring = Ring([sb(f"ysb{l}{i}", [128, D], st=s3) for i in range(4)])
                    pTe_ring = Ring([ps(f"pTe{l}{i}", [128, 8, 128], BF16, st=s3) for i in range(2)])
                    psG_ring = Ring([ps(f"psG{l}{i}", [128, 512], st=s3) for i in range(2)])
                    psU_ring = Ring([ps(f"psU{l}{i}", [128, 512], st=s3) for i in range(2)])
                    psY_ring = Ring([ps(f"psY{l}{i}", [128, 512], st=s3) for i in range(2)])
                    R_ys = [Reg() for _ in range(NTILE * 4)]
                    evi = 0
                    wq = 0
                    R_wguk = [[Reg() for _ in range(8)] for _ in range(2)]
                    R_wdnk = [[Reg() for _ in range(8)] for _ in range(2)]
                    for j in range(NTILE):
                        wgu, _ = wgu_ring.next()
                        wdn, _ = wdn_ring.next()
                        R_wgu = R_wguk[j % 2]
                        R_wdn = R_wdnk[j % 2]
                        for k in range(8):
                            P.dmaf("pool", lambda e_, j=j, k=k, wgu=wgu: e_.indirect_dma_start(
                                out=wgu[:, k, :], out_offset=None, in_=wgu_rows[:, :],
                                in_offset=bass.IndirectOffsetOnAxis(ap=IDXW[:, j, k:k + 1], axis=0), bounds_check=bnd_reg(e_, "w1"), oob_is_err=False),
                                f"wg{wq % 16}", reads=[R_IDXW], writes=[R_wgu[k]], extra=g1_done)
                            wq += 1
                        for k in range(8):
                            P.dmaf("pool", lambda e_, j=j, k=k, wdn=wdn: e_.indirect_dma_start(
                                out=wdn[:, k, :], out_offset=None, in_=wdn_rows[:, :],
                                in_offset=bass.IndirectOffsetOnAxis(ap=IDXW[:, j, k:k + 1], axis=0), bounds_check=bnd_reg(e_, "w1"), oob_is_err=False),
                                f"wg{wq % 16}", reads=[R_IDXW], writes=[R_wdn[k]], extra=g1_done)
                            wq += 1
                        hg, R_hg = hg_ring.bufs[0], hg_ring.regs[0]
                        if j == 0:
                            P.dma("sp", hg[:], hs[0:CAP, :].rearrange("(s p) d -> p s d", p=128), "hg0", reads=all_hs, writes=[R_hg], extra=g1_done)
                        hgT, R_hgT = hgT_ring.next()
                        for s_ in range(4):
                            pTe, R_pTe = pTe_ring.next()
                            P.op("pe", [tp(pTe[:, k, :], hg[:, s_, k * 128:(k + 1) * 128], identb) for k in range(8)], reads=[R_hg, R_cstb], writes=[R_pTe])
                            if s_ % 2 == 0:
                                P.op("dve", cp(hgT[:, :, s_ * 128:(s_ + 1) * 128], pTe[:]), reads=[R_pTe], writes=[R_hgT])
                            else:
                                P.op("act", actf(hgT[:, :, s_ * 128:(s_ + 1) * 128], pTe[:], AF.Copy), reads=[R_pTe], writes=[R_hgT])
                        if j + 1 < NTILE:
                            P.dma("sp", hg[:], hs[(j + 1) * CAP:(j + 2) * CAP, :].rearrange("(s p) d -> p s d", p=128), "hg0", reads=all_hs, writes=[R_hg], extra=g1_done)
                        actT, R_actT = actT_ring.next()
                        for fc in range(8):
                            psG, R_psG = psG_ring.next()
                            psU, R_psU = psU_ring.next()
                            P.op("pe", [mm(psG[:, :], wgu[:, k, fc * 128:(fc + 1) * 128], hgT[:, k, :], k == 0, k == 7) for k in range(8)],
                                 reads=R_wgu + [R_hgT], writes=[R_psG])
                            P.op("pe", [mm(psU[:, :], wgu[:, k, D + fc * 128:D + (fc + 1) * 128], hgT[:, k, :], k == 0, k == 7) for k in range(8)],
                                 reads=R_wgu + [R_hgT], writes=[R_psU])
                            sS, R_sS = sS_ring.next()
                            uS, R_uS = uS_ring.next()
                            P.op("act", actf(sS[:], psG[:], AF.Silu, bias=bgsel[:, j, fc:fc + 1], scale=ALPHA), reads=[R_psG, R_bsel], writes=[R_sS])
                            P.op("act", actf(uS[:], psU[:], AF.Identity, bias=bsel[:, j, 8 + fc:9 + fc]), reads=[R_psU, R_bsel], writes=[R_uS])
                            P.op("dve", ts(sS[:], sS[:], C7, 1.0 / ALPHA, ALU.min, ALU.mult), reads=[R_sS], writes=[R_sS])
                            P.op("dve", ts(uS[:], uS[:], LIM, -LIM, ALU.min, ALU.max), reads=[R_uS], writes=[R_uS])
                            P.op("dve", stt(actT[:, fc, :], uS[:], 1.0, sS[:], ALU.add, ALU.mult), reads=[R_uS, R_sS], writes=[R_actT])
                        for s_ in range(4):
                            ysb, R_ysb = ysb_ring.next()
                            for half in range(2):
                                pY, R_pY = psY_ring.next()
                                P.op("pe", [mm(pY[:, :], actT[:, fc, s_ * 128:(s_ + 1) * 128], wdn[:, fc, half * 512:(half + 1) * 512], fc == 0, fc == 7) for fc in range(8)],
                                     reads=R_wdn + [R_actT], writes=[R_pY])
                                if evi % 2 == 0:
                                    P.op("act", actf(ysb[:, half * 512:(half + 1) * 512], pY[:, :], AF.Copy), reads=[R_pY], writes=[R_ysb])
                                else:
                                    P.op("dve", cp(ysb[:, half * 512:(half + 1) * 512], pY[:, :]), reads=[R_pY], writes=[R_ysb])
                                evi += 1
                            P.dma("sp", ys[j * CAP + s_ * 128: j * CAP + (s_ + 1) * 128, :], ysb[:], f"ysb{(ysb_ring.i - 1) % 4}", reads=[R_ysb], writes=[R_ys[j * 4 + s_]])
                    e_done0 = [("Epe", P.cnt["pe"]), ("Eact", P.cnt["act"]), ("Edve", P.cnt["dve"])] + [r.w for r in R_ys]
                    for t in range(NT):
                        for half in range(2):
                            pY, R_pY = psY_ring.next()
                            P.op("pe", mm(pY[:, :], GT[0:E, t * 128:(t + 1) * 128], bd_sb[0:E, half * 512:(half + 1) * 512]),
                                 reads=[R_GT[t], R_bd], writes=[R_pY], extra=e_done0)
                            P.op("act", actf(XB[:, t, half * 512:(half + 1) * 512], pY[:, :], AF.Copy), reads=[R_pY], writes=[R_XB[t]], extra=e_done0)
                    e_done = [("Epe", P.cnt["pe"]), ("Eact", P.cnt["act"]), ("Edve", P.cnt["dve"])] + [r.w for r in R_ys]
                with contextlib.ExitStack() as s5:
                    yg_ring = Ring([sb(f"yg{l}{i}", [128, D], st=s5) for i in range(4)])
                    for yg, R_yg in zip(yg_ring.bufs, yg_ring.regs):
                        P.op("dve", lambda e, yg=yg: e.memset(yg[:], 0.0), writes=[R_yg], extra=e_done)
                    for t in range(NT):
                        for k in range(4):
                            yg, R_yg = yg_ring.next()
                            P.dmaf("pool", lambda e_, t=t, k=k, yg=yg: e_.indirect_dma_start(
                                out=yg[:, :], out_offset=None, in_=ys[:, :],
                                in_offset=bass.IndirectOffsetOnAxis(ap=SLOT[:, t, k:k + 1], axis=0), bounds_check=bnd_reg(e_), oob_is_err=False),
                                f"yg{(yg_ring.i - 1) % 4}", reads=R_ys + [R_SLOT[t]], writes=[R_yg], extra=e_done)
                            P.op("dve", stt(XB[:, t, :], yg[:], GK[:, t, k:k + 1], XB[:, t, :], ALU.mult, ALU.add), reads=[R_yg, R_GK[t], R_XB[t]], writes=[R_XB[t]])
                    g3_done = [("Epe", P.cnt["pe"]), ("Eact", P.cnt["act"]), ("Edve", P.cnt["dve"]), ("Epool", P.cnt["pool"])] + [r.w for r in yg_ring.regs]
                with contextlib.ExitStack() as s4:
                    gfb = sb(f"gfb{l}", [128, D], st=s4)
                    R_gfb = Reg()
                    P.dma("sp", gfb[:], bc(mv(l, 0, 5)), "c0", reads=[R_mvec[l]], writes=[R_gfb], extra=g3_done)
                    xt_ring = Ring([sb(f"mxt{l}{i}", [128, D], st=s4) for i in range(2)])
                    for t in range(NT):
                        xt, R_xt = xt_ring.next()
                        P.dma("sp", xt[:], xspill[t * 128:(t + 1) * 128, :], f"mxt{t % 2}", reads=[R_xspill], writes=[R_xt], extra=g3_done)
                        P.op("dve", tt(XB[:, t, :], XB[:, t, :], gfb[:], ALU.mult), reads=[R_XB[t], R_gfb], writes=[R_XB[t]])
                        P.op("dve", tt(XB[:, t, :], XB[:, t, :], xt[:], ALU.add), reads=[R_XB[t], R_xt], writes=[R_XB[t]])
                    return [P.op("dve", cp(dmy[:, 0:1], epsc[:, 0:1]), writes=[R_dmy], reads=[R_eps] + R_XB + xt_ring.regs),
                            ("Epe", P.cnt["pe"]), ("Eact", P.cnt["act"]), ("Edve", P.cnt["dve"])]

        moe0_done = (moe_sparse if SPARSE else moe)(0)

        with contextlib.ExitStack() as sh:
            hP = sb("hP", [128, NT, D], BF16, st=sh)
            R_hP = [Reg() for _ in range(NT)]
            bandsb = sb("bandsb", [128, 36, 128], BF16, st=sh)
            R_bands = Reg()
            P.dma("sp", bandsb[:], bands, "c0", writes=[R_bands], extra=moe0_done)
            A1b = sb("A1b", [128, D], st=sh)
            B1b = sb("B1b", [128, D], st=sh)
            tmpb = sb("tmpb", [128, D], st=sh)
            gpb = sb("gpb", [128, D], st=sh)
            R_A1b, R_B1b, R_tmpb, R_gpb = Reg(), Reg(), Reg(), Reg()
            P.dma("sp", A1b[:], bc(mv(1, 0, 1)), "c0", reads=[R_mvec[1]], writes=[R_A1b], extra=moe0_done)
            P.dma("sp", tmpb[:], bc(norm_mix[1]), "c0", writes=[R_tmpb], extra=moe0_done)
            P.op("dve", stt(A1b[:], A1b[:], 1.0, tmpb[:], ALU.add, ALU.mult), reads=[R_A1b, R_tmpb], writes=[R_A1b])
            P.dma("sp", B1b[:], bc(mv(1, 0, 0)), "c0", reads=[R_mvec[1]], writes=[R_B1b], extra=moe0_done)
            P.dma("sp", gpb[:], bc(mv(1, 0, 2)), "c0", reads=[R_mvec[1]], writes=[R_gpb], extra=moe0_done)
            P.dma("sp", tmpb[:], bc(pool_scale), "c0", writes=[R_tmpb], reads=[R_tmpb])
            P.op("dve", tt(gpb[:], gpb[:], tmpb[:], ALU.mult), reads=[R_gpb, R_tmpb], writes=[R_gpb])
            Wpb = sb("Wpb", [128, 4, 2, 256], BF16, st=sh)
            R_Wpb = Reg()
            pws = sb("pws", [128, 4, 2, 256], st=sh)
            R_pws = Reg()
            for gi in range(4):
                P.dma("sp", pws[:, gi, :, :], pool_w[gi].rearrange("(c p) n -> p c n", p=128), "c0", writes=[R_pws], extra=moe0_done)
            for gi in range(4):
                for cc in range(2):
                    P.op("dve", tt(Wpb[:, gi, cc, :], pws[:, gi, cc, :], gpb[:, gi * 256:(gi + 1) * 256], ALU.mult), reads=[R_pws, R_gpb], writes=[R_Wpb])
            st_ring = Ring([sb(f"pst{i}", [128, 2], st=sh) for i in range(2)])
            junk = sb("pjunk", [128, D], BF16, st=sh)
            R_junk = Reg()
            for t in range(NT):
                stt_, R_st = st_ring.next()
                rstd_tile(XB[:, t, :], junk[:], R_junk, stt_, R_st, [R_XB[t]])
                P.op("dve", stt(tmpb[:], XB[:, t, :], stt_[:, 1:2], A1b[:], ALU.mult, ALU.mult), reads=[R_XB[t], R_st, R_A1b, R_tmpb], writes=[R_tmpb])
                P.op("dve", tt(hP[:, t, :], tmpb[:], B1b[:], ALU.add), reads=[R_tmpb, R_B1b], writes=[R_hP[t]])
                if t == 16:
                    P.op("dve", ts(hP[:, t, :], hP[:, t, :], hm[:, 0:1], None, ALU.mult), reads=[R_hP[t], R_hm], writes=[R_hP[t]])
            psP_ring = Ring([ps(f"psP{i}", [128, 8, 128], st=sh) for i in range(2)])
            psY2_ring = Ring([ps(f"psY2{i}", [128, D], st=sh) for i in range(2)])
            pT_ring2 = Ring([sb(f"plT{i}", [128, 8, 128], BF16, st=sh) for i in range(2)])
            for t in range(16):
                if t == 0:
                    srcs = [(16, 7), (0, 3), (1, 4)]
                elif t == 15:
                    srcs = [(14, 5), (15, 6), (16, 8)]
                else:
                    srcs = [(t - 1, 0), (t, 1), (t + 1, 2)]
                psP, R_psP = psP_ring.next()
                fns = []
                for dc in range(8):
                    gi = dc // 2
                    for si, (st_, kind) in enumerate(srcs):
                        fns.append(mm(psP[:, dc, :], hP[:, st_, dc * 128:(dc + 1) * 128], bandsb[:, gi * 9 + kind, :], si == 0, si == 2))
                P.op("pe", fns, reads=[R_hP[s_] for s_, _ in srcs] + [R_bands], writes=[R_psP])
                plT, R_plT = pT_ring2.next()
                P.op("act", actf(plT[:], psP[:], AF.Copy), reads=[R_psP], writes=[R_plT])
                psY2, R_psY2 = psY2_ring.next()
                fns = []
                for gi in range(4):
                    for cc in range(2):
                        fns.append(mm(psY2[:, gi * 256:(gi + 1) * 256], plT[:, gi * 2 + cc, :], Wpb[:, gi, cc, :], cc == 0, cc == 1))
                P.op("pe", fns, reads=[R_plT, R_Wpb], writes=[R_psY2])
                for half in range(2):
                    P.op("dve", tt(XB[:, t, half * 512:(half + 1) * 512], XB[:, t, half * 512:(half + 1) * 512], psY2[:, half * 512:(half + 1) * 512], ALU.add),
                         reads=[R_psY2, R_XB[t]], writes=[R_XB[t]])
            for t in range(NT):
                P.dma("sp", xspill[t * 128:(t + 1) * 128, :], XB[:, t, :], f"xb{t % 4}", reads=[R_XB[t]], writes=[R_xspill])
            pool_done = [P.op("dve", cp(dmy[:, 0:1], epsc[:, 0:1]), writes=[R_dmy], reads=[R_eps] + R_XB + R_hP),
                         ("Epe", P.cnt["pe"]), ("Eact", P.cnt["act"]), R_xspill.w]
        for t in range(4):
            pool_done.append(("Dxb%d" % t, P.dma_sems["xb%d" % t]))
        P.wait("pool", pool_done)
        P.wait("pe", pool_done)
        P.wait("act", pool_done)
        P.wait("dve", pool_done)
        P.wait("sp", pool_done)

        moe1_done = (moe_sparse if SPARSE else moe)(1)

        with contextlib.ExitStack() as sj:
            fnb = sb("fnb", [128, D], st=sj)
            R_fnb = Reg()
            P.dma("sp", fnb[:], bc(final_norm), "c0", writes=[R_fnb], extra=moe1_done)
            st_ring = Ring([sb(f"jst{i}", [128, 2], st=sj) for i in range(2)])
            junk = sb("jjunk", [128, D], BF16, st=sj)
            R_junk = Reg()
            ot_ring = Ring([sb(f"ot{i}", [128, D], st=sj) for i in range(2)])
            outs = []
            for t in range(16):
                stt_, R_st = st_ring.next()
                rstd_tile(XB[:, t, :], junk[:], R_junk, stt_, R_st, [R_XB[t]])
                ot, R_ot = ot_ring.next()
                P.op("dve", stt(ot[:], XB[:, t, :], stt_[:, 1:2], fnb[:], ALU.mult, ALU.mult), reads=[R_XB[t], R_st, R_fnb], writes=[R_ot])
                outs.append(P.dma("sp", out[t * 128:(t + 1) * 128, :], ot[:], f"ot{t % 2}", reads=[R_ot]))
            P.wait("sp", outs[-2:])
        P.emit()
    return nc


def _rope_tables(pos_row, pos_col):
    half = 32
    inv = (np.float32(10000.0) ** (-(np.arange(half, dtype=np.float32) / np.float32(half)))).astype(np.float32)
    ang_r = pos_row.astype(np.float32)[None, :] * inv[:, None]
    ang_c = pos_col.astype(np.float32)[None, :] * inv[:, None]
    cos = np.concatenate([np.cos(ang_r), np.cos(ang_r), np.cos(ang_c), np.cos(ang_c)], 0).astype(np.float32)
    sin = np.concatenate([np.sin(ang_r), np.sin(ang_r), np.sin(ang_c), np.sin(ang_c)], 0).astype(np.float32)
    return np.stack([cos, sin], 0)


def _coef(s, t, w):
    lo = np.clip(t - w // 2, 0, S)
    hi = np.clip(t - w // 2 + w, 0, S)
    cnt = (hi - lo).astype(np.float32)
    inside = (s >= lo) & (s < hi) & (s >= 0) & (s < S)
    return np.where(inside, np.float32(1.0) / cnt, np.float32(0.0)).astype(np.float32) - (s == t).astype(np.float32)


def _bands(c):
    base = OWN * c
    out = np.zeros((128, 36, 128), np.float32)
    i = np.arange(128)[:, None]
    j = np.arange(128)[None, :]
    for gi, w in enumerate((2, 4, 8, 16)):
        gb = OWN * 3 + 128 * 5
        def blk(src0, dst0):
            return _coef(src0 + i + 0 * j, dst0 + j + 0 * i, w)
        kinds = [
            blk(gb - 128, gb), blk(gb, gb), blk(gb + 128, gb),
            blk(base, base), blk(base + 128, base),
            blk(base + 128 * 14, base + 128 * 15), blk(base + 128 * 15, base + 128 * 15),
        ]
        hl = np.zeros((128, 128), np.float32)
        hl[0:8] = _coef(base - 8 + np.arange(8)[:, None] + 0 * j, base + j + 0 * np.arange(8)[:, None], w)
        hr = np.zeros((128, 128), np.float32)
        hr[8:16] = _coef(base + OWN + np.arange(8)[:, None] + 0 * j, base + 128 * 15 + j + 0 * np.arange(8)[:, None], w)
        kinds += [hl, hr]
        for k, m in enumerate(kinds):
            out[:, gi * 9 + k, :] = m
    return out.astype(ml_dtypes.bfloat16)


def _consts():
    cs = np.zeros((7, 128, 128), np.float32)
    cs[5, :, 40:48] = (np.arange(8)[None, :] * 128 + np.arange(128)[:, None]).astype(np.float32)
    cs[4] = (np.arange(128)[:, None] < np.arange(128)[None, :]).astype(np.float32)
    cs[5, :, 0:32] = np.arange(32, dtype=np.float32)[None, :]
    cs[5, :16, 32] = 1.0
    cs[5, 16:, 33] = 1.0e6
    cs[0] = np.eye(128, dtype=np.float32)
    cs[1] = 1.0 / 128.0
    rot = np.zeros((128, 128), np.float32)
    for m in range(128):
        if (m % 64) < 32:
            rot[m + 32, m] = -1.0
        else:
            rot[m - 32, m] = 1.0
    cs[2] = rot
    cs[3] = 1.0
    return cs


def prep(inputs):
    f = lambda a: np.ascontiguousarray(np.asarray(a, dtype=np.float32))
    x = f(inputs["x"])[0]
    ctx = f(inputs["ctx"])[0]
    tpos = np.arange(S)
    ropek = _rope_tables(tpos // 64, tpos % 64)
    shared = {
        "x_all": x, "ctx": ctx,
        "c2": np.stack([f(inputs["c"])[0], f(inputs["c_ctx"])], 0),
        "ada_w": f(inputs["ada_w"]), "ada_b": f(inputs["ada_b"]),
        "norm_mix": f(inputs["norm_mix"]), "norm_ffn": f(inputs["norm_ffn"]),
        "wqkv": f(inputs["attn_w_qkv"])[0],
        "qk_g": np.stack([f(inputs["attn_q_norm"])[0], f(inputs["attn_k_norm"])[0]], 0),
        "wo": f(inputs["attn_w_o"])[0], "pool_w": f(inputs["pool_w"])[0], "pool_scale": f(inputs["pool_scale"])[0],
        "router_w": f(inputs["moe_router_w"]), "router_b": f(inputs["moe_router_b"]),
        "w_gu": f(inputs["moe_w_gu"]), "b_gu": f(inputs["moe_b_gu"]),
        "w_down": f(inputs["moe_w_down"]), "b_down": f(inputs["moe_b_down"]),
        "final_norm": f(inputs["final_norm"]), "ropek": ropek, "consts": _consts(),
    }
    maps = []
    for c in range(NCORES):
        base = OWN * c
        idx = np.zeros(NEXT, np.int64)
        idx[:OWN] = base + np.arange(OWN)
        idx[OWN:OWN + 8] = base - 8 + np.arange(8) if c > 0 else base + np.arange(8)
        idx[OWN + 8:OWN + 16] = base + OWN + np.arange(8) if c < NCORES - 1 else base + np.arange(8)
        idx[OWN + 16:] = base
        x_ext = x[idx].copy()
        x_ext[OWN + 16:] = 0.0
        hmask = np.zeros((128, 1), np.float32)
        if c > 0:
            hmask[0:8] = 1.0
        if c < NCORES - 1:
            hmask[8:16] = 1.0
        m = dict(shared)
        m.update({"x_ext": x_ext, "ropeq": np.ascontiguousarray(ropek[:, :, idx]), "bands": _bands(c), "hmask": hmask})
        maps.append(m)
    return maps


_NC_CACHE = {}


def kernel(**inputs):
    maps = prep(inputs)
    if "nc" not in _NC_CACHE:
        _NC_CACHE["nc"] = build()
    res = run_bass_kernel_spmd(_NC_CACHE["nc"], maps, core_ids=list(range(NCORES)))
    return np.concatenate([r["out"] for r in res.results], axis=0)[None].astype(np.float32)
```

```python
import contextlib
import math
import numpy as np
import ml_dtypes
import concourse.bass as bass
import concourse.mybir as mybir
from concourse.bass_utils import run_bass_kernel_spmd

F32 = mybir.dt.float32
BF16 = mybir.dt.bfloat16
ALU = mybir.AluOpType
AF = mybir.ActivationFunctionType

NCORES = 8
D = 1024
S = 16384
CTX = 256
OWN = S // NCORES
NT = 17
NEXT = NT * 128
NKT = (S + CTX) // 128
E = 32
EPS = 1e-6
ALPHA = 1.702
LIM = 7.0
ENGS = ("pe", "act", "dve", "pool", "sp")
SPARSE = True
CAP = 512
NTILE = 48
NSLOT = NTILE * CAP
BLKS = [(0, 512), (512, 512), (1024, 512), (1536, 512), (2048, 16)]


class Reg:
    __slots__ = ("w", "r")

    def __init__(self):
        self.w = None
        self.r = {}


class Prog:
    def __init__(self, nc):
        self.nc = nc
        self.q = {e: [] for e in ENGS}
        self.cnt = {e: 0 for e in ENGS}
        self.dma_sems = {}
        self.sem_handles = {}

    def _deps(self, eng, reads, writes):
        deps = {}

        def add(t):
            if t is None:
                return
            k, v = t
            if eng == "pe" and k == "Epe":
                return
            if deps.get(k, 0) < v:
                deps[k] = v
        for x in reads:
            add(x.w)
        for x in writes:
            add(x.w)
            for k, v in x.r.items():
                add((k, v))
        return list(deps.items())

    def _mark(self, t, reads, writes):
        k, v = t
        for x in reads:
            if x.r.get(k, 0) < v:
                x.r[k] = v
        for x in writes:
            x.w = t
            x.r = {}

    def op(self, eng, fns, reads=(), writes=(), extra=()):
        if callable(fns):
            fns = [fns]
        deps = self._deps(eng, reads, writes) + [d for d in extra if d is not None]
        self.cnt[eng] += 1
        t = ("E" + eng, self.cnt[eng])
        self.q[eng].append(("op", fns, deps, t))
        self._mark(t, reads, writes)
        return t

    def dma(self, eng, out, in_, sem, reads=(), writes=(), extra=(), **kw):
        if sem in ("c0", "c1", "dbg"):
            self.auto_i = getattr(self, "auto_i", 0) + 1
            sem = f"a{self.auto_i % 24}"
        deps = self._deps(eng, reads, writes) + [d for d in extra if d is not None]
        if sem in self.dma_sems:
            deps.append(("D" + sem, self.dma_sems[sem]))
        self.dma_sems[sem] = self.dma_sems.get(sem, 0) + 16
        t = ("D" + sem, self.dma_sems[sem])
        self.q[eng].append(("dma", (out, in_, kw), deps, t))
        self._mark(t, reads, writes)
        return t

    def dmaf(self, eng, fn, sem, reads=(), writes=(), extra=()):
        deps = self._deps(eng, reads, writes) + [d for d in extra if d is not None]
        if sem in self.dma_sems:
            deps.append(("D" + sem, self.dma_sems[sem]))
        self.dma_sems[sem] = self.dma_sems.get(sem, 0) + 16
        t = ("D" + sem, self.dma_sems[sem])
        self.q[eng].append(("dmaf", fn, deps, t))
        self._mark(t, reads, writes)
        return t

    def wait(self, eng, deps):
        self.q[eng].append(("wait", None, [d for d in deps if d is not None], None))

    def emit(self):
        nc = self.nc
        keys = ["E" + e for e in ENGS] + ["D" + s for s in self.dma_sems]
        with contextlib.ExitStack() as st:
            for k in keys:
                self.sem_handles[k] = st.enter_context(nc.semaphore(k))
            blk = st.enter_context(nc.Block())
            engmap = {"pe": blk.tensor, "act": blk.scalar, "dve": blk.vector,
                      "pool": blk.gpsimd, "sp": blk.sync}
            sh = self.sem_handles
            for e in ENGS:
                items = self.q[e]

                def body(engine, items=items):
                    waited = {}
                    for kind, payload, deps, t in items:
                        for (k, v) in deps:
                            if waited.get(k, 0) >= v:
                                continue
                            engine.wait_ge(sh[k], v)
                            waited[k] = v
                        if kind == "wait":
                            continue
                        if kind == "op":
                            ins = None
                            for f in payload:
                                ins = f(engine)
                            ins.then_inc(sh[t[0]], 1)
                        elif kind == "dmaf":
                            payload(engine).then_inc(sh[t[0]], 16)
                        else:
                            out, in_, kw = payload
                            engine.dma_start(out=out, in_=in_, **kw).then_inc(sh[t[0]], 16)
                engmap[e](body)


def mm(out, lhsT, rhs, start=True, stop=True):
    return lambda e: e.matmul(out, lhsT, rhs, start=start, stop=stop)


def tp(out, in_, ident):
    return lambda e: e.transpose(out, in_, ident)


def actf(out, in_, func, bias=None, scale=None, accum=None):
    def f(e):
        kw = {}
        if bias is not None:
            kw["bias"] = bias
        if scale is not None:
            kw["scale"] = scale
        if accum is not None:
            kw["accum_out"] = accum
        return e.activation(out, in_, func, **kw)
    return f


def ts(out, in0, s1, s2, op0, op1=None):
    if op1 is None:
        return lambda e: e.tensor_scalar(out, in0, s1, None, op0)
    return lambda e: e.tensor_scalar(out, in0, s1, s2, op0, op1)


def tt(out, in0, in1, op):
    return lambda e: e.tensor_tensor(out, in0, in1, op)


def stt(out, in0, scalar, in1, op0, op1, accum=None):
    if accum is None:
        return lambda e: e.scalar_tensor_tensor(out, in0, scalar, in1, op0, op1)
    return lambda e: e.scalar_tensor_tensor(out, in0, scalar, in1, op0, op1, accum_out=accum)


def cp(out, in_):
    return lambda e: e.tensor_copy(out, in_)


class Ring:
    def __init__(self, bufs):
        self.bufs = bufs
        self.regs = [Reg() for _ in bufs]
        self.i = 0

    def next(self):
        s = self.i % len(self.bufs)
        self.i += 1
        return self.bufs[s], self.regs[s]


def build(dbg=None):
    nc = bass.Bass("TRN2", target_bir_lowering=False)
    P = Prog(nc)

    def din(name, shape, dt=F32):
        return nc.dram_tensor(name, list(shape), dt, kind="ExternalInput").ap()

    x_all = din("x_all", [S, D])
    x_ext = din("x_ext", [NEXT, D])
    ctx_in = din("ctx", [CTX, D])
    c2 = din("c2", [2, D])
    ada_w = din("ada_w", [2, D, 6 * D])
    ada_b = din("ada_b", [2, 6 * D])
    norm_mix = din("norm_mix", [2, D])
    norm_ffn = din("norm_ffn", [2, D])
    wqkv = din("wqkv", [D, 1536])
    qk_g = din("qk_g", [2, 128])
    wo = din("wo", [D, D])
    pool_w = din("pool_w", [4, 256, 256])
    pool_scale = din("pool_scale", [D])
    router_w = din("router_w", [2, D, E])
    router_b = din("router_b", [2, E])
    w_gu = din("w_gu", [2, E, D, 2 * D])
    b_gu = din("b_gu", [2, E, 2 * D])
    w_down = din("w_down", [2, E, D, D])
    b_down = din("b_down", [2, E, D])
    final_norm = din("final_norm", [D])
    ropek = din("ropek", [2, 128, S])
    ropeq = din("ropeq", [2, 128, NEXT])
    consts = din("consts", [7, 128, 128])
    bands = din("bands", [128, 36, 128], BF16)
    hmask = din("hmask", [128, 1])
    out = nc.dram_tensor("out", [OWN, D], F32, kind="ExternalOutput").ap()
    dbg_out = None
    if dbg is not None:
        dbg_out = nc.dram_tensor("dbg", list(dbg[1]), F32, kind="ExternalOutput").ap()

    mvec = nc.dram_tensor("mvec", [2, 2, 6 * D], F32).ap()
    bvec = nc.dram_tensor("bvec", [2, 1536], F32).ap()
    xspill = nc.dram_tensor("xspill", [NEXT, D], F32).ap()
    R_mvec = [Reg(), Reg()]
    R_bvec = Reg()
    R_xspill = Reg()

    def col(ap1d, n=8):
        return ap1d.rearrange("(k p) -> p k", p=128)

    def bc(ap1d, parts=128):
        return ap1d.rearrange("(o n) -> o n", o=1).partition_broadcast(parts)

    NC_KW = dict(allow_slow_non_contiguous=True)
    final_waits = []

    with contextlib.ExitStack() as top:
        def sb(name, shape, dt=F32, st=top):
            return st.enter_context(nc.sbuf_tensor(name, list(shape), dt))

        def ps(name, shape, dt=F32, st=top):
            return st.enter_context(nc.psum_tensor(name, list(shape), dt))

        cst = sb("cst", [128, 7, 128])
        R_cst = Reg()
        P.dma("sp", cst[:], consts.rearrange("c p n -> p c n"), "c0", writes=[R_cst])
        identf = cst[:, 0, :]
        cstb = sb("cstb", [128, 7, 128], BF16)
        R_cstb = Reg()
        P.op("dve", cp(cstb[:], cst[:]), reads=[R_cst], writes=[R_cstb])
        identb = cstb[:, 0, :]
        onesmb = cstb[:, 1, :]
        rotb = cstb[:, 2, :]
        onesb = cstb[:, 3, :]
        ustrb = cstb[:, 4, :]
        iota_e = cst[:, 5, 0:32]
        vmask = cst[:, 5, 32:33]
        notvbig = cst[:, 5, 33:34]
        kpc = cst[:, 5, 40:48]
        epsc = sb("epsc", [128, 1])
        R_eps = Reg()
        P.op("pool", lambda e: e.memset(epsc[:], EPS), writes=[R_eps])
        dmy = sb("dmy", [128, 1])
        R_dmy = Reg()
        hm = sb("hm", [128, 1])
        R_hm = Reg()
        P.dma("sp", hm[:], hmask, "c0", writes=[R_hm])

        hs_d = [nc.dram_tensor(f"hs{l}", [NSLOT, D], BF16).ap() for l in range(2)]
        ys_d = [nc.dram_tensor(f"ys{l}", [NSLOT, D], F32).ap() for l in range(2)]
        zt = sb("zt", [128, D], BF16)
        R_zt = Reg()
        P.op("pool", lambda e: e.memset(zt[:], 0.0), writes=[R_zt])
        R_hs0 = [[Reg() for _ in range(NTILE)] for _ in range(2)]

        qT_d = nc.dram_tensor("qT_d", [128, 8, NEXT], BF16).ap()
        R_qTd = [Reg() for _ in BLKS]

        with contextlib.ExitStack() as sa:
            c2col = sb("c2col", [128, 8, 2], st=sa)
            R_c2 = Reg()
            for r in range(2):
                P.dma("sp", c2col[:, :, r], col(c2[r]), "c0", writes=[R_c2], **NC_KW)
            sc2 = sb("sc2", [128, 8, 2], st=sa)
            R_sc2 = Reg()
            P.op("act", actf(sc2[:], c2col[:], AF.Silu), reads=[R_c2], writes=[R_sc2])
            adab = sb("adab", [2, 2, 6 * D], st=sa)
            R_adab = Reg()
            for l in range(2):
                for r in range(2):
                    P.dma("sp", adab[r:r + 1, l, :], ada_b[l].rearrange("(o n) -> o n", o=1), "c0", writes=[R_adab])
            mrow = sb("mrow", [2, 2, 6 * D], st=sa)
            awr = Ring([sb(f"aw{i}", [128, 8, 512], st=sa) for i in range(2)])
            psA = Ring([ps(f"psA{i}", [128, 512], st=sa) for i in range(2)])
            for l in range(2):
                R_m = Reg()
                for nb in range(12):
                    wbuf, wreg = awr.next()
                    P.dma("sp", wbuf[:], ada_w[l][:, nb * 512:(nb + 1) * 512].rearrange("(k p) n -> p k n", p=128),
                          f"aw{(awr.i - 1) % 2}", writes=[wreg])
                    pb, preg = psA.next()
                    P.op("pe", [mm(pb[0:2, :], sc2[:, k, :], wbuf[:, k, :], k == 0, k == 7) for k in range(8)],
                         reads=[R_sc2, wreg], writes=[preg])
                    P.op("dve", tt(mrow[0:2, l, nb * 512:(nb + 1) * 512], pb[0:2, :], adab[0:2, l, nb * 512:(nb + 1) * 512], ALU.add),
                         reads=[preg, R_adab], writes=[R_m])
                P.dma("sp", mvec[l], mrow[0:2, l, :], "c1", reads=[R_m], writes=[R_mvec[l]])

        def mv(l, r, j):
            return mvec[l, r, j * D:(j + 1) * D]

        vecs = sb("vecs", [128, 16, 8])
        R_vecs = Reg()

        def load_cols(idx, ap1d, dep_regs):
            P.dma("sp", vecs[:, idx, :], col(ap1d), "c0", reads=dep_regs, writes=[R_vecs], **NC_KW)

        load_cols(0, norm_mix[0], [])
        load_cols(1, mv(0, 0, 1), [R_mvec[0]])
        load_cols(2, mv(0, 0, 0), [R_mvec[0]])
        load_cols(3, mv(0, 1, 1), [R_mvec[0]])
        load_cols(4, mv(0, 1, 0), [R_mvec[0]])
        load_cols(5, norm_ffn[0], [])
        load_cols(6, mv(0, 0, 4), [R_mvec[0]])
        load_cols(7, mv(0, 0, 3), [R_mvec[0]])
        load_cols(8, norm_ffn[1], [])
        load_cols(9, mv(1, 0, 4), [R_mvec[1]])
        load_cols(10, mv(1, 0, 3), [R_mvec[1]])
        AB = sb("AB", [128, 4, 8])
        R_AB = Reg()
        for i, (scx, nmx) in enumerate([(1, 0), (3, 0), (6, 5), (9, 8)]):
            P.op("dve", stt(AB[:, i, :], vecs[:, scx, :], 1.0, vecs[:, nmx, :], ALU.add, ALU.mult),
                 reads=[R_vecs], writes=[R_AB])

        if dbg is not None and dbg[0] == "A":
            P.dma("sp", dbg_out[:, 0:32].rearrange("p (i k) -> p i k", k=8), AB[:], "dbg", reads=[R_AB])
            final_waits.append(P.dma("sp", dbg_out[:, 32:160].rearrange("p (i k) -> p i k", k=8), vecs[:], "dbg", reads=[R_vecs]))
            P.wait("sp", final_waits)
            P.emit()
            return nc


        with contextlib.ExitStack() as sattn:
            KT = sb("KT", [128, 2, S + CTX], BF16, st=sattn)
            Vs = sb("Vs", [128, NKT, 256], BF16, st=sattn)
            R_KT = [[Reg() for _ in range(33)] for _ in range(2)]
            R_V = [Reg() for _ in range(NKT)]
            R_Wl, R_Wc = Reg(), Reg()
            bcol = sb("bcol", [128, 12], st=sattn)
            bvb = sb("bvb", [128, 2, 256], st=sattn)
            gcol = sb("gcol", [128, 2], st=sattn)
            negB = sb("negB", [128, 1], st=sattn)
            sw = contextlib.ExitStack()
            Wkv = sb("Wkv", [128, 8, 512], BF16, st=sw)
            Wc = sb("Wc", [128, 8, 512], BF16, st=sw)
            sq_ = contextlib.ExitStack()
            Wq = sb("Wq", [128, 8, 1024], BF16, st=sq_)
            R_bcol, R_bvb, R_gcol, R_negB = Reg(), Reg(), Reg(), Reg()

            with contextlib.ExitStack() as sbp:
                wst_ring = Ring([sb(f"wst{i}", [128, 8, 512], st=sbp) for i in range(1)])
                Bc2 = sb("Bc2", [128, 8, 2], st=sbp)
                R_Bc2 = Reg()
                P.op("dve", cp(Bc2[:, :, 0], vecs[:, 2, :]), reads=[R_vecs], writes=[R_Bc2])
                P.op("dve", cp(Bc2[:, :, 1], vecs[:, 4, :]), reads=[R_vecs], writes=[R_Bc2])
                brow = sb("brow", [2, 1536], st=sbp)
                R_brow = Reg()
                psB = ps("psB", [128, 512], st=sbp)
                R_psB = Reg()
                for nb in range(3):
                    wst, R_wst = wst_ring.next()
                    P.dma("sp", wst[:], wqkv[:, nb * 512:(nb + 1) * 512].rearrange("(k p) n -> p k n", p=128), "wst0", writes=[R_wst])
                    P.op("pe", [mm(psB[0:2, :], Bc2[:, k, :], wst[:, k, :], k == 0, k == 7) for k in range(8)],
                         reads=[R_Bc2, R_wst], writes=[R_psB])
                    P.op("dve", cp(brow[0:2, nb * 512:(nb + 1) * 512], psB[0:2, :]), reads=[R_psB], writes=[R_brow])
                    for k in range(8):
                        P.op("dve", ts(Wq[:, k, nb * 512:(nb + 1) * 512] if nb < 2 else Wkv[:, k, :], wst[:, k, :], AB[:, 0, k:k + 1], None, ALU.mult), reads=[R_wst, R_AB], writes=[R_Wl])
                        if nb == 2:
                            P.op("dve", ts(Wc[:, k, :], wst[:, k, :], AB[:, 1, k:k + 1], None, ALU.mult), reads=[R_wst, R_AB], writes=[R_Wc])
                P.dma("sp", bvec, brow[:], "c1", reads=[R_brow], writes=[R_bvec])
                P.dma("sp", bcol[:, 0:10], col(bvec[0, 0:1280], 10), "c0", reads=[R_bvec], writes=[R_bcol], **NC_KW)
                P.dma("sp", bcol[:, 10:12], col(bvec[1, 1024:1280], 2), "c0", reads=[R_bvec], writes=[R_bcol], **NC_KW)
                P.dma("sp", bvb[:, 0, :], bc(bvec[0, 1280:1536]), "c0", reads=[R_bvec], writes=[R_bvb])
                P.dma("sp", bvb[:, 1, :], bc(bvec[1, 1280:1536]), "c0", reads=[R_bvec], writes=[R_bvb])
                P.dma("sp", gcol[:], qk_g.rearrange("r p -> p r"), "c0", writes=[R_gcol], **NC_KW)
                gb = sb("gb", [128, 2, 128], st=sbp)
                R_gb = Reg()
                for r in range(2):
                    P.dma("sp", gb[:, r, :], bc(qk_g[r]), "c0", writes=[R_gb])
                gb2 = sb("gb2", [128, 2, 128], st=sbp)
                P.op("dve", stt(gb2[:].rearrange("p a b -> p (a b)"), gb[:].rearrange("p a b -> p (a b)"), -1.0, gb[:].rearrange("p a b -> p (a b)"), ALU.mult, ALU.max), reads=[R_gb], writes=[R_gb])
                mx = sb("mx", [128, 2], st=sbp)
                R_mx = Reg()
                for r in range(2):
                    P.op("dve", lambda e, r=r: e.reduce_max(mx[:, r:r + 1], gb2[:, r, :], mybir.AxisListType.X), reads=[R_gb], writes=[R_mx])
                P.op("dve", stt(negB[:], mx[:, 0:1], -math.sqrt(128.0), mx[:, 1:2], ALU.mult, ALU.mult), reads=[R_mx], writes=[R_negB])
                phaseB_done = [R_Wl.w, R_Wc.w, R_negB.w, R_bvec.w]

            if dbg is not None and dbg[0] == "B":
                t = P.dma("sp", dbg_out[:, 0:12], bcol[:], "dbg", reads=[R_bcol])
                t2 = P.dma("sp", dbg_out[:, 12:13], negB[:], "dbg", reads=[R_negB], **NC_KW)
                t3 = P.dma("sp", dbg_out[:, 16:528], bvb[:].rearrange("p a b -> p (a b)"), "dbg", reads=[R_bvb])
                P.wait("sp", [t, t2, t3])
                P.emit()
                return nc

            for phase_ in ("D", "C"):
                deep = phase_ == "C"
                with contextlib.ExitStack() as scd:
                    xs_ring = Ring([sb(f"xs{phase_}{i}", [128, D], st=scd) for i in range(3 if deep else 2)])
                    xn_ring = Ring([sb(f"xn{phase_}{i}", [128, D], BF16, st=scd) for i in range(2)])
                    st_ring = Ring([sb(f"st{phase_}{i}", [128, 2], st=scd) for i in range(4 if deep else 3)])
                    pT_ring = Ring([ps(f"pT{phase_}{i}", [128, 8, 128], BF16, st=scd) for i in range(2)])
                    xnT_ring = Ring([sb(f"xnT{phase_}{i}", [128, 8, 512], BF16, st=scd) for i in range(2 if deep else 1)])
                    cs_ring = Ring([sb(f"cs{phase_}{i}", [128, 2, 512], st=scd) for i in range(1 if deep else 1)])
                    psK_ring = Ring([ps(f"psK{phase_}{i}", [128, 512], st=scd) for i in range(2)])
                    psVb = [ps(f"psV{phase_}{i}", [128, 512], st=scd) for i in range(2)]
                    R_psV = [Reg(), Reg()]
                    psM = ps("psM" + phase_, [128, 512], st=scd)
                    psR = ps("psR" + phase_, [128, 512], st=scd)
                    R_psM, R_psR = Reg(), Reg()
                    kb_ring = Ring([sb(f"kb{phase_}{i}", [128, 512], st=scd) for i in range(2 if deep else 1)])
                    sq_ring = Ring([sb(f"sq{phase_}{i}", [128, 512], BF16, st=scd) for i in range(1 if deep else 1)])
                    rk_ring = Ring([sb(f"rk{phase_}{i}", [128, 512], st=scd) for i in range(2 if deep else 1)])
                    kn_ring = Ring([sb(f"kn{phase_}{i}", [128, 512], BF16, st=scd) for i in range(1 if deep else 1)])
                    t1_ring = Ring([sb(f"t1{phase_}{i}", [128, 512], st=scd) for i in range(1 if deep else 1)])
                    t2_ring = Ring([sb(f"t2{phase_}{i}", [128, 512], st=scd) for i in range(1 if deep else 1)])
                    xs_cnt = [0]

                    def norm_transpose_tile(src_rows, dst, R_dst, first_extra=()):
                        xs, R_xs = xs_ring.next()
                        P.dma("sp", xs[:], src_rows, f"xs{xs_cnt[0] % 3}", writes=[R_xs], extra=first_extra)
                        xs_cnt[0] += 1
                        xn, R_xn = xn_ring.next()
                        stt_, R_st = st_ring.next()
                        P.op("act", actf(xn[:], xs[:], AF.Square, accum=stt_[:, 0:1]), reads=[R_xs], writes=[R_xn, R_st])
                        P.op("act", actf(stt_[:, 1:2], stt_[:, 0:1], AF.Ln, bias=epsc[:, 0:1], scale=1.0 / D), reads=[R_st, R_eps], writes=[R_st])
                        P.op("act", actf(stt_[:, 1:2], stt_[:, 1:2], AF.Exp, scale=-0.5), reads=[R_st], writes=[R_st])
                        P.op("act", actf(xn[:], xs[:], AF.Copy, scale=stt_[:, 1:2]), reads=[R_xs, R_st], writes=[R_xn])
                        pT, R_pT = pT_ring.next()
                        P.op("pe", [tp(pT[:, k, :], xn[:, k * 128:(k + 1) * 128], identb) for k in range(8)],
                             reads=[R_xn, R_cstb], writes=[R_pT])
                        P.op("dve", cp(dst, pT[:]), reads=[R_pT], writes=[R_dst])

                    def qk_post(psX, R_psX, n, bias_ap, g_ap, cs, R_cs, out_ap, R_out):
                        kb, R_kb = kb_ring.next()
                        sq, R_sq = sq_ring.next()
                        P.op("act", actf(kb[:, :n], psX[:, :n], AF.Identity, bias=bias_ap), reads=[R_psX, R_bcol], writes=[R_kb])
                        P.op("act", actf(sq[:, :n], psX[:, :n], AF.Square, bias=bias_ap), reads=[R_psX, R_bcol], writes=[R_sq])
                        P.op("pe", mm(psM[:, :n], onesmb, sq[:, :n]), reads=[R_sq, R_cstb], writes=[R_psM])
                        rk, R_rk = rk_ring.next()
                        P.op("act", actf(rk[:, :n], psM[:, :n], AF.Ln, bias=epsc[:, 0:1]), reads=[R_psM, R_eps], writes=[R_rk])
                        P.op("act", actf(rk[:, :n], rk[:, :n], AF.Exp, scale=-0.5), reads=[R_rk], writes=[R_rk])
                        if cs is None:
                            P.op("dve", stt(out_ap, kb[:, :n], g_ap, rk[:, :n], ALU.mult, ALU.mult), reads=[R_kb, R_rk, R_gcol], writes=[R_out])
                            return
                        kn, R_kn = kn_ring.next()
                        P.op("dve", stt(kn[:, :n], kb[:, :n], g_ap, rk[:, :n], ALU.mult, ALU.mult), reads=[R_kb, R_rk, R_gcol], writes=[R_kn])
                        P.op("pe", mm(psR[:, :n], rotb, kn[:, :n]), reads=[R_kn, R_cstb], writes=[R_psR])
                        t1, R_t1 = t1_ring.next()
                        t2, R_t2 = t2_ring.next()
                        P.op("pool", tt(t1[:, :n], kn[:, :n], cs[:, 0, :n], ALU.mult), reads=[R_kn, R_cs], writes=[R_t1])
                        P.op("dve", tt(t2[:, :n], psR[:, :n], cs[:, 1, :n], ALU.mult), reads=[R_psR, R_cs], writes=[R_t2])
                        P.op("pool", tt(out_ap, t1[:, :n], t2[:, :n], ALU.add), reads=[R_t1, R_t2], writes=[R_out])

                    if phase_ == "C":
                        nblk_c = 33 if not (dbg and dbg[0] == "Csmall") else 2
                        def c_stage1(b):
                            isctx = b == 32
                            ntile = 2 if isctx else 4
                            xnT, R_xnT = xnT_ring.next()
                            for j in range(ntile):
                                rows = ctx_in[j * 128:(j + 1) * 128, :] if isctx else x_all[b * 512 + j * 128: b * 512 + (j + 1) * 128, :]
                                norm_transpose_tile(rows, xnT[:, :, j * 128:(j + 1) * 128], R_xnT, first_extra=())
                            return xnT, R_xnT

                        c_pend = {0: c_stage1(0)}
                        for b in range(nblk_c):
                            isctx = b == 32
                            ntile = 2 if isctx else 4
                            n = ntile * 128
                            if b + 1 < nblk_c:
                                c_pend[b + 1] = c_stage1(b + 1)
                            xnT, R_xnT = c_pend.pop(b)
                            Wsrc, R_W, koff, voff = (Wc, R_Wc, 0, 256) if isctx else (Wkv, R_Wl, 0, 256)
                            cs, R_cs = (None, None)
                            if not isctx:
                                cs, R_cs = cs_ring.next()
                                P.dma("sp", cs[:], ropek[:, :, b * 512:(b + 1) * 512].rearrange("c p n -> p c n"), "cs0", writes=[R_cs])
                            for j in range(ntile):
                                kt = b * 4 + j
                                hv = j % 2
                                P.op("pe", [mm(psVb[hv][:, 0:256], xnT[:, k, j * 128:(j + 1) * 128], Wsrc[:, k, voff:voff + 256], k == 0, k == 7) for k in range(8)],
                                     reads=[R_xnT, R_W], writes=[R_psV[hv]])
                                P.op("dve", tt(Vs[:, kt, :], psVb[hv][:, 0:256], bvb[:, 1 if isctx else 0, :], ALU.add), reads=[R_psV[hv], R_bvb], writes=[R_V[kt]])
                            for g in range(2):
                                psK, R_psK = psK_ring.next()
                                P.op("pe", [mm(psK[:, :n], Wsrc[:, k, koff + g * 128: koff + (g + 1) * 128], xnT[:, k, :n], k == 0, k == 7) for k in range(8)],
                                     reads=[R_xnT, R_W], writes=[R_psK])
                                bidx = (10 if isctx else 8) + g
                                qk_post(psK, R_psK, n, bcol[:, bidx:bidx + 1], gcol[:, 1:2], cs, R_cs,
                                        KT[:, g, b * 512: b * 512 + n], R_KT[g][b])

                    else:
                        qst_ring = Ring([sb(f"qst{i}", [128, 512], BF16, st=scd) for i in range(1)])
                        for bi, (b0, n) in enumerate(BLKS):
                            ntile = max(1, n // 128)
                            xnT, R_xnT = xnT_ring.next()
                            for j in range(ntile):
                                norm_transpose_tile(x_ext[b0 + j * 128: b0 + (j + 1) * 128, :], xnT[:, :, j * 128:(j + 1) * 128], R_xnT, first_extra=phaseB_done if (bi == 0 and j == 0) else ())
                            cs, R_cs = cs_ring.next()
                            P.dma("sp", cs[:, :, :n], ropeq[:, :, b0:b0 + n].rearrange("c p n -> p c n"), "cs0", writes=[R_cs], **NC_KW)
                            for h in range(8):
                                psK, R_psK = psK_ring.next()
                                P.op("pe", [mm(psK[:, :n], Wq[:, k, h * 128:(h + 1) * 128], xnT[:, k, :n], k == 0, k == 7) for k in range(8)],
                                     reads=[R_xnT, R_Wl], writes=[R_psK])
                                qst, R_qst = qst_ring.next()
                                qk_post(psK, R_psK, n, bcol[:, h:h + 1], gcol[:, 0:1], cs, R_cs, qst[:, :n], R_qst)
                                P.dma("sp", qT_d[:, h, b0:b0 + n], qst[:, :n], "qst0", reads=[R_qst], writes=[R_qTd[bi]], **NC_KW)
                    ph_done = [("E" + e_, P.cnt[e_]) for e_ in ("pe", "act", "dve", "pool")] + [("D" + k_, v_) for k_, v_ in P.dma_sems.items()]
                    for e_ in ENGS:
                        P.wait(e_, ph_done)
                if phase_ == "D":
                    sq_.close()
            cd_done = [P.op("dve", cp(dmy[:, 0:1], epsc[:, 0:1]), writes=[R_dmy], reads=[R_eps] + R_V + [r for rr in R_KT for r in rr] + R_qTd)]
            sw.close()

            with contextlib.ExitStack() as se:
                for l_ in range(2):
                    for e_ in range(NTILE):
                        P.dma("pool", hs_d[l_][e_ * CAP:(e_ + 1) * CAP, :].rearrange("(s p) d -> p s d", p=128),
                              zt[:].unsqueeze(1).to_broadcast([128, CAP // 128, D]), f"zf{(l_ * NTILE + e_) % 8}", reads=[R_zt], writes=[R_hs0[l_][e_]], extra=cd_done)
                g0b = sb("g0b", [128, D], st=se)
                R_g0b = Reg()
                P.dma("sp", g0b[:], bc(mv(0, 0, 2)), "c0", reads=[R_mvec[0]], writes=[R_g0b], extra=cd_done)
                Wob = sb("Wob", [128, 8, D], BF16, st=se)
                R_Wob = Reg()
                wos_ring = Ring([sb(f"wos{i}", [128, D], st=se) for i in range(1)])
                for k in range(8):
                    wos, R_wos = wos_ring.next()
                    P.dma("sp", wos[:], wo[k * 128:(k + 1) * 128, :], "wos0", writes=[R_wos], extra=cd_done)
                    P.op("dve", tt(Wob[:, k, :], wos[:], g0b[:], ALU.mult), reads=[R_wos, R_g0b], writes=[R_Wob])
                QT_ring = Ring([sb(f"QTb{i}", [128, 8, 512], BF16, st=se) for i in range(2)])
                PT_ring = Ring([sb(f"PT{i}", [128, 512], BF16, st=se) for i in range(4)])
                attnT = sb("attnT", [128, 8, 512], BF16, st=se)
                R_attnT = Reg()
                rc = sb("rc", [128, 512], st=se)
                R_rc = Reg()
                xt_ring = Ring([sb(f"xt{i}", [128, D], st=se) for i in range(1)])
                x1_ring = Ring([sb(f"x1{i}", [128, D], st=se) for i in range(2)])
                psS_ring = Ring([ps(f"psS{i}", [128, 512], st=se) for i in range(4)])
                pO = [ps(f"pO{i}", [128, 512], st=se) for i in range(2)]
                pD = [ps(f"pD{i}", [128, 512], st=se) for i in range(2)]
                R_pO = [Reg(), Reg()]
                LA = 2
                for bi, (b0, n) in enumerate(BLKS):
                    QTb, R_QTb = QT_ring.next()
                    P.dma("sp", QTb[:, :, :n], qT_d[:, :, b0:b0 + n], f"qtb{bi % 2}", reads=[R_qTd[bi]], writes=[R_QTb], **NC_KW)
                    for hp in range(4):
                        g = hp // 2
                        units = [(kt, hh) for kt in range(NKT) for hh in range(2)]
                        pend = []
                        for i in range(len(units) + LA):
                            if i < len(units):
                                kt, hh = units[i]
                                h = hp * 2 + hh
                                pS, R_pS = psS_ring.next()
                                P.op("pe", mm(pS[:, :n], KT[:, g, kt * 128:(kt + 1) * 128], QTb[:, h, :n]),
                                     reads=[R_KT[g][kt // 4], R_QTb], writes=[R_pS])
                                PT, R_PT = PT_ring.next()
                                P.op("act", actf(PT[:, :n], pS[:, :n], AF.Exp, bias=negB[:, 0:1], scale=1.0 / math.sqrt(128.0)),
                                     reads=[R_pS, R_negB], writes=[R_PT])
                                pend.append((kt, hh, PT, R_PT))
                            if i >= LA:
                                kt, hh, PT, R_PT = pend[i - LA]
                                P.op("pe", [mm(pO[hh][:, :n], Vs[:, kt, g * 128:(g + 1) * 128], PT[:, :n], kt == 0, kt == NKT - 1),
                                            mm(pD[hh][:, :n], onesb, PT[:, :n], kt == 0, kt == NKT - 1)],
                                     reads=[R_PT, R_V[kt], R_cstb], writes=[R_pO[hh]])
                        for hh in range(2):
                            h = hp * 2 + hh
                            P.op("dve", lambda e, hh=hh, n=n: e.reciprocal(rc[:, :n], pD[hh][:, :n]), reads=[R_pO[hh]], writes=[R_rc])
                            P.op("dve", tt(attnT[:, h, :n], pO[hh][:, :n], rc[:, :n], ALU.mult), reads=[R_pO[hh], R_rc], writes=[R_attnT])
                    for j in range(max(1, n // 128)):
                        m = min(128, n)
                        ti = b0 // 128 + j
                        xt, R_xt = xt_ring.next()
                        P.dma("sp", xt[:], x_ext[ti * 128:(ti + 1) * 128, :], "xt0", writes=[R_xt])
                        x1, R_x1 = x1_ring.next()
                        if m < 128:
                            P.op("dve", cp(x1[:], xt[:]), reads=[R_xt], writes=[R_x1])
                        for half in range(2):
                            pY, R_pY = psS_ring.next()
                            P.op("pe", [mm(pY[:m, :], attnT[:, h, j * 128:j * 128 + m], Wob[:, h, half * 512:(half + 1) * 512], h == 0, h == 7) for h in range(8)],
                                 reads=[R_attnT, R_Wob], writes=[R_pY])
                            P.op("dve", tt(x1[:m, half * 512:(half + 1) * 512], pY[:m, :], xt[:m, half * 512:(half + 1) * 512], ALU.add),
                                 reads=[R_pY, R_xt], writes=[R_x1])
                        P.dma("sp", xspill[ti * 128:(ti + 1) * 128, :], x1[:], f"x1{(x1_ring.i - 1) % 2}", reads=[R_x1], writes=[R_xspill])
                attn_done = [P.op("dve", cp(dmy[:, 0:1], epsc[:, 0:1]), writes=[R_dmy], reads=[R_eps, R_attnT, R_rc] + x1_ring.regs + xt_ring.regs + PT_ring.regs)]
                attn_done.append(R_xspill.w)
                attn_done.append(("Epe", P.cnt["pe"]))
                attn_done.append(("Eact", P.cnt["act"]))

        XB = sb("XB", [128, NT, D])
        R_XB = [Reg() for _ in range(NT)]
        for t in range(NT):
            P.dma("sp", XB[:, t, :], xspill[t * 128:(t + 1) * 128, :], f"xb{t % 4}", reads=[R_xspill], writes=[R_XB[t]], extra=attn_done)

        C7 = (LIM * ALPHA) / (1.0 + math.exp(-LIM * ALPHA))

        def rstd_tile(src_ap, junk, R_junk, stt_, R_st, rd):
            P.op("act", actf(junk, src_ap, AF.Square, accum=stt_[:, 0:1]), reads=rd, writes=[R_junk, R_st])
            P.op("act", actf(stt_[:, 1:2], stt_[:, 0:1], AF.Ln, bias=epsc[:, 0:1], scale=1.0 / D), reads=[R_st, R_eps], writes=[R_st])
            P.op("act", actf(stt_[:, 1:2], stt_[:, 1:2], AF.Exp, scale=-0.5), reads=[R_st], writes=[R_st])

        def moe(l):
            with contextlib.ExitStack() as sm:
                hfT = sb(f"hfT{l}", [128, 8, NEXT], BF16, st=sm)
                R_hf = [Reg() for _ in BLKS]
                Gt = sb(f"G{l}", [128, NT, E], st=sm)
                R_G = [Reg() for _ in range(NT)]
                GT = sb(f"GT{l}", [E, NEXT], st=sm)
                R_GT = [Reg() for _ in range(NT)]
                bd_sb = sb(f"bd{l}", [E, D], st=sm)
                R_bd = Reg()
                P.dma("sp", bd_sb[:], b_down[l], "c0", writes=[R_bd])
                bguc = sb(f"bguc{l}", [128, 16, E], st=sm)
                bgs = sb(f"bgs{l}", [128, 8, E], st=sm)
                R_bguc = Reg()
                ifirst = vecs[:, 7 if l == 0 else 10, :]
                Afc = AB[:, 2 + l, :]
                with contextlib.ExitStack() as s1:
                    bgr = sb(f"bgr{l}", [E, 2 * D], st=s1)
                    R_bgr = Reg()
                    P.dma("sp", bgr[:], b_gu[l], "c0", writes=[R_bgr])
                    psT = ps(f"psT{l}", [128, 16, E], st=s1)
                    R_psT = Reg()
                    P.op("pe", [tp(psT[:, c, :], bgr[0:E, c * 128:(c + 1) * 128], identf[0:E, 0:E]) for c in range(16)], reads=[R_bgr, R_cst], writes=[R_psT])
                    P.op("dve", cp(bguc[:], psT[:]), reads=[R_psT], writes=[R_bguc])
                    P.op("dve", ts(bgs[:].rearrange("p a b -> p (a b)"), bguc[:, 0:8, :].rearrange("p a b -> p (a b)"), ALPHA, None, ALU.mult), reads=[R_bguc], writes=[R_bguc])
                    wr = sb(f"wr{l}", [128, 8, E], st=s1)
                    R_wr = Reg()
                    P.dma("sp", wr[:], router_w[l].rearrange("(k p) e -> p k e", p=128), "c0", writes=[R_wr])
                    rbb = sb(f"rbb{l}", [128, E], st=s1)
                    R_rbb = Reg()
                    P.dma("sp", rbb[:], bc(router_b[l]), "c0", writes=[R_rbb])
                    xnf_ring = Ring([sb(f"xnf{l}{i}", [128, D], st=s1) for i in range(2)])
                    st_ring = Ring([sb(f"mst{l}{i}", [128, 2], st=s1) for i in range(2)])
                    h32_ring = Ring([sb(f"h32{l}{i}", [128, 8, 128], st=s1) for i in range(2)])
                    p32_ring = Ring([ps(f"p32{l}{i}", [128, 8, 128], st=s1) for i in range(2)])
                    psL = ps(f"psL{l}", [128, 512], st=s1)
                    R_psL = Reg()
                    psG2 = ps(f"psG2{l}", [128, 512], st=s1)
                    R_psG2 = Reg()
                    sm_ring = Ring([sb(f"smx{l}{i}", [128, 4, E], st=s1) for i in range(2)])
                    s8_ring = Ring([sb(f"s8{l}{i}", [128, 12], st=s1) for i in range(2)])
                    for t in range(NT):
                        xnf, R_xnf = xnf_ring.next()
                        stt_, R_st = st_ring.next()
                        rstd_tile(XB[:, t, :], xnf[:], R_xnf, stt_, R_st, [R_XB[t]])
                        P.op("act", actf(xnf[:], XB[:, t, :], AF.Copy, scale=stt_[:, 1:2]), reads=[R_XB[t], R_st], writes=[R_xnf])
                        p32, R_p32 = p32_ring.next()
                        P.op("pe", [tp(p32[:, k, :], xnf[:, k * 128:(k + 1) * 128], identf) for k in range(8)], reads=[R_xnf, R_cst], writes=[R_p32])
                        h32, R_h32 = h32_ring.next()
                        for k in range(8):
                            P.op("dve", ts(h32[:, k, :], p32[:, k, :], Afc[:, k:k + 1], ifirst[:, k:k + 1], ALU.mult, ALU.add), reads=[R_p32, R_AB, R_vecs], writes=[R_h32])
                        bi = min(t // 4, 4)
                        P.op("pool", cp(hfT[:, :, t * 128:(t + 1) * 128], h32[:]), reads=[R_h32], writes=[R_hf[bi]])
                        P.op("pe", [mm(psL[:, 0:E], h32[:, k, :], wr[:, k, :], k == 0, k == 7) for k in range(8)], reads=[R_h32, R_wr], writes=[R_psL])
                        sx, R_sx = sm_ring.next()
                        s8, R_s8 = s8_ring.next()
                        P.op("dve", tt(sx[:, 0, :], psL[:, 0:E], rbb[:], ALU.add), reads=[R_psL, R_rbb], writes=[R_sx])
                        P.op("dve", lambda e, s8=s8, sx=sx: e.max(s8[:, 0:8], sx[:, 0, :]), reads=[R_sx], writes=[R_s8])
                        P.op("dve", ts(s8[:, 8:9], s8[:, 0:1], -1.0, None, ALU.mult), reads=[R_s8], writes=[R_s8])
                        P.op("act", actf(sx[:, 1, :], sx[:, 0, :], AF.Exp, bias=s8[:, 8:9]), reads=[R_sx, R_s8], writes=[R_sx])
                        P.op("dve", ts(sx[:, 2, :], sx[:, 0, :], s8[:, 3:4], None, ALU.is_ge), reads=[R_sx, R_s8], writes=[R_sx])
                        P.op("dve", stt(sx[:, 3, :], sx[:, 1, :], 1.0, sx[:, 2, :], ALU.mult, ALU.mult, accum=s8[:, 9:10]), reads=[R_sx], writes=[R_sx, R_s8])
                        P.op("dve", lambda e, s8=s8: e.reciprocal(s8[:, 10:11], s8[:, 9:10]), reads=[R_s8], writes=[R_s8])
                        P.op("dve", ts(Gt[:, t, :], sx[:, 3, :], s8[:, 10:11], None, ALU.mult), reads=[R_sx, R_s8], writes=[R_G[t]])
                        P.op("pe", tp(psG2[0:E, 0:128], Gt[:, t, :], identf), reads=[R_G[t], R_cst], writes=[R_psG2])
                        P.op("dve", cp(GT[0:E, t * 128:(t + 1) * 128], psG2[0:E, 0:128]), reads=[R_psG2], writes=[R_GT[t]])
                    g1_done = [P.op("dve", cp(dmy[:, 0:1], epsc[:, 0:1]), writes=[R_dmy], reads=[R_eps] + R_hf + R_GT + R_XB + [R_bguc]),
                               ("Epe", P.cnt["pe"]), ("Eact", P.cnt["act"]), ("Epool", P.cnt["pool"])]
                with contextlib.ExitStack() as s3:
                    actT = sb(f"actT{l}", [128, 8, NEXT], BF16, st=s3)
                    R_act = [[Reg() for _ in BLKS] for _ in range(8)]
                    wring = Ring([sb(f"wp{l}{i}", [128, 8, 512], BF16, st=s3) for i in range(4)])
                    sS_ring = Ring([sb(f"sS{l}{i}", [128, 512], st=s3) for i in range(2)])
                    uS_ring = Ring([sb(f"uS{l}{i}", [128, 512], st=s3) for i in range(2)])
                    psG_ring = Ring([ps(f"psG{l}{i}", [128, 512], st=s3) for i in range(2)])
                    psU_ring = Ring([ps(f"psU{l}{i}", [128, 512], st=s3) for i in range(2)])
                    psY_ring = Ring([ps(f"psY{l}{i}", [128, 512], st=s3) for i in range(3)])
                    for t in range(NT):
                        for half in range(2):
                            pY, R_pY = psY_ring.next()
                            P.op("pe", mm(pY[:, :], GT[0:E, t * 128:(t + 1) * 128], bd_sb[0:E, half * 512:(half + 1) * 512]),
                                 reads=[R_GT[t], R_bd], writes=[R_pY], extra=g1_done)
                            P.op("act", actf(XB[:, t, half * 512:(half + 1) * 512], pY[:, :], AF.Copy), reads=[R_pY], writes=[R_XB[t]], extra=g1_done)
                    wcnt = [0]

                    def wload(parts):
                        wb, R_wb = wring.next()
                        slot = (wring.i - 1) % 4
                        for dst, src in parts:
                            P.dma("pool", dst(wb), src, f"w{slot}_{wcnt[0] % 2}", writes=[R_wb], extra=g1_done if wcnt[0] < 8 else ())
                            wcnt[0] += 1
                        return wb, R_wb

                    for e in range(E):
                        wge = w_gu[l, e].rearrange("(k p) n -> p k n", p=128)
                        for pi in range(4):
                            wb, R_wb = wload([(lambda b: b[:, :, 0:256], wge[:, :, pi * 256:(pi + 1) * 256]),
                                              (lambda b: b[:, :, 256:512], wge[:, :, D + pi * 256: D + (pi + 1) * 256])])
                            for jj in range(2):
                                fc = pi * 2 + jj
                                for bi, (b0, n) in enumerate(BLKS):
                                    psG, R_psG = psG_ring.next()
                                    psU, R_psU = psU_ring.next()
                                    P.op("pe", [mm(psG[:, :n], wb[:, k, jj * 128:(jj + 1) * 128], hfT[:, k, b0:b0 + n], k == 0, k == 7) for k in range(8)],
                                         reads=[R_wb, R_hf[bi]], writes=[R_psG])
                                    P.op("pe", [mm(psU[:, :n], wb[:, k, 256 + jj * 128:256 + (jj + 1) * 128], hfT[:, k, b0:b0 + n], k == 0, k == 7) for k in range(8)],
                                         reads=[R_wb, R_hf[bi]], writes=[R_psU])
                                    sS, R_sS = sS_ring.next()
                                    uS, R_uS = uS_ring.next()
                                    P.op("act", actf(sS[:, :n], psG[:, :n], AF.Silu, bias=bgs[:, fc, e:e + 1], scale=ALPHA), reads=[R_psG, R_bguc], writes=[R_sS])
                                    P.op("act", actf(uS[:, :n], psU[:, :n], AF.Identity, bias=bguc[:, 8 + fc, e:e + 1]), reads=[R_psU, R_bguc], writes=[R_uS])
                                    P.op("dve", ts(sS[:, :n], sS[:, :n], C7, 1.0 / ALPHA, ALU.min, ALU.mult), reads=[R_sS], writes=[R_sS])
                                    P.op("dve", ts(uS[:, :n], uS[:, :n], LIM, -LIM, ALU.min, ALU.max), reads=[R_uS], writes=[R_uS])
                                    P.op("dve", stt(actT[:, fc, b0:b0 + n], uS[:, :n], 1.0, sS[:, :n], ALU.add, ALU.mult), reads=[R_uS, R_sS], writes=[R_act[fc][bi]])
                        wde = w_down[l, e].rearrange("(k p) n -> p k n", p=128)
                        for half in range(2):
                            wb, R_wb = wload([(lambda b: b[:, :, :], wde[:, :, half * 512:(half + 1) * 512])])
                            for t in range(NT):
                                m = 128 if t < 16 else 16
                                bi = min(t // 4, 4)
                                pY, R_pY = psY_ring.next()
                                P.op("pe", [mm(pY[:m, :], actT[:, fc, t * 128:t * 128 + m], wb[:, fc, :], fc == 0, fc == 7) for fc in range(8)],
                                     reads=[R_wb] + [R_act[fc][bi] for fc in range(8)], writes=[R_pY])
                                P.op("dve", stt(XB[:m, t, half * 512:(half + 1) * 512], pY[:m, :], Gt[:m, t, e:e + 1], XB[:m, t, half * 512:(half + 1) * 512], ALU.mult, ALU.add),
                                     reads=[R_pY, R_G[t]], writes=[R_XB[t]])
                    g3_done = [("Epe", P.cnt["pe"]), ("Eact", P.cnt["act"]), ("Edve", P.cnt["dve"])]
                with contextlib.ExitStack() as s4:
                    gfb = sb(f"gfb{l}", [128, D], st=s4)
                    R_gfb = Reg()
                    P.dma("sp", gfb[:], bc(mv(l, 0, 5)), "c0", reads=[R_mvec[l]], writes=[R_gfb], extra=g3_done)
                    xt_ring = Ring([sb(f"mxt{l}{i}", [128, D], st=s4) for i in range(2)])
                    for t in range(NT):
                        xt, R_xt = xt_ring.next()
                        P.dma("sp", xt[:], xspill[t * 128:(t + 1) * 128, :], f"mxt{t % 2}", reads=[R_xspill], writes=[R_xt], extra=g3_done)
                        P.op("dve", tt(XB[:, t, :], XB[:, t, :], gfb[:], ALU.mult), reads=[R_XB[t], R_gfb], writes=[R_XB[t]])
                        P.op("dve", tt(XB[:, t, :], XB[:, t, :], xt[:], ALU.add), reads=[R_XB[t], R_xt], writes=[R_XB[t]])
                    return [P.op("dve", cp(dmy[:, 0:1], epsc[:, 0:1]), writes=[R_dmy], reads=[R_eps] + R_XB + xt_ring.regs),
                            ("Epe", P.cnt["pe"]), ("Eact", P.cnt["act"]), ("Edve", P.cnt["dve"])]


        I32 = mybir.dt.int32
        U32 = mybir.dt.uint32

        _bnd = {}

        def bnd_reg(engine, which="r"):
            if which not in _bnd:
                _bnd[which] = engine.to_reg(NSLOT - 1 if which == "r" else (int(which[1:]) + 1) * E * D - 1)
            return _bnd[which]

        def moe_sparse(l):
            hs, ys = hs_d[l], ys_d[l]
            wgu_rows = w_gu.rearrange("l e r n -> (l e r) n")
            wdn_rows = w_down.rearrange("l e r n -> (l e r) n")
            with contextlib.ExitStack() as sm:
                Gt = sb(f"G{l}", [128, NT, E], st=sm)
                R_G = [Reg() for _ in range(NT)]
                GT = sb(f"GT{l}", [E, NEXT], st=sm)
                R_GT = [Reg() for _ in range(NT)]
                bd_sb = sb(f"bd{l}", [E, D], st=sm)
                R_bd = Reg()
                P.dma("sp", bd_sb[:], b_down[l], "c0", writes=[R_bd])
                SLOT = sb(f"SLOT{l}", [128, NT, 4], I32, st=sm)
                GK = sb(f"GK{l}", [128, NT, 4], st=sm)
                R_SLOT = [Reg() for _ in range(NT)]
                R_GK = [Reg() for _ in range(NT)]
                R_hs = [[Reg() for _ in range(4)] for _ in range(NT)]
                IDXW = sb(f"IDXW{l}", [128, NTILE, 8], I32, st=sm)
                bsel = sb(f"bsel{l}", [128, NTILE, 16], st=sm)
                bgsel = sb(f"bgsel{l}", [128, NTILE, 8], st=sm)
                R_IDXW, R_bsel = Reg(), Reg()
                ifirst = vecs[:, 7 if l == 0 else 10, :]
                Afc = AB[:, 2 + l, :]
                with contextlib.ExitStack() as s1:
                    bguc = sb(f"bguc{l}", [128, 16, E], st=s1)
                    R_bguc = Reg()
                    bgr = sb(f"bgr{l}", [E, 2 * D], st=s1)
                    R_bgr = Reg()
                    P.dma("sp", bgr[:], b_gu[l], "c0", writes=[R_bgr])
                    psT = ps(f"psT{l}", [128, 16, E], st=s1)
                    R_psT = Reg()
                    P.op("pe", [tp(psT[:, c, :], bgr[0:E, c * 128:(c + 1) * 128], identf[0:E, 0:E]) for c in range(16)], reads=[R_bgr, R_cst], writes=[R_psT])
                    P.op("dve", cp(bguc[:], psT[:]), reads=[R_psT], writes=[R_bguc])
                    wr = sb(f"wr{l}", [128, 8, E], st=s1)
                    R_wr = Reg()
                    P.dma("sp", wr[:], router_w[l].rearrange("(k p) e -> p k e", p=128), "c0", writes=[R_wr])
                    rbb = sb(f"rbb{l}", [128, E], st=s1)
                    R_rbb = Reg()
                    P.dma("sp", rbb[:], bc(router_b[l]), "c0", writes=[R_rbb])
                    Afr = sb(f"Afr{l}", [128, D], st=s1)
                    Bfr = sb(f"Bfr{l}", [128, D], st=s1)
                    tmr = sb(f"tmr{l}", [128, D], st=s1)
                    R_Afr, R_Bfr, R_tmr = Reg(), Reg(), Reg()
                    P.dma("sp", Afr[:], bc(mv(l, 0, 4)), "c0", reads=[R_mvec[l]], writes=[R_Afr])
                    P.dma("sp", tmr[:], bc(norm_ffn[l]), "c0", writes=[R_tmr])
                    P.op("dve", stt(Afr[:], Afr[:], 1.0, tmr[:], ALU.add, ALU.mult), reads=[R_Afr, R_tmr], writes=[R_Afr])
                    P.dma("sp", Bfr[:], bc(mv(l, 0, 3)), "c0", reads=[R_mvec[l]], writes=[R_Bfr])
                    MSK = sb(f"MSK{l}", [128, NT, E], BF16, st=s1)
                    R_MSK = [Reg() for _ in range(NT)]
                    RK4 = sb(f"RK4{l}", [128, NT, 4], st=s1)
                    E4 = sb(f"E4{l}", [128, NT, 4], st=s1)
                    R_RK4 = [Reg() for _ in range(NT)]
                    HFT = sb(f"HFT{l}", [128, NT, D], BF16, st=s1)
                    R_HFT = [Reg() for _ in range(NT)]
                    xnf_ring = Ring([sb(f"xnf{l}{i}", [128, D], st=s1) for i in range(2)])
                    st_ring = Ring([sb(f"mst{l}{i}", [128, 2], st=s1) for i in range(2)])
                    h32_ring = Ring([sb(f"h32{l}{i}", [128, 8, 128], st=s1) for i in range(2)])
                    p32_ring = Ring([ps(f"p32{l}{i}", [128, 8, 128], st=s1) for i in range(2)])
                    psL = ps(f"psL{l}", [128, 512], st=s1)
                    R_psL = Reg()
                    psG2 = ps(f"psG2{l}", [128, 512], st=s1)
                    R_psG2 = Reg()
                    psRk = ps(f"psRk{l}", [128, 512], st=s1)
                    R_psRk = Reg()
                    sm_ring = Ring([sb(f"smx{l}{i}", [128, 6, E], st=s1) for i in range(2)])
                    s8_ring = Ring([sb(f"s8{l}{i}", [128, 40], st=s1) for i in range(2)])
                    i8_ring = Ring([sb(f"i8{l}{i}", [128, 8], U32, st=s1) for i in range(2)])
                    for t in range(NT):
                        xnf, R_xnf = xnf_ring.next()
                        stt_, R_st = st_ring.next()
                        rstd_tile(XB[:, t, :], xnf[:], R_xnf, stt_, R_st, [R_XB[t]])
                        P.op("act", actf(xnf[:], XB[:, t, :], AF.Copy, scale=stt_[:, 1:2]), reads=[R_XB[t], R_st], writes=[R_xnf])
                        p32, R_p32 = p32_ring.next()
                        P.op("pe", [tp(p32[:, k, :], xnf[:, k * 128:(k + 1) * 128], identf) for k in range(8)], reads=[R_xnf, R_cst], writes=[R_p32])
                        h32, R_h32 = h32_ring.next()
                        for k in range(8):
                            P.op("dve", ts(h32[:, k, :], p32[:, k, :], Afc[:, k:k + 1], ifirst[:, k:k + 1], ALU.mult, ALU.add), reads=[R_p32, R_AB, R_vecs], writes=[R_h32])
                        P.op("pool", tt(tmr[:], xnf[:], Afr[:], ALU.mult), reads=[R_xnf, R_Afr, R_tmr], writes=[R_tmr])
                        P.op("pool", tt(HFT[:, t, :], tmr[:], Bfr[:], ALU.add), reads=[R_tmr, R_Bfr], writes=[R_HFT[t]])
                        P.op("pe", [mm(psL[:, 0:E], h32[:, k, :], wr[:, k, :], k == 0, k == 7) for k in range(8)], reads=[R_h32, R_wr], writes=[R_psL])
                        sx, R_sx = sm_ring.next()
                        s8, R_s8 = s8_ring.next()
                        i8, R_i8 = i8_ring.next()
                        P.op("dve", tt(sx[:, 0, :], psL[:, 0:E], rbb[:], ALU.add), reads=[R_psL, R_rbb], writes=[R_sx])
                        P.op("dve", lambda e, s8=s8, sx=sx: e.max(s8[:, 0:8], sx[:, 0, :]), reads=[R_sx], writes=[R_s8])
                        P.op("dve", lambda e, s8=s8, sx=sx, i8=i8: e.max_index(i8[:, 0:8], s8[:, 0:8], sx[:, 0, :]), reads=[R_sx, R_s8], writes=[R_i8])
                        P.op("dve", ts(s8[:, 8:9], s8[:, 0:1], -1.0, None, ALU.mult), reads=[R_s8], writes=[R_s8])
                        P.op("act", actf(s8[:, 12:16], s8[:, 0:4], AF.Exp, bias=s8[:, 8:9], accum=s8[:, 9:10]), reads=[R_s8], writes=[R_s8])
                        P.op("dve", lambda e, s8=s8: e.reciprocal(s8[:, 10:11], s8[:, 9:10]), reads=[R_s8], writes=[R_s8])
                        P.op("dve", ts(GK[:, t, :], s8[:, 12:16], s8[:, 10:11], None, ALU.mult), reads=[R_s8], writes=[R_GK[t]])
                        P.op("dve", cp(E4[:, t, :], i8[:, 0:4]), reads=[R_i8], writes=[R_RK4[t]])
                        for k in range(4):
                            P.op("dve", ts(sx[:, 2 + k, :], iota_e, E4[:, t, k:k + 1], None, ALU.is_equal), reads=[R_RK4[t], R_cst], writes=[R_sx])
                        P.op("dve", tt(sx[:, 1, :], sx[:, 2, :], sx[:, 3, :], ALU.add), reads=[R_sx], writes=[R_sx])
                        P.op("dve", tt(sx[:, 1, :], sx[:, 1, :], sx[:, 4, :], ALU.add), reads=[R_sx], writes=[R_sx])
                        P.op("dve", tt(sx[:, 1, :], sx[:, 1, :], sx[:, 5, :], ALU.add), reads=[R_sx], writes=[R_sx])
                        if t == 16:
                            P.op("dve", ts(sx[:, 1, :], sx[:, 1, :], vmask, None, ALU.mult), reads=[R_sx, R_cst], writes=[R_sx])
                        P.op("dve", cp(MSK[:, t, :], sx[:, 1, :]), reads=[R_sx], writes=[R_MSK[t]])
                        P.op("dve", ts(Gt[:, t, :], sx[:, 2, :], GK[:, t, 0:1], None, ALU.mult), reads=[R_sx, R_GK[t]], writes=[R_G[t]])
                        for k in range(1, 4):
                            P.op("dve", stt(Gt[:, t, :], sx[:, 2 + k, :], GK[:, t, k:k + 1], Gt[:, t, :], ALU.mult, ALU.add), reads=[R_sx, R_GK[t], R_G[t]], writes=[R_G[t]])
                        P.op("pe", tp(psG2[0:E, 0:128], Gt[:, t, :], identf), reads=[R_G[t], R_cst], writes=[R_psG2])
                        P.op("dve", cp(GT[0:E, t * 128:(t + 1) * 128], psG2[0:E, 0:128]), reads=[R_psG2], writes=[R_GT[t]])
                        fns = [mm(psRk[:, 0:E], onesb, MSK[:, tp_, :], tp_ == 0, False) for tp_ in range(t)]
                        fns.append(mm(psRk[:, 0:E], ustrb, MSK[:, t, :], t == 0, True))
                        P.op("pe", fns, reads=R_MSK[0:t + 1] + [R_cstb], writes=[R_psRk])
                        for k in range(4):
                            P.op("dve", stt(sx[:, 2 + k, :], sx[:, 2 + k, :], 1.0, psRk[:, 0:E], ALU.mult, ALU.mult, accum=RK4[:, t, k:k + 1]),
                                 reads=[R_sx, R_psRk], writes=[R_sx, R_RK4[t]])
                    gl = sb(f"gl{l}", [128, 8, E], st=s1)
                    R_gl = Reg()
                    P.op("pe", [mm(psRk[:, 0:E], onesb, MSK[:, tp_, :], tp_ == 0, tp_ == NT - 1) for tp_ in range(NT)], reads=R_MSK + [R_cstb], writes=[R_psRk])
                    P.op("dve", cp(gl[:, 0, :], psRk[:, 0:E]), reads=[R_psRk], writes=[R_gl])
                    P.op("dve", ts(gl[:, 3, :], gl[:, 0, :], 0.0, None, ALU.is_gt), reads=[R_gl], writes=[R_gl])
                    for m_ in range(1, 5):
                        P.op("dve", stt(gl[:, 3, :], gl[:, 0, :], float(m_ * CAP), gl[:, 3, :], ALU.is_gt, ALU.add), reads=[R_gl], writes=[R_gl])
                    P.op("dve", cp(gl[:, 4, :], gl[:, 3, :]), reads=[R_gl], writes=[R_gl])
                    cur, nxt = 4, 5
                    for sft in (1, 2, 4, 8, 16):
                        P.op("dve", cp(gl[:, nxt, 0:sft], gl[:, cur, 0:sft]), reads=[R_gl], writes=[R_gl])
                        P.op("dve", tt(gl[:, nxt, sft:E], gl[:, cur, sft:E], gl[:, cur, 0:E - sft], ALU.add), reads=[R_gl], writes=[R_gl])
                        cur, nxt = nxt, cur
                    cum = gl[:, cur, :]
                    P.op("dve", tt(gl[:, 6, :], cum, gl[:, 3, :], ALU.subtract), reads=[R_gl], writes=[R_gl])
                    P.op("dve", ts(gl[:, 6, :], gl[:, 6, :], float(CAP), None, ALU.mult), reads=[R_gl], writes=[R_gl])
                    ejt = sb(f"ejt{l}", [128, NTILE], st=s1)
                    R_ej = Reg()
                    for j in range(NTILE):
                        P.op("dve", stt(gl[:, 7, :], cum, float(j), cst[:, 3, 0:E], ALU.is_le, ALU.mult, accum=ejt[:, j:j + 1]), reads=[R_gl, R_cst], writes=[R_gl, R_ej])
                    P.op("dve", ts(ejt[:], ejt[:], float(E - 1), None, ALU.min), reads=[R_ej], writes=[R_ej])
                    idxf = sb(f"idxf{l}", [128, NTILE, 8], st=s1)
                    R_idxf = Reg()
                    for k in range(8):
                        P.op("dve", ts(idxf[:, :, k], ejt[:], float(D), kpc[:, k:k + 1], ALU.mult, ALU.add), reads=[R_ej, R_cst], writes=[R_idxf])
                        if l == 1:
                            P.op("dve", ts(idxf[:, :, k], idxf[:, :, k], float(E * D), None, ALU.add), reads=[R_idxf], writes=[R_idxf])
                    P.op("dve", cp(IDXW[:], idxf[:]), reads=[R_idxf], writes=[R_IDXW])
                    ohj = sb(f"ohj{l}", [128, E], st=s1)
                    tmpb2 = sb(f"tmpb2{l}", [128, 16, E], st=s1)
                    R_ohj, R_tmpb2 = Reg(), Reg()
                    for j in range(NTILE):
                        P.op("dve", ts(ohj[:], iota_e, ejt[:, j:j + 1], None, ALU.is_equal), reads=[R_ej, R_cst], writes=[R_ohj])
                        P.op("dve", tt(tmpb2[:], bguc[:], ohj[:].unsqueeze(1).to_broadcast([128, 16, E]), ALU.mult), reads=[R_ohj, R_bguc], writes=[R_tmpb2])
                        P.op("dve", lambda e_, j=j: e_.reduce_sum(bsel[:, j, :], tmpb2[:], mybir.AxisListType.X), reads=[R_tmpb2], writes=[R_bsel])
                    P.op("dve", ts(bgsel[:].rearrange("p a b -> p (a b)").rearrange("p (a b) -> p a b", b=8), bsel[:, :, 0:8], ALPHA, None, ALU.mult), reads=[R_bsel], writes=[R_bsel])
                    s8b_ring = Ring([sb(f"s8b{l}{i}", [128, 8], st=s1) for i in range(2)])
                    ohk_ring = Ring([sb(f"ohk{l}{i}", [128, E], st=s1) for i in range(2)])
                    for t in range(NT):
                        s8b, R_s8b = s8b_ring.next()
                        for k in range(4):
                            ohk, R_ohk = ohk_ring.next()
                            P.op("dve", ts(ohk[:], iota_e, E4[:, t, k:k + 1], None, ALU.is_equal), reads=[R_RK4[t], R_cst], writes=[R_ohk])
                            P.op("dve", stt(ohk[:], ohk[:], 1.0, gl[:, 6, :], ALU.mult, ALU.mult, accum=s8b[:, k:k + 1]), reads=[R_ohk, R_gl], writes=[R_ohk, R_s8b])
                        P.op("dve", tt(s8b[:, 4:8], s8b[:, 0:4], RK4[:, t, :], ALU.add), reads=[R_s8b, R_RK4[t]], writes=[R_s8b])
                        if t == 16:
                            P.op("dve", ts(s8b[:, 4:8], s8b[:, 4:8], notvbig, None, ALU.add), reads=[R_s8b, R_cst], writes=[R_s8b])
                        P.op("dve", cp(SLOT[:, t, :], s8b[:, 4:8]), reads=[R_s8b], writes=[R_SLOT[t]])
                        for k in range(4):
                            P.dmaf("pool", lambda e, t=t, k=k: e.indirect_dma_start(
                                out=hs[:, :], out_offset=bass.IndirectOffsetOnAxis(ap=SLOT[:, t, k:k + 1], axis=0),
                                in_=HFT[:, t, :], in_offset=None, bounds_check=bnd_reg(e), oob_is_err=False),
                                f"sc{(t * 4 + k) % 8}", reads=[R_HFT[t], R_SLOT[t]], writes=[R_hs[t][k]], extra=[r.w for r in R_hs0[l]])
                    g1_done = [P.op("dve", cp(dmy[:, 0:1], epsc[:, 0:1]), writes=[R_dmy], reads=[R_eps] + R_GT + R_XB + [R_bsel, R_IDXW] + R_SLOT + R_GK),
                               ("Epe", P.cnt["pe"]), ("Eact", P.cnt["act"]), ("Epool", P.cnt["pool"])]
                    all_hs = [r for rr in R_hs for r in rr]
                    for t in range(4):
                        pass
                    g1_done += [r.w for r in all_hs]
                with contextlib.ExitStack() as s3:
                    hg_ring = Ring([sb(f"hg{l}{i}", [128, 4, D], BF16, st=s3) for i in range(1)])
                    hgT_ring = Ring([sb(f"hgT{l}{i}", [128, 8, CAP], BF16, st=s3) for i in range(2)])
                    actT_ring = Ring([sb(f"actT{l}{i}", [128, 8, CAP], BF16, st=s3) for i in range(2)])
                    wgu_ring = Ring([XB[:, 0:8, :].bitcast(BF16), XB[:, 8:16, :].bitcast(BF16)])
                    wdn_ring = Ring([sb(f"wdn{l}{i}", [128, 8, D], BF16, st=s3) for i in range(2)])
                    sS_ring = Ring([sb(f"sS{l}{i}", [128, 512], st=s3) for i in range(2)])
                    uS_ring = Ring([sb(f"uS{l}{i}", [128, 512], st=s3) for i in range(2)])
                    ysb_ring = Ring([sb(f"ysb{l}{i}", [128, D], st=s3) for i in range(4)])
                    pTe_ring = Ring([ps(f"pTe{l}{i}", [128, 8, 128], BF16, st=s3) for i in range(2)])
                    psG_ring = Ring([ps(f"psG{l}{i}", [128, 512], st=s3) for i in range(2)])
                    psU_ring = Ring([ps(f"psU{l}{i}", [128, 512], st=s3) for i in range(2)])
                    psY_ring = Ring([ps(f"psY{l}{i}", [128, 512], st=s3) for i in range(2)])
                    R_ys = [Reg() for _ in range(NTILE * 4)]
                    evi = 0
                    wq = 0
                    R_wguk = [[Reg() for _ in range(8)] for _ in range(2)]
                    R_wdnk = [[Reg() for _ in range(8)] for _ in range(2)]
                    for j in range(NTILE):
                        wgu, _ = wgu_ring.next()
                        wdn, _ = wdn_ring.next()
                        R_wgu = R_wguk[j % 2]
                        R_wdn = R_wdnk[j % 2]
                        for k in range(8):
                            P.dmaf("pool", lambda e_, j=j, k=k, wgu=wgu: e_.indirect_dma_start(
                                out=wgu[:, k, :], out_offset=None, in_=wgu_rows[:, :],
                                in_offset=bass.IndirectOffsetOnAxis(ap=IDXW[:, j, k:k + 1], axis=0), bounds_check=bnd_reg(e_, "w1"), oob_is_err=False),
                                f"wg{wq % 16}", reads=[R_IDXW], writes=[R_wgu[k]], extra=g1_done)
                            wq += 1
                        for k in range(8):
                            P.dmaf("pool", lambda e_, j=j, k=k, wdn=wdn: e_.indirect_dma_start(
                                out=wdn[:, k, :], out_offset=None, in_=wdn_rows[:, :],
                                in_offset=bass.IndirectOffsetOnAxis(ap=IDXW[:, j, k:k + 1], axis=0), bounds_check=bnd_reg(e_, "w1"), oob_is_err=False),
                                f"wg{wq % 16}", reads=[R_IDXW], writes=[R_wdn[k]], extra=g1_done)
                            wq += 1
                        hg, R_hg = hg_ring.bufs[0], hg_ring.regs[0]
                        if j == 0:
                            P.dma("sp", hg[:], hs[0:CAP, :].rearrange("(s p) d -> p s d", p=128), "hg0", reads=all_hs, writes=[R_hg], extra=g1_done)
                        hgT, R_hgT = hgT_ring.next()
                        for s_ in range(4):
                            pTe, R_pTe = pTe_ring.next()
                            P.op("pe", [tp(pTe[:, k, :], hg[:, s_, k * 128:(k + 1) * 128], identb) for k in range(8)], reads=[R_hg, R_cstb], writes=[R_pTe])
                            if s_ % 2 == 0:
                                P.op("dve", cp(hgT[:, :, s_ * 128:(s_ + 1) * 128], pTe[:]), reads=[R_pTe], writes=[R_hgT])
                            else:
                                P.op("act", actf(hgT[:, :, s_ * 128:(s_ + 1) * 128], pTe[:], AF.Copy), reads=[R_pTe], writes=[R_hgT])
                        if j + 1 < NTILE:
                            P.dma("sp", hg[:], hs[(j + 1) * CAP:(j + 2) * CAP, :].rearrange("(s p) d -> p s d", p=128), "hg0", reads=all_hs, writes=[R_hg], extra=g1_done)
                        actT, R_actT = actT_ring.next()
                        for fc in range(8):
                            psG, R_psG = psG_ring.next()
                            psU, R_psU = psU_ring.next()
                            P.op("pe", [mm(psG[:, :], wgu[:, k, fc * 128:(fc + 1) * 128], hgT[:, k, :], k == 0, k == 7) for k in range(8)],
                                 reads=R_wgu + [R_hgT], writes=[R_psG])
                            P.op("pe", [mm(psU[:, :], wgu[:, k, D + fc * 128:D + (fc + 1) * 128], hgT[:, k, :], k == 0, k == 7) for k in range(8)],
                                 reads=R_wgu + [R_hgT], writes=[R_psU])
                            sS, R_sS = sS_ring.next()
                            uS, R_uS = uS_ring.next()
                            P.op("act", actf(sS[:], psG[:], AF.Silu, bias=bgsel[:, j, fc:fc + 1], scale=ALPHA), reads=[R_psG, R_bsel], writes=[R_sS])
                            P.op("act", actf(uS[:], psU[:], AF.Identity, bias=bsel[:, j, 8 + fc:9 + fc]), reads=[R_psU, R_bsel], writes=[R_uS])
                            P.op("dve", ts(sS[:], sS[:], C7, 1.0 / ALPHA, ALU.min, ALU.mult), reads=[R_sS], writes=[R_sS])
                            P.op("dve", ts(uS[:], uS[:], LIM, -LIM, ALU.min, ALU.max), reads=[R_uS], writes=[R_uS])
                            P.op("dve", stt(actT[:, fc, :], uS[:], 1.0, sS[:], ALU.add, ALU.mult), reads=[R_uS, R_sS], writes=[R_actT])
                        for s_ in range(4):
                            ysb, R_ysb = ysb_ring.next()
                            for half in range(2):
                                pY, R_pY = psY_ring.next()
                                P.op("pe", [mm(pY[:, :], actT[:, fc, s_ * 128:(s_ + 1) * 128], wdn[:, fc, half * 512:(half + 1) * 512], fc == 0, fc == 7) for fc in range(8)],
                                     reads=R_wdn + [R_actT], writes=[R_pY])
                                if evi % 2 == 0:
                                    P.op("act", actf(ysb[:, half * 512:(half + 1) * 512], pY[:, :], AF.Copy), reads=[R_pY], writes=[R_ysb])
                                else:
                                    P.op("dve", cp(ysb[:, half * 512:(half + 1) * 512], pY[:, :]), reads=[R_pY], writes=[R_ysb])
                                evi += 1
                            P.dma("sp", ys[j * CAP + s_ * 128: j * CAP + (s_ + 1) * 128, :], ysb[:], f"ysb{(ysb_ring.i - 1) % 4}", reads=[R_ysb], writes=[R_ys[j * 4 + s_]])
                    e_done0 = [("Epe", P.cnt["pe"]), ("Eact", P.cnt["act"]), ("Edve", P.cnt["dve"])] + [r.w for r in R_ys]
                    for t in range(NT):
                        for half in range(2):
                            pY, R_pY = psY_ring.next()
                            P.op("pe", mm(pY[:, :], GT[0:E, t * 128:(t + 1) * 128], bd_sb[0:E, half * 512:(half + 1) * 512]),
                                 reads=[R_GT[t], R_bd], writes=[R_pY], extra=e_done0)
                            P.op("act", actf(XB[:, t, half * 512:(half + 1) * 512], pY[:, :], AF.Copy), reads=[R_pY], writes=[R_XB[t]], extra=e_done0)
                    e_done = [("Epe", P.cnt["pe"]), ("Eact", P.cnt["act"]), ("Edve", P.cnt["dve"])] + [r.w for r in R_ys]
                with contextlib.ExitStack() as s5:
                    yg_ring = Ring([sb(f"yg{l}{i}", [128, D], st=s5) for i in range(4)])
                    for yg, R_yg in zip(yg_ring.bufs, yg_ring.regs):
                        P.op("dve", lambda e, yg=yg: e.memset(yg[:], 0.0), writes=[R_yg], extra=e_done)
                    for t in range(NT):
                        for k in range(4):
                            yg, R_yg = yg_ring.next()
                            P.dmaf("pool", lambda e_, t=t, k=k, yg=yg: e_.indirect_dma_start(
                                out=yg[:, :], out_offset=None, in_=ys[:, :],
                                in_offset=bass.IndirectOffsetOnAxis(ap=SLOT[:, t, k:k + 1], axis=0), bounds_check=bnd_reg(e_), oob_is_err=False),
                                f"yg{(yg_ring.i - 1) % 4}", reads=R_ys + [R_SLOT[t]], writes=[R_yg], extra=e_done)
                            P.op("dve", stt(XB[:, t, :], yg[:], GK[:, t, k:k + 1], XB[:, t, :], ALU.mult, ALU.add), reads=[R_yg, R_GK[t], R_XB[t]], writes=[R_XB[t]])
                    g3_done = [("Epe", P.cnt["pe"]), ("Eact", P.cnt["act"]), ("Edve", P.cnt["dve"]), ("Epool", P.cnt["pool"])] + [r.w for r in yg_ring.regs]
                with contextlib.ExitStack() as s4:
                    gfb = sb(f"gfb{l}", [128, D], st=s4)
                    R_gfb = Reg()
                    P.dma("sp", gfb[:], bc(mv(l, 0, 5)), "c0", reads=[R_mvec[l]], writes=[R_gfb], extra=g3_done)
                    xt_ring = Ring([sb(f"mxt{l}{i}", [128, D], st=s4) for i in range(2)])
                    for t in range(NT):
                        xt, R_xt = xt_ring.next()
                        P.dma("sp", xt[:], xspill[t * 128:(t + 1) * 128, :], f"mxt{t % 2}", reads=[R_xspill], writes=[R_xt], extra=g3_done)
                        P.op("dve", tt(XB[:, t, :], XB[:, t, :], gfb[:], ALU.mult), reads=[R_XB[t], R_gfb], writes=[R_XB[t]])
                        P.op("dve", tt(XB[:, t, :], XB[:, t, :], xt[:], ALU.add), reads=[R_XB[t], R_xt], writes=[R_XB[t]])
                    return [P.op("dve", cp(dmy[:, 0:1], epsc[:, 0:1]), writes=[R_dmy], reads=[R_eps] + R_XB + xt_ring.regs),
                            ("Epe", P.cnt["pe"]), ("Eact", P.cnt["act"]), ("Edve", P.cnt["dve"])]

        moe0_done = (moe_sparse if SPARSE else moe)(0)

        with contextlib.ExitStack() as sh:
            hP = sb("hP", [128, NT, D], BF16, st=sh)
            R_hP = [Reg() for _ in range(NT)]
            bandsb = sb("bandsb", [128, 36, 128], BF16, st=sh)
            R_bands = Reg()
            P.dma("sp", bandsb[:], bands, "c0", writes=[R_bands], extra=moe0_done)
            A1b = sb("A1b", [128, D], st=sh)
            B1b = sb("B1b", [128, D], st=sh)
            tmpb = sb("tmpb", [128, D], st=sh)
            gpb = sb("gpb", [128, D], st=sh)
            R_A1b, R_B1b, R_tmpb, R_gpb = Reg(), Reg(), Reg(), Reg()
            P.dma("sp", A1b[:], bc(mv(1, 0, 1)), "c0", reads=[R_mvec[1]], writes=[R_A1b], extra=moe0_done)
            P.dma("sp", tmpb[:], bc(norm_mix[1]), "c0", writes=[R_tmpb], extra=moe0_done)
            P.op("dve", stt(A1b[:], A1b[:], 1.0, tmpb[:], ALU.add, ALU.mult), reads=[R_A1b, R_tmpb], writes=[R_A1b])
            P.dma("sp", B1b[:], bc(mv(1, 0, 0)), "c0", reads=[R_mvec[1]], writes=[R_B1b], extra=moe0_done)
            P.dma("sp", gpb[:], bc(mv(1, 0, 2)), "c0", reads=[R_mvec[1]], writes=[R_gpb], extra=moe0_done)
            P.dma("sp", tmpb[:], bc(pool_scale), "c0", writes=[R_tmpb], reads=[R_tmpb])
            P.op("dve", tt(gpb[:], gpb[:], tmpb[:], ALU.mult), reads=[R_gpb, R_tmpb], writes=[R_gpb])
            Wpb = sb("Wpb", [128, 4, 2, 256], BF16, st=sh)
            R_Wpb = Reg()
            pws = sb("pws", [128, 4, 2, 256], st=sh)
            R_pws = Reg()
            for gi in range(4):
                P.dma("sp", pws[:, gi, :, :], pool_w[gi].rearrange("(c p) n -> p c n", p=128), "c0", writes=[R_pws], extra=moe0_done)
            for gi in range(4):
                for cc in range(2):
                    P.op("dve", tt(Wpb[:, gi, cc, :], pws[:, gi, cc, :], gpb[:, gi * 256:(gi + 1) * 256], ALU.mult), reads=[R_pws, R_gpb], writes=[R_Wpb])
            st_ring = Ring([sb(f"pst{i}", [128, 2], st=sh) for i in range(2)])
            junk = sb("pjunk", [128, D], BF16, st=sh)
            R_junk = Reg()
            for t in range(NT):
                stt_, R_st = st_ring.next()
                rstd_tile(XB[:, t, :], junk[:], R_junk, stt_, R_st, [R_XB[t]])
                P.op("dve", stt(tmpb[:], XB[:, t, :], stt_[:, 1:2], A1b[:], ALU.mult, ALU.mult), reads=[R_XB[t], R_st, R_A1b, R_tmpb], writes=[R_tmpb])
                P.op("dve", tt(hP[:, t, :], tmpb[:], B1b[:], ALU.add), reads=[R_tmpb, R_B1b], writes=[R_hP[t]])
                if t == 16:
                    P.op("dve", ts(hP[:, t, :], hP[:, t, :], hm[:, 0:1], None, ALU.mult), reads=[R_hP[t], R_hm], writes=[R_hP[t]])
            psP_ring = Ring([ps(f"psP{i}", [128, 8, 128], st=sh) for i in range(2)])
            psY2_ring = Ring([ps(f"psY2{i}", [128, D], st=sh) for i in range(2)])
            pT_ring2 = Ring([sb(f"plT{i}", [128, 8, 128], BF16, st=sh) for i in range(2)])
            for t in range(16):
                if t == 0:
                    srcs = [(16, 7), (0, 3), (1, 4)]
                elif t == 15:
                    srcs = [(14, 5), (15, 6), (16, 8)]
                else:
                    srcs = [(t - 1, 0), (t, 1), (t + 1, 2)]
                psP, R_psP = psP_ring.next()
                fns = []
                for dc in range(8):
                    gi = dc // 2
                    for si, (st_, kind) in enumerate(srcs):
                        fns.append(mm(psP[:, dc, :], hP[:, st_, dc * 128:(dc + 1) * 128], bandsb[:, gi * 9 + kind, :], si == 0, si == 2))
                P.op("pe", fns, reads=[R_hP[s_] for s_, _ in srcs] + [R_bands], writes=[R_psP])
                plT, R_plT = pT_ring2.next()
                P.op("act", actf(plT[:], psP[:], AF.Copy), reads=[R_psP], writes=[R_plT])
                psY2, R_psY2 = psY2_ring.next()
                fns = []
                for gi in range(4):
                    for cc in range(2):
                        fns.append(mm(psY2[:, gi * 256:(gi + 1) * 256], plT[:, gi * 2 + cc, :], Wpb[:, gi, cc, :], cc == 0, cc == 1))
                P.op("pe", fns, reads=[R_plT, R_Wpb], writes=[R_psY2])
                for half in range(2):
                    P.op("dve", tt(XB[:, t, half * 512:(half + 1) * 512], XB[:, t, half * 512:(half + 1) * 512], psY2[:, half * 512:(half + 1) * 512], ALU.add),
                         reads=[R_psY2, R_XB[t]], writes=[R_XB[t]])
            for t in range(NT):
                P.dma("sp", xspill[t * 128:(t + 1) * 128, :], XB[:, t, :], f"xb{t % 4}", reads=[R_XB[t]], writes=[R_xspill])
            pool_done = [P.op("dve", cp(dmy[:, 0:1], epsc[:, 0:1]), writes=[R_dmy], reads=[R_eps] + R_XB + R_hP),
                         ("Epe", P.cnt["pe"]), ("Eact", P.cnt["act"]), R_xspill.w]
        for t in range(4):
            pool_done.append(("Dxb%d" % t, P.dma_sems["xb%d" % t]))
        P.wait("pool", pool_done)
        P.wait("pe", pool_done)
        P.wait("act", pool_done)
        P.wait("dve", pool_done)
        P.wait("sp", pool_done)

        moe1_done = (moe_sparse if SPARSE else moe)(1)

        with contextlib.ExitStack() as sj:
            fnb = sb("fnb", [128, D], st=sj)
            R_fnb = Reg()
            P.dma("sp", fnb[:], bc(final_norm), "c0", writes=[R_fnb], extra=moe1_done)
            st_ring = Ring([sb(f"jst{i}", [128, 2], st=sj) for i in range(2)])
            junk = sb("jjunk", [128, D], BF16, st=sj)
            R_junk = Reg()
            ot_ring = Ring([sb(f"ot{i}", [128, D], st=sj) for i in range(2)])
            outs = []
            for t in range(16):
                stt_, R_st = st_ring.next()
                rstd_tile(XB[:, t, :], junk[:], R_junk, stt_, R_st, [R_XB[t]])
                ot, R_ot = ot_ring.next()
                P.op("dve", stt(ot[:], XB[:, t, :], stt_[:, 1:2], fnb[:], ALU.mult, ALU.mult), reads=[R_XB[t], R_st, R_fnb], writes=[R_ot])
                outs.append(P.dma("sp", out[t * 128:(t + 1) * 128, :], ot[:], f"ot{t % 2}", reads=[R_ot]))
            P.wait("sp", outs[-2:])
        P.emit()
    return nc


def _rope_tables(pos_row, pos_col):
    half = 32
    inv = (np.float32(10000.0) ** (-(np.arange(half, dtype=np.float32) / np.float32(half)))).astype(np.float32)
    ang_r = pos_row.astype(np.float32)[None, :] * inv[:, None]
    ang_c = pos_col.astype(np.float32)[None, :] * inv[:, None]
    cos = np.concatenate([np.cos(ang_r), np.cos(ang_r), np.cos(ang_c), np.cos(ang_c)], 0).astype(np.float32)
    sin = np.concatenate([np.sin(ang_r), np.sin(ang_r), np.sin(ang_c), np.sin(ang_c)], 0).astype(np.float32)
    return np.stack([cos, sin], 0)


def _coef(s, t, w):
    lo = np.clip(t - w // 2, 0, S)
    hi = np.clip(t - w // 2 + w, 0, S)
    cnt = (hi - lo).astype(np.float32)
    inside = (s >= lo) & (s < hi) & (s >= 0) & (s < S)
    return np.where(inside, np.float32(1.0) / cnt, np.float32(0.0)).astype(np.float32) - (s == t).astype(np.float32)


def _bands(c):
    base = OWN * c
    out = np.zeros((128, 36, 128), np.float32)
    i = np.arange(128)[:, None]
    j = np.arange(128)[None, :]
    for gi, w in enumerate((2, 4, 8, 16)):
        gb = OWN * 3 + 128 * 5
        def blk(src0, dst0):
            return _coef(src0 + i + 0 * j, dst0 + j + 0 * i, w)
        kinds = [
            blk(gb - 128, gb), blk(gb, gb), blk(gb + 128, gb),
            blk(base, base), blk(base + 128, base),
            blk(base + 128 * 14, base + 128 * 15), blk(base + 128 * 15, base + 128 * 15),
        ]
        hl = np.zeros((128, 128), np.float32)
        hl[0:8] = _coef(base - 8 + np.arange(8)[:, None] + 0 * j, base + j + 0 * np.arange(8)[:, None], w)
        hr = np.zeros((128, 128), np.float32)
        hr[8:16] = _coef(base + OWN + np.arange(8)[:, None] + 0 * j, base + 128 * 15 + j + 0 * np.arange(8)[:, None], w)
        kinds += [hl, hr]
        for k, m in enumerate(kinds):
            out[:, gi * 9 + k, :] = m
    return out.astype(ml_dtypes.bfloat16)


def _consts():
    cs = np.zeros((7, 128, 128), np.float32)
    cs[5, :, 40:48] = (np.arange(8)[None, :] * 128 + np.arange(128)[:, None]).astype(np.float32)
    cs[4] = (np.arange(128)[:, None] < np.arange(128)[None, :]).astype(np.float32)
    cs[5, :, 0:32] = np.arange(32, dtype=np.float32)[None, :]
    cs[5, :16, 32] = 1.0
    cs[5, 16:, 33] = 1.0e6
    cs[0] = np.eye(128, dtype=np.float32)
    cs[1] = 1.0 / 128.0
    rot = np.zeros((128, 128), np.float32)
    for m in range(128):
        if (m % 64) < 32:
            rot[m + 32, m] = -1.0
        else:
            rot[m - 32, m] = 1.0
    cs[2] = rot
    cs[3] = 1.0
    return cs


def prep(inputs):
    f = lambda a: np.ascontiguousarray(np.asarray(a, dtype=np.float32))
    x = f(inputs["x"])[0]
    ctx = f(inputs["ctx"])[0]
    tpos = np.arange(S)
    ropek = _rope_tables(tpos // 64, tpos % 64)
    shared = {
        "x_all": x, "ctx": ctx,
        "c2": np.stack([f(inputs["c"])[0], f(inputs["c_ctx"])], 0),
        "ada_w": f(inputs["ada_w"]), "ada_b": f(inputs["ada_b"]),
        "norm_mix": f(inputs["norm_mix"]), "norm_ffn": f(inputs["norm_ffn"]),
        "wqkv": f(inputs["attn_w_qkv"])[0],
        "qk_g": np.stack([f(inputs["attn_q_norm"])[0], f(inputs["attn_k_norm"])[0]], 0),
        "wo": f(inputs["attn_w_o"])[0], "pool_w": f(inputs["pool_w"])[0], "pool_scale": f(inputs["pool_scale"])[0],
        "router_w": f(inputs["moe_router_w"]), "router_b": f(inputs["moe_router_b"]),
        "w_gu": f(inputs["moe_w_gu"]), "b_gu": f(inputs["moe_b_gu"]),
        "w_down": f(inputs["moe_w_down"]), "b_down": f(inputs["moe_b_down"]),
        "final_norm": f(inputs["final_norm"]), "ropek": ropek, "consts": _consts(),
    }
    maps = []
    for c in range(NCORES):
        base = OWN * c
        idx = np.zeros(NEXT, np.int64)
        idx[:OWN] = base + np.arange(OWN)
        idx[OWN:OWN + 8] = base - 8 + np.arange(8) if c > 0 else base + np.arange(8)
        idx[OWN + 8:OWN + 16] = base + OWN + np.arange(8) if c < NCORES - 1 else base + np.arange(8)
        idx[OWN + 16:] = base
        x_ext = x[idx].copy()
        x_ext[OWN + 16:] = 0.0
        hmask = np.zeros((128, 1), np.float32)
        if c > 0:
            hmask[0:8] = 1.0
        if c < NCORES - 1:
            hmask[8:16] = 1.0
        m = dict(shared)
        m.update({"x_ext": x_ext, "ropeq": np.ascontiguousarray(ropek[:, :, idx]), "bands": _bands(c), "hmask": hmask})
        maps.append(m)
    return maps


_NC_CACHE = {}


def kernel(**inputs):
    maps = prep(inputs)
    if "nc" not in _NC_CACHE:
        _NC_CACHE["nc"] = build()
    res = run_bass_kernel_spmd(_NC_CACHE["nc"], maps, core_ids=list(range(NCORES)))
    return np.concatenate([r["out"] for r in res.results], axis=0)[None].astype(np.float32)
```
